# Optimizing a Trainium2 kernel written in Bass

```python
import jax, jax.numpy as jnp
from jax import lax
import numpy as np

D_MODEL = 2048
BATCH = 2
SEQ = 4096
DEPTH = 1

CHUNK = 64
Q_BLOCK = 128
ROPE_THETA = 500000.0
N_MEM = 256
EPS = 1e-6

MLA_HEADS = 8
MLA_NOPE = 128
MLA_ROPE = 64
MLA_V = 128
MLA_Q_LORA = 512
MLA_KV_LORA = 256
DSA_HEADS = 8
DSA_HEAD_DIM = 128
DSA_ROT = DSA_HEAD_DIM // 4
IDX_HEADS = 16
IDX_DIM = 64
IDX_ROT = IDX_DIM // 4
TOPK_MAX = 256
X_HEADS = 4
X_HEAD_DIM = 128
D_FF = 4 * D_MODEL

MIX_WIDTH = MLA_HEADS * MLA_V + DSA_HEADS * DSA_HEAD_DIM
IN_SPLITS = (
    MLA_Q_LORA,
    MLA_KV_LORA,
    MLA_ROPE,
    DSA_HEADS * DSA_HEAD_DIM,
    DSA_HEADS * DSA_HEAD_DIM,
    DSA_HEADS * DSA_HEAD_DIM,
    IDX_HEADS * IDX_DIM,
    IDX_DIM,
    IDX_HEADS,
)
D_IN = int(sum(IN_SPLITS))

kernel_name = "hymba_mla_dsa_stream_block"


def rmsnorm(x, g):
    xf = x.astype(jnp.float32)
    y = xf * lax.rsqrt(jnp.mean(xf * xf, axis=-1, keepdims=True) + EPS)
    return (y * g.astype(jnp.float32)).astype(x.dtype)


def rope(x, pos, rot_dim):
    half = rot_dim // 2
    inv_freq = ROPE_THETA ** (-jnp.arange(half, dtype=jnp.float32) / half)
    ang = pos.astype(jnp.float32)[..., None] * inv_freq
    ang = ang.reshape(ang.shape[:2] + (1,) * (x.ndim - 3) + (half,))
    cos, sin = jnp.cos(ang), jnp.sin(ang)
    xf = x.astype(jnp.float32)
    x1, x2, rest = xf[..., :half], xf[..., half:rot_dim], xf[..., rot_dim:]
    out = jnp.concatenate([x1 * cos - x2 * sin, x2 * cos + x1 * sin, rest], axis=-1)
    return out.astype(x.dtype)


def to_blocks(a):
    b, s = a.shape[:2]
    a = a.reshape((b, s // Q_BLOCK, Q_BLOCK) + a.shape[2:])
    return jnp.moveaxis(a, 1, 0)


def from_blocks(a):
    a = jnp.moveaxis(a, 0, 1)
    return a.reshape((a.shape[0], a.shape[1] * a.shape[2]) + a.shape[3:])


def query_chunks(i):
    return (i * Q_BLOCK + jnp.arange(Q_BLOCK)) // CHUNK


def mla_mixer(c_q, c_kv, k_rope, pos, g_cq, g_ckv, w_qb, w_kvb):
    b, s, _ = c_q.shape
    q = (rmsnorm(c_q, g_cq) @ w_qb).reshape(b, s, MLA_HEADS, MLA_NOPE + MLA_ROPE)
    q_nope = q[..., :MLA_NOPE]
    q_rope = rope(q[..., MLA_NOPE:], pos, MLA_ROPE)
    kv = (rmsnorm(c_kv, g_ckv) @ w_kvb).reshape(b, s, MLA_HEADS, MLA_NOPE + MLA_V)
    k_nope, v = kv[..., :MLA_NOPE], kv[..., MLA_NOPE:]
    k_rope = rope(k_rope, pos, MLA_ROPE)
    scale = (MLA_NOPE + MLA_ROPE) ** -0.5
    key_chunk = jnp.arange(s) // CHUNK

    def block(args):
        i, qn, qr = args
        sc = (jnp.einsum('bqhd,bkhd->bhqk', qn, k_nope)
              + jnp.einsum('bqhr,bkr->bhqk', qr, k_rope)).astype(jnp.float32) * scale
        allowed = key_chunk[None, :] <= query_chunks(i)[:, None]
        sc = jnp.where(allowed[None, None], sc, -jnp.inf)
        p = jax.nn.softmax(sc, axis=-1).astype(v.dtype)
        return jnp.einsum('bhqk,bkhd->bqhd', p, v)

    o = lax.map(block, (jnp.arange(s // Q_BLOCK), to_blocks(q_nope), to_blocks(q_rope)))
    return from_blocks(o).reshape(b, s, MLA_HEADS * MLA_V)


def dsa_mixer(q, k, v, q_idx, k_idx, w_idx, pos):
    b, s, _ = q.shape
    q = rope(q.reshape(b, s, DSA_HEADS, DSA_HEAD_DIM), pos, DSA_ROT)
    k = rope(k.reshape(b, s, DSA_HEADS, DSA_HEAD_DIM), pos, DSA_ROT)
    v = v.reshape(b, s, DSA_HEADS, DSA_HEAD_DIM)
    q_idx = rope(q_idx.reshape(b, s, IDX_HEADS, IDX_DIM), pos, IDX_ROT)
    k_idx = rope(k_idx, pos, IDX_ROT)
    w_idx = w_idx * (IDX_HEADS ** -0.5)
    n_sel = min(TOPK_MAX, s // 4)
    key_chunk = jnp.arange(s) // CHUNK
    gather = jax.vmap(lambda arr, idx: arr[idx])

    def block(args):
        i, qb, qib, wb = args
        qc = query_chunks(i)
        logits = jax.nn.relu(jnp.einsum('bqhd,bkd->bqhk', qib, k_idx).astype(jnp.float32) * (IDX_DIM ** -0.5))
        score = jnp.einsum('bqhk,bqh->bqk', logits, wb.astype(jnp.float32))
        allowed = key_chunk[None, :] <= qc[:, None]
        score = jnp.where(allowed[None], score, -jnp.inf)
        _, sel = lax.top_k(score, n_sel)
        sel_ok = (sel // CHUNK) <= qc[None, :, None]
        k_sel = gather(k, sel)
        v_sel = gather(v, sel)
        sc = jnp.einsum('bqhd,bqnhd->bhqn', qb, k_sel).astype(jnp.float32) * (DSA_HEAD_DIM ** -0.5)
        sc = jnp.where(sel_ok[:, None], sc, -jnp.inf)
        p = jax.nn.softmax(sc, axis=-1).astype(v.dtype)
        return jnp.einsum('bhqn,bqnhd->bqhd', p, v_sel)

    o = lax.map(block, (jnp.arange(s // Q_BLOCK), to_blocks(q), to_blocks(q_idx), to_blocks(w_idx)))
    return from_blocks(o).reshape(b, s, DSA_HEADS * DSA_HEAD_DIM)


def memory_cross_attention(h, mem_n, wq, wk, wv, wo):
    b, s, _ = h.shape
    m = mem_n.shape[1]
    q = (h @ wq).reshape(b, s, X_HEADS, X_HEAD_DIM)
    k = (mem_n @ wk).reshape(b, m, X_HEADS, X_HEAD_DIM)
    v = (mem_n @ wv).reshape(b, m, X_HEADS, X_HEAD_DIM)
    sc = jnp.einsum('bshd,bmhd->bhsm', q, k).astype(jnp.float32) * (X_HEAD_DIM ** -0.5)
    p = jax.nn.softmax(sc, axis=-1).astype(v.dtype)
    o = jnp.einsum('bhsm,bmhd->bshd', p, v).reshape(b, s, X_HEADS * X_HEAD_DIM)
    return o @ wo


def setup_inputs(seed: int = 0) -> dict:
    key = jax.random.key(seed)
    ks = jax.random.split(key, 24)
    f32 = jnp.float32

    def w(k, shape, fan_in):
        return jax.random.normal(k, (DEPTH,) + shape, f32) * (fan_in ** -0.5)

    def gain(k, n):
        return 1.0 + 0.02 * jax.random.normal(k, (DEPTH, n), f32)

    x = jax.random.normal(ks[0], (BATCH, SEQ, D_MODEL), f32)
    mem = jax.random.normal(ks[1], (BATCH, N_MEM, D_MODEL), f32)
    offset = jax.random.randint(ks[2], (BATCH, 1), 0, 16, dtype=jnp.int32) * CHUNK
    positions = (offset + jnp.arange(SEQ, dtype=jnp.int32)[None, :]).astype(jnp.int32)
    return {
        "x": x,
        "mem": mem,
        "positions": positions,
        "g_mix": gain(ks[3], D_MODEL),
        "w_in": w(ks[4], (D_MODEL, D_IN), D_MODEL),
        "g_cq": gain(ks[5], MLA_Q_LORA),
        "g_ckv": gain(ks[6], MLA_KV_LORA),
        "w_qb": w(ks[7], (MLA_Q_LORA, MLA_HEADS * (MLA_NOPE + MLA_ROPE)), MLA_Q_LORA),
        "w_kvb": w(ks[8], (MLA_KV_LORA, MLA_HEADS * (MLA_NOPE + MLA_V)), MLA_KV_LORA),
        "w_out": w(ks[9], (MIX_WIDTH, D_MODEL), MIX_WIDTH),
        "g_cross": gain(ks[10], D_MODEL),
        "g_mem": gain(ks[11], D_MODEL),
        "w_q_cross": w(ks[12], (D_MODEL, X_HEADS * X_HEAD_DIM), D_MODEL),
        "w_k_cross": w(ks[13], (D_MODEL, X_HEADS * X_HEAD_DIM), D_MODEL),
        "w_v_cross": w(ks[14], (D_MODEL, X_HEADS * X_HEAD_DIM), D_MODEL),
        "w_o_cross": w(ks[15], (X_HEADS * X_HEAD_DIM, D_MODEL), X_HEADS * X_HEAD_DIM),
        "g_mlp": gain(ks[16], D_MODEL),
        "w_up": w(ks[17], (D_MODEL, D_FF), D_MODEL),
        "w_down": w(ks[18], (D_FF, D_MODEL), D_FF),
        "g_final": 1.0 + 0.02 * jax.random.normal(ks[19], (D_MODEL,), f32),
    }


def reference(x, mem, positions, g_mix, w_in, g_cq, g_ckv, w_qb, w_kvb, w_out,
              g_cross, g_mem, w_q_cross, w_k_cross, w_v_cross, w_o_cross,
              g_mlp, w_up, w_down, g_final):
    offsets = np.cumsum(np.array(IN_SPLITS))[:-1].tolist()
    for l in range(DEPTH):
        h = rmsnorm(x, g_mix[l])
        c_q, c_kv, k_rope, q_d, k_d, v_d, q_idx, k_idx, w_idx = jnp.split(h @ w_in[l], offsets, axis=-1)
        a = mla_mixer(c_q, c_kv, k_rope, positions, g_cq[l], g_ckv[l], w_qb[l], w_kvb[l])
        bb = dsa_mixer(q_d, k_d, v_d, q_idx, k_idx, w_idx, positions)
        x = x + jnp.concatenate([a, bb], axis=-1) @ w_out[l]
        hc = rmsnorm(x, g_cross[l])
        mem_n = rmsnorm(mem, g_mem[l])
        x = x + memory_cross_attention(hc, mem_n, w_q_cross[l], w_k_cross[l], w_v_cross[l], w_o_cross[l])
        hm = rmsnorm(x, g_mlp[l])
        x = x + jnp.square(jax.nn.relu(hm @ w_up[l])) @ w_down[l]
    return rmsnorm(x, g_final)
```

```python
import math
from contextlib import ExitStack

import numpy as np
import concourse.bass as bass
import concourse.mybir as mybir
from concourse.bass_utils import run_bass_kernel_spmd

F32 = mybir.dt.float32
BF16 = mybir.dt.bfloat16
I32 = mybir.dt.int32
AF = mybir.ActivationFunctionType
ALU = mybir.AluOpType
AX = mybir.AxisListType
PI = math.pi

D = 2048
S = 4096
NT = 32
NQ = 8
EPS = 1e-6
THETA = 500000.0
DFF = 8192
NBIS = 18
import os
KVV = int(os.environ.get('KVV', '0'))
BIS_POOL = tuple(int(c) for c in os.environ.get('BIS_POOL', '').split(',') if c != '')
BIG = 1.0e30

ENGS = ("pe", "act", "dve", "pool", "sp")


class Buf:
    __slots__ = ("name", "w", "r", "excl", "strict")

    def __init__(self, name=""):
        self.name = name
        self.w = None
        self.r = []
        self.excl = False
        self.strict = False


class TT:
    def __init__(self, t, name):
        self.t = t
        self.b = Buf(name)

    def __getitem__(self, k):
        return self.t[k]


def _b(x):
    return x.b if isinstance(x, TT) else x


class Op:
    __slots__ = ("eng", "fn", "dma", "deps", "signal", "tok", "slot_wait", "epoch")

    def __init__(self, eng, fn, dma):
        self.eng = eng
        self.fn = fn
        self.dma = dma
        self.deps = []
        self.signal = False
        self.tok = None
        self.slot_wait = None
        self.epoch = 0


class Prog:
    def __init__(self, nslots=6):
        self.ops = []
        self.nslots = nslots
        self.epoch = 0
        self.barriers = []
        self.stopped = False
        self.last = {}

    def op(self, eng, fn, reads=(), writes=(), dma=False):
        if self.stopped:
            return None
        o = Op(eng, fn, dma)
        o.epoch = self.epoch
        deps = {}
        reads = [_b(x) for x in reads]
        writes = [_b(x) for x in writes]
        writes = writes + [b for b in reads if b.excl and b not in writes]
        reads = [b for b in reads if not b.excl]
        for b in reads:
            if b.w is not None:
                deps[id(b.w)] = (b.w, True)
        for b in writes:
            if b.w is not None and id(b.w) not in deps:
                deps[id(b.w)] = (b.w, b.strict)
            for r in b.r:
                if id(r) not in deps:
                    deps[id(r)] = (r, b.strict)
        for d, raw in deps.values():
            if d.epoch != o.epoch:
                continue
            if (not d.dma) and (not dma) and d.eng == eng:
                if eng == "pe" or not raw:
                    continue
            d.signal = True
            o.deps.append(d)
        for b in map(_b, reads):
            if not dma:
                b.r = [r for r in b.r if r.dma or r.eng != eng or r.epoch != o.epoch]
            b.r.append(o)
        for b in map(_b, writes):
            b.w = o
            b.r = []
        self.ops.append(o)
        if not dma:
            self.last[eng] = o
        return o

    def barrier(self):
        if self.stopped:
            return
        for e, o in self.last.items():
            o.signal = True
        self.last = {}
        self.epoch += 1
        self.barriers.append(len(self.ops))

    def dma(self, q, out, in_, reads=(), writes=(), **kw):
        return self.op(q, lambda e: e.dma_start(out=out, in_=in_, **kw), reads, writes, dma=True)

    def emit(self, nc, final_waits=()):
        with ExitStack() as st:
            esem = {e: st.enter_context(nc.semaphore("s_" + e)) for e in ("pe", "act", "dve", "pool")}
            qsem = {q: [st.enter_context(nc.semaphore("d_%s%d" % (q, i))) for i in range(self.nslots)]
                    for q in ("sp", "act", "pool")}
            ecnt = {e: 0 for e in esem}
            qn = {q: 0 for q in qsem}
            qtot = {q: [0] * self.nslots for q in qsem}
            bar_toks = []
            nb = 0
            for i, o in enumerate(self.ops):
                while nb < len(self.barriers) and self.barriers[nb] == i:
                    toks = [(esem[e], ecnt[e]) for e in esem if ecnt[e] > 0]
                    for q in qsem:
                        for s_ in range(self.nslots):
                            if qtot[q][s_] > 0:
                                toks.append((qsem[q][s_], qtot[q][s_]))
                    bar_toks.append(toks)
                    nb += 1
                if o.dma:
                    q = o.eng
                    s_ = qn[q] % self.nslots
                    qn[q] += 1
                    if qtot[q][s_] > 0:
                        o.slot_wait = (qsem[q][s_], qtot[q][s_])
                    qtot[q][s_] += 16
                    o.tok = (qsem[q][s_], qtot[q][s_])
                elif o.signal:
                    ecnt[o.eng] += 1
                    o.tok = (esem[o.eng], ecnt[o.eng])
            while nb < len(self.barriers):
                bar_toks.append([])
                nb += 1
            per = {e: [o for o in self.ops if o.eng == e] for e in ENGS}
            block = st.enter_context(nc.Block())

            def run(e, eng):
                waited = {}
                cur_epoch = 0

                def w(tok):
                    sem, val = tok
                    k = id(sem)
                    if waited.get(k, 0) < val:
                        eng.wait_ge(sem, val)
                        waited[k] = val

                for o in per[e]:
                    while cur_epoch < o.epoch:
                        for t in bar_toks[cur_epoch]:
                            w(t)
                        cur_epoch += 1
                    if o.slot_wait is not None:
                        w(o.slot_wait)
                    for d in o.deps:
                        w(d.tok)
                    inst = o.fn(eng)
                    if o.dma:
                        inst.then_inc(o.tok[0], 16)
                    elif o.signal:
                        inst.then_inc(o.tok[0], 1)
                if e == "sp":
                    for o in final_waits:
                        w(o.tok)

            @block.tensor
            def _(eng):
                run("pe", eng)

            @block.scalar
            def _(eng):
                run("act", eng)

            @block.vector
            def _(eng):
                run("dve", eng)

            @block.gpsimd
            def _(eng):
                run("pool", eng)

            @block.sync
            def _(eng):
                run("sp", eng)


class _Stop(Exception):
    pass


def build_program(stop=None):
    nc = bass.Bass("TRN2", target_bir_lowering=False)
    dump_ops = []

    def dump(ph, name, ap, shape, dt):
        if stop != ph or P.stopped:
            return
        o_ = nc.dram_tensor("dbg_" + name, list(shape), dt, kind="ExternalOutput").ap()
        P.barrier()
        dump_ops.append(P.dma("sp", o_, ap))

    def checkpoint(name):
        if stop == name:
            P.stopped = True

    def din(name, shape, dt=F32):
        return nc.dram_tensor(name, list(shape), dt, kind="ExternalInput").ap()

    x_all = din("x_all", [S, D])
    xq = din("xq", [NQ * 128, D])
    posk = din("posk", [128, NT], I32)
    posq = din("posq", [128, NQ], I32)
    mem = din("mem", [256, D])
    invf = din("invf", [128, 32])
    pow2 = din("pow2", [128, NBIS])
    ident = din("ident", [128, 128])
    zmT = din("zmT", [128, NQ, 512])
    zb = din("zb", [128, NQ, 512])
    wk_side = din("wk_side", [D, 2432])
    wq_side = din("wq_side", [D, 2576])
    g_mix = din("g_mix", [128, 16])
    w_qb = din("w_qb", [512, 1536])
    g_cq = din("g_cq", [128, 4])
    w_kvb = din("w_kvb", [256, 2048])
    g_ckv = din("g_ckv", [128, 2])
    w_out = din("w_out", [D, D])
    g_cross = din("g_cross", [128, 16])
    g_mem = din("g_mem", [128, 16])
    wqc = din("wqc", [D, 512])
    wkc = din("wkc", [D, 512])
    wvc = din("wvc", [D, 512])
    woc = din("woc", [512, D])
    g_mlp = din("g_mlp", [128, 16])
    w_up = din("w_up", [D, DFF])
    w_down = din("w_down", [DFF, D])
    g_fin = din("g_fin", [128, D])
    out = nc.dram_tensor("out", [NQ * 128, D], F32, kind="ExternalOutput").ap()

    def dscr(name, shape):
        return nc.dram_tensor(name, list(shape), BF16, kind="Internal").ap()

    KnT_scr = dscr("KnT_scr", [128, 8, S])
    KdT_scr = dscr("KdT_scr", [128, 8, S])
    Vm_scr = dscr("Vm_scr", [8, 128, NT, 129])
    Vd_scr = dscr("Vd_scr", [8, 128, NT, 129])
    maskT_scr = dscr("maskT_scr", [128, 144, 128])
    O_scr = dscr("O_scr", [NQ * 128, D])

    P = Prog()
    cnt = [0]

    def MM(out_, lhsT, rhs, start, stop, reads, writes):
        P.op("pe", lambda e: e.matmul(out_, lhsT=lhsT, rhs=rhs, start=start, stop=stop), reads, writes)

    def ACTV(out_, in_, func, reads, writes, **kw):
        P.op("act", lambda e: e.activation(out=out_, in_=in_, func=func, **kw), reads, writes)

    def TS(eng, out_, in0, s1, s2, op0, op1, reads, writes, accum_out=None):
        if op1 is None:
            P.op(eng, lambda e: e.tensor_scalar(out=out_, in0=in0, scalar1=s1, scalar2=None, op0=op0), reads, writes)
        elif accum_out is None:
            P.op(eng, lambda e: e.tensor_scalar(out=out_, in0=in0, scalar1=s1, scalar2=s2, op0=op0, op1=op1), reads, writes)
        else:
            P.op(eng, lambda e: e.tensor_scalar(out=out_, in0=in0, scalar1=s1, scalar2=s2, op0=op0, op1=op1,
                                                accum_out=accum_out), reads, writes)

    def TTO(eng, out_, in0, in1, op, reads, writes):
        P.op(eng, lambda e: e.tensor_tensor(out=out_, in0=in0, in1=in1, op=op), reads, writes)

    def STT(eng, out_, in0, scalar, in1, op0, op1, reads, writes):
        P.op(eng, lambda e: e.scalar_tensor_tensor(out=out_, in0=in0, scalar=scalar, in1=in1, op0=op0, op1=op1),
             reads, writes)

    def CP(eng, out_, in_, reads, writes):
        if eng == "act":
            ACTV(out_, in_, AF.Copy, reads, writes)
        else:
            P.op(eng, lambda e: e.tensor_copy(out=out_, in_=in_), reads, writes)

    def MEMSET(eng, ap, val, writes):
        P.op(eng, lambda e: e.memset(ap, val), (), writes)

    with ExitStack() as top:
      try:
        def alloc(st, name, shape, dt):
            cnt[0] += 1
            t_ = TT(st.enter_context(nc.sbuf_tensor("%s_%d" % (name, cnt[0]), list(shape), dt)), name)
            t_.b.strict = name.startswith("junk")
            return t_

        def palloc(st, name, shape, dt):
            cnt[0] += 1
            t_ = TT(st.enter_context(nc.psum_tensor("%s_%d" % (name, cnt[0]), list(shape), dt)), name)
            t_.b.excl = True
            return t_

        class Ring:
            def __init__(self, st, name, n, shape, dt, psum=False):
                self.items = [(palloc if psum else alloc)(st, "%s%d" % (name, i), shape, dt) for i in range(n)]
                self.i = 0

            def next(self):
                t = self.items[self.i % len(self.items)]
                self.i += 1
                return t

        def TRN(out_, in_, reads, writes):
            P.op("pe", lambda e: e.transpose(out=out_, in_=in_, identity=identb[:]), list(reads) + [identb], writes)

        identf = alloc(top, "identf", [128, 128], F32)
        identb = alloc(top, "identb", [128, 128], BF16)
        invf_t = alloc(top, "invf", [128, 32], F32)
        pow2_t = alloc(top, "pow2", [128, NBIS], F32)
        mhalf = alloc(top, "mhalf", [128, 1], F32)
        stK = ExitStack()
        kropeT = alloc(stK, "kropeT", [128, S], BF16)
        kidx_lo = alloc(stK, "kidx_lo", [128, S], BF16)
        kidx_hi = alloc(stK, "kidx_hi", [128, S], BF16)

        P.dma("sp", identf[:], ident, writes=[identf])
        P.dma("sp", invf_t[:], invf, writes=[invf_t])
        P.dma("sp", pow2_t[:], pow2, writes=[pow2_t])
        CP("dve", identb[:], identf[:], [identf], [identb])
        MEMSET("pool", mhalf[:], -0.5, [mhalf])
        MEMSET("pool", kidx_lo[:], 0.0, [kidx_lo])
        MEMSET("pool", kidx_hi[:], 0.0, [kidx_hi])

        def make_tables(pos_dram, n, cos_t, sin_t):
            with ExitStack() as st:
                posi = alloc(st, "posi", [128, n], I32)
                posf = alloc(st, "posf", [128, n], F32)
                ang = alloc(st, "ang", [128, n, 32], F32)
                a2 = alloc(st, "a2", [128, n, 32], F32)
                tp = alloc(st, "tp", [128, n, 32], F32)
                ki = alloc(st, "ki", [128, n, 32], I32)
                P.dma("sp", posi[:], pos_dram, writes=[posi])
                CP("dve", posf[:], posi[:], [posi], [posf])
                for t in range(n):
                    TS("dve", ang[:, t, :], invf_t[:], posf[:, t:t + 1], None, ALU.mult, None, [invf_t, posf], [ang])
                HI = 6.28125
                LO = 2 * PI - 6.28125
                for (dst, shift) in ((sin_t, 0.0), (cos_t, PI / 2)):
                    TS("dve", a2[:], ang[:], shift, None, ALU.add, None, [ang], [a2])
                    TS("dve", tp[:], a2[:], 1.0 / (2 * PI), None, ALU.mult, None, [a2], [tp])
                    CP("dve", ki[:], tp[:], [tp], [ki])
                    CP("dve", tp[:], ki[:], [ki], [tp])
                    STT("dve", a2[:], tp[:], -HI, a2[:], ALU.mult, ALU.add, [tp, a2], [a2])
                    STT("dve", a2[:], tp[:], -LO, a2[:], ALU.mult, ALU.add, [tp, a2], [a2])
                    TS("dve", tp[:], a2[:], PI, None, ALU.is_gt, None, [a2], [tp])
                    STT("dve", a2[:], tp[:], -2 * PI, a2[:], ALU.mult, ALU.add, [tp, a2], [a2])
                    TS("dve", tp[:], a2[:], -PI, None, ALU.is_lt, None, [a2], [tp])
                    STT("dve", a2[:], tp[:], 2 * PI, a2[:], ALU.mult, ALU.add, [tp, a2], [a2])
                    ACTV(dst[:], a2[:], AF.Sin, [a2], [dst])
                P.barrier()

        def rope(src, dst, H, half, cos_ap, sin_ap, tmp, reads, writes):
            cb = cos_ap.to_broadcast([128, H, half])
            sb_ = sin_ap.to_broadcast([128, H, half])
            n = H * half
            ta = tmp[:, 0, 0:n].rearrange("p (h d) -> p h d", h=H)
            tb = tmp[:, 1, 0:n].rearrange("p (h d) -> p h d", h=H)
            tc_ = tmp[:, 2, 0:n].rearrange("p (h d) -> p h d", h=H)
            td = tmp[:, 3, 0:n].rearrange("p (h d) -> p h d", h=H)
            x1_ = src[:, :, 0:half]
            x2_ = src[:, :, half:2 * half]
            TTO("dve", ta, x1_, cb, ALU.mult, reads, [tmp])
            TTO("dve", tb, x2_, sb_, ALU.mult, reads, [tmp])
            TTO("dve", tc_, x2_, cb, ALU.mult, reads, [tmp])
            TTO("dve", td, x1_, sb_, ALU.mult, reads, [tmp])
            TTO("pool", dst[:, :, 0:half], ta, tb, ALU.subtract, [tmp], writes)
            TTO("pool", dst[:, :, half:2 * half], tc_, td, ALU.add, [tmp], writes)

        def rstd_of(src_ap, n, reads, junk, stat):
            ACTV(junk[:, 0:n], src_ap, AF.Square, reads, [junk, stat], accum_out=stat[:, 0:1])
            TS("dve", stat[:, 1:2], stat[:, 0:1], 1.0 / n, EPS, ALU.mult, ALU.add, [stat], [stat])
            TTO("pool", stat[:, 2:3], stat[:, 1:2], mhalf[:], ALU.pow, [stat, mhalf], [stat])
            return stat[:, 2:3]

        def load_w(dst, src, kc, g_t=None, eng_fold="dve"):
            for c in range(kc):
                P.dma("pool", dst[:, c, :], src[c * 128:(c + 1) * 128, :], writes=[dst])
            if g_t is not None:
                for c in range(kc):
                    TS(eng_fold, dst[:, c, :], dst[:, c, :], g_t[:, c:c + 1], None, ALU.mult, None, [dst, g_t], [dst])

        def load_g(st, name, src, k):
            t = alloc(st, name, [128, k], F32)
            P.dma("sp", t[:], src, writes=[t])
            return t

        def norm_transpose(x_ap, x_tt, dstf, dst_tt, junk, stat, xb, ptr_ring):
            rs = rstd_of(x_ap, D, [x_tt], junk, stat)
            TS("dve", xb[:], x_ap, rs, None, ALU.mult, None, [x_tt, stat], [xb])
            for g in range(2):
                pt = ptr_ring.next()
                for j in range(8):
                    c = g * 8 + j
                    TRN(pt[:, j, :], xb[:, c * 128:(c + 1) * 128], [xb], [pt])
                CP("act" if g == 0 else "dve", dstf(g), pt[:], [pt], [dst_tt])

        with ExitStack() as st:
            cosK = alloc(st, "cosK", [128, NT, 32], F32)
            sinK = alloc(st, "sinK", [128, NT, 32], F32)
            make_tables(posk, NT, cosK, sinK)
            dump("T", "cosK", cosK[:], [128, NT, 32], F32)
            dump("T", "sinK", sinK[:], [128, NT, 32], F32)
            checkpoint("T")
            wk = alloc(st, "wk", [128, 16, 2432], BF16)
            wkv = alloc(st, "wkv", [128, 2, 2048], BF16)
            gm = load_g(st, "g_mix", g_mix, 16)
            gk = load_g(st, "g_ckv", g_ckv, 2)
            load_w(wk, wk_side, 16, gm)
            load_w(wkv, w_kvb, 2, gk)
            xring = Ring(st, "xa", 1, [128, D], F32)
            junk = alloc(st, "junkA", [128, 256], BF16)
            statr = Ring(st, "statA", 6, [128, 4], F32)
            xbr = Ring(st, "xbA", 2, [128, D], BF16)
            hTr = Ring(st, "hTA", 2, [128, 16, 128], BF16)
            ptr_ring = Ring(st, "ptrA", 2, [128, 8, 128], BF16, psum=True)
            pm_ring = Ring(st, "pmA", 6, [128, 512], F32, psum=True)
            tmp_ring = Ring(st, "ropetmp", 2, [128, 4, 256], F32)
            ckvn_r = Ring(st, "ckvn", 2, [128, 256], BF16)
            ckvT_r = Ring(st, "ckvT", 2, [128, 2, 128], BF16)
            kr_tok_r = Ring(st, "kr_tok", 2, [128, 128], BF16)
            ki_tok_r = Ring(st, "ki_tok", 2, [128, 128], BF16)
            kd_tok_r = Ring(st, "kd_tok", 2, [128, 8, 128], BF16)
            kn_tok_r = Ring(st, "kn_tok", 2, [128, 8, 128], BF16)
            KnT_st_r = Ring(st, "KnT_st", 1, [128, 8, 512], BF16)
            KdT_st_r = Ring(st, "KdT_st", 1, [128, 8, 512], BF16)
            Vm_st_r = Ring(st, "Vm_st", 1, [128, 8, 4, 129], BF16)
            Vd_st_r = Ring(st, "Vd_st", 2, [128, 8, 4, 129], BF16)
            for rg in (Vm_st_r, Vd_st_r):
                for it in rg.items:
                    MEMSET("pool", it[:, :, :, 128:129], 1.0, [it])
            dump("W", "wk", wk[:, :, 0:512], [128, 16, 512], BF16)
            dump("W", "wkv", wkv[:, :, 0:512], [128, 2, 512], BF16)
            checkpoint("W")

            Vd_groups = {}
            ctx = {}

            def stage0(t):
                x_t = xring.next()
                P.dma("sp", x_t[:], x_all[t * 128:(t + 1) * 128, :], writes=[x_t])
                xb = xbr.next()
                stat = statr.next()
                rs = rstd_of(x_t[:], D, [x_t], xb, stat)
                TS("dve", xb[:], x_t[:], rs, None, ALU.mult, None, [x_t, stat], [xb])
                ctx[("xb", t)] = xb

            def stage1(t):
                tl = t % 4
                g4 = t // 4
                if tl == 0:
                    Vd_groups[g4] = Vd_st_r.next()
                Vd_st = Vd_groups[g4]
                xb = ctx.pop(("xb", t))
                hT = hTr.next()
                for g in range(2):
                    pt = ptr_ring.next()
                    for j in range(8):
                        c = g * 8 + j
                        TRN(pt[:, j, :], xb[:, c * 128:(c + 1) * 128], [xb], [pt])
                    CP("act" if g == 0 else "dve", hT[:, g * 8:(g + 1) * 8, :], pt[:], [pt], [hT])
                pms = []
                for g, (c0, n) in enumerate(((0, 384), (384, 512), (896, 512), (1408, 512), (1920, 512))):
                    pm = pm_ring.next()
                    for c in range(16):
                        MM(pm[:, 0:n], hT[:, c, :], wk[:, c, c0:c0 + n], c == 0, c == 15, [hT, wk], [pm])
                    pms.append(pm)
                p0 = pms[0]
                stat = statr.next()
                rs = rstd_of(p0[:, 0:256], 256, [p0], junk, stat)
                ckvn = ckvn_r.next()
                TS("dve", ckvn[:], p0[:, 0:256], rs, None, ALU.mult, None, [p0, stat], [ckvn])
                tmp = tmp_ring.next()
                kr_tok = kr_tok_r.next()
                rope(p0[:, 256:320].rearrange("p (h d) -> p h d", h=1), kr_tok[:, 0:64].rearrange("p (h d) -> p h d", h=1),
                     1, 32, cosK[:, t:t + 1, :], sinK[:, t:t + 1, :], tmp, [p0, cosK, sinK], [kr_tok])
                CP("pool", kr_tok[:, 64:128], kr_tok[:, 0:64], [kr_tok], [kr_tok])
                tmp = tmp_ring.next()
                ki_tok = ki_tok_r.next()
                rope(p0[:, 320:384].rearrange("p (h d) -> p h d", h=1), ki_tok[:, 0:64].rearrange("p (h d) -> p h d", h=1),
                     1, 8, cosK[:, t:t + 1, 0:32:4], sinK[:, t:t + 1, 0:32:4], tmp, [p0, cosK, sinK], [ki_tok])
                CP("act", ki_tok[:, 16:64], p0[:, 336:384], [p0], [ki_tok])
                CP("pool", ki_tok[:, 64:128], ki_tok[:, 0:64], [ki_tok], [ki_tok])
                kd_tok = kd_tok_r.next()
                for g in range(2):
                    pm = pms[1 + g]
                    tmp = tmp_ring.next()
                    src = pm[:].rearrange("p (h d) -> p h d", h=4)
                    dst = kd_tok[:, g * 4:(g + 1) * 4, :]
                    rope(src, dst, 4, 16, cosK[:, t:t + 1, 0:32:2], sinK[:, t:t + 1, 0:32:2], tmp, [pm, cosK, sinK], [kd_tok])
                    CP("act", dst[:, :, 32:128], src[:, :, 32:128], [pm], [kd_tok])
                for g in range(2):
                    pm = pms[3 + g]
                    CP("act" if g == 0 else "dve", Vd_st[:, g * 4:(g + 1) * 4, tl, 0:128],
                       pm[:].rearrange("p (h d) -> p h d", h=4), [pm], [Vd_st])
                ctx[t] = (ckvn, kr_tok, ki_tok, kd_tok, Vd_st)

            def stage2(t):
                tl = t % 4
                ckvn, kr_tok, ki_tok, kd_tok, Vd_st = ctx.pop(t)
                if tl == 0:
                    ctx["st"] = (KnT_st_r.next(), KdT_st_r.next(), Vm_st_r.next())
                KnT_st, KdT_st, Vm_st = ctx["st"]
                ckvT = ckvT_r.next()
                pt = ptr_ring.next()
                for c in range(2):
                    TRN(pt[:, c, :], ckvn[:, c * 128:(c + 1) * 128], [ckvn], [pt])
                CP("dve", ckvT[:], pt[:, 0:2, :], [pt], [ckvT])
                kn_tok = kn_tok_r.next()
                for g in range(4):
                    pm = pm_ring.next()
                    for c in range(2):
                        MM(pm[:], ckvT[:, c, :], wkv[:, c, g * 512:(g + 1) * 512], c == 0, c == 1, [ckvT, wkv], [pm])
                    v4 = pm[:].rearrange("p (h e d) -> p h e d", h=2, e=2)
                    CP("act", kn_tok[:, 2 * g:2 * g + 2, :], v4[:, :, 0, :], [pm], [kn_tok])
                    CP("dve", Vm_st[:, 2 * g:2 * g + 2, tl, 0:128], v4[:, :, 1, :], [pm], [Vm_st])
                for (src_tok, dst_st) in ((kd_tok, KdT_st), (kn_tok, KnT_st)):
                    pt = ptr_ring.next()
                    for h in range(8):
                        TRN(pt[:, h, :], src_tok[:, h, :], [src_tok], [pt])
                    CP("act" if src_tok is kd_tok else "dve", dst_st[:, :, tl * 128:(tl + 1) * 128], pt[:], [pt], [dst_st])
                pt = ptr_ring.next()
                TRN(pt[:, 0, :], kr_tok[:], [kr_tok], [pt])
                TRN(pt[:, 1, :], ki_tok[:], [ki_tok], [pt])
                CP("act", kropeT[:, t * 128:(t + 1) * 128], pt[:, 0, :], [pt], [kropeT])
                CP("dve", kidx_lo[0:64, t * 128:(t + 1) * 128], pt[0:64, 1, :], [pt], [kidx_lo])
                CP("dve", kidx_hi[64:128, t * 128:(t + 1) * 128], pt[64:128, 1, :], [pt], [kidx_hi])
                if tl == 3:
                    g4 = t // 4
                    P.dma("sp", KnT_scr[:, :, g4 * 512:(g4 + 1) * 512], KnT_st[:], reads=[KnT_st])
                    P.dma("sp", KdT_scr[:, :, g4 * 512:(g4 + 1) * 512], KdT_st[:], reads=[KdT_st])
                    P.dma("sp", Vm_scr[:, :, g4 * 4:(g4 + 1) * 4, :].rearrange("h p t c -> p h t c"), Vm_st[:],
                          reads=[Vm_st])
                    P.dma("sp", Vd_scr[:, :, g4 * 4:(g4 + 1) * 4, :].rearrange("h p t c -> p h t c"), Vd_st[:],
                          reads=[Vd_st])

            stage0(0)
            stage0(1)
            stage1(0)
            for t in range(NT):
                if t + 2 < NT:
                    stage0(t + 2)
                if t + 1 < NT:
                    stage1(t + 1)
                stage2(t)
            P.barrier()
            dump("A", "kropeT", kropeT[:], [128, S], BF16)
            dump("A", "KnT", KnT_scr, [128, 8, S], BF16)
            dump("A", "KdT", KdT_scr, [128, 8, S], BF16)
            dump("A", "Vm", Vm_scr, [8, 128, NT, 129], BF16)
            dump("A", "Vd", Vd_scr, [8, 128, NT, 129], BF16)
            if stop == "A":
                P.barrier()
            checkpoint("A")

        with ExitStack() as stB:
            QnT = alloc(stB, "QnT", [128, 8, NQ * 128], BF16)
            QrT_lo = alloc(stB, "QrT_lo", [128, 4, NQ * 128], BF16)
            QrT_hi = alloc(stB, "QrT_hi", [128, 4, NQ * 128], BF16)
            MEMSET("pool", QrT_lo[:], 0.0, [QrT_lo])
            MEMSET("pool", QrT_hi[:], 0.0, [QrT_hi])
            QdT = alloc(stB, "QdT", [128, 8, NQ * 128], BF16)
            with ExitStack() as stI:
                qiT = alloc(stI, "qiT", [128, 8, NQ * 128], BF16)
                wsc = alloc(stI, "wsc", [128, NQ, 16], F32)
                cosQ = alloc(stI, "cosQ", [128, NQ, 32], F32)
                sinQ = alloc(stI, "sinQ", [128, NQ, 32], F32)
                make_tables(posq, NQ, cosQ, sinQ)

                def qside(pass_id):
                    with ExitStack() as st:
                        ncol = 1552 if pass_id == 0 else 1024
                        col0 = 0 if pass_id == 0 else 1552
                        wq = alloc(st, "wq", [128, 16, ncol], BF16)
                        gm = load_g(st, "g_mix2", g_mix, 16)
                        for c in range(16):
                            P.dma("pool", wq[:, c, :], wq_side[c * 128:(c + 1) * 128, col0:col0 + ncol], writes=[wq])
                        for c in range(16):
                            TS("dve", wq[:, c, :], wq[:, c, :], gm[:, c:c + 1], None, ALU.mult, None, [wq, gm], [wq])
                        if pass_id == 0:
                            wqb = alloc(st, "wqb", [128, 4, 1536], BF16)
                            gq = load_g(st, "g_cq", g_cq, 4)
                            load_w(wqb, w_qb, 4, gq)
                        xring = Ring(st, "xb1", 2, [128, D], F32)
                        junk = alloc(st, "junkB", [128, 512], BF16)
                        statr = Ring(st, "statB", 6, [128, 4], F32)
                        xbr = Ring(st, "xbB", 2, [128, D], BF16)
                        hTr = Ring(st, "hTB", 2, [128, 16, 128], BF16)
                        ptr_ring = Ring(st, "ptrB", 2, [128, 8, 128], BF16, psum=True)
                        pm_ring = Ring(st, "pmB", 6, [128, 512], F32, psum=True)
                        tmp_ring = Ring(st, "ropetmpB", 2, [128, 4, 256], F32)
                        if pass_id == 0:
                            cqn_r = Ring(st, "cqn", 2, [128, 512], BF16)
                            cqT_r = Ring(st, "cqT", 2, [128, 4, 128], BF16)
                            qn_tok_r = Ring(st, "qn_tok", 2, [128, 8, 128], BF16)
                            qr_tok_r = Ring(st, "qr_tok", 2, [128, 8, 64], BF16)
                            qi_tok_r = Ring(st, "qi_tok", 2, [128, 16, 64], BF16)
                        else:
                            qd_tok_r = Ring(st, "qd_tok", 2, [128, 8, 128], BF16)
                        qctx = {}

                        def q0(s):
                            x_t = xring.next()
                            P.dma("sp", x_t[:], xq[s * 128:(s + 1) * 128, :], writes=[x_t])
                            xb = xbr.next()
                            stat = statr.next()
                            rs = rstd_of(x_t[:], D, [x_t], xb, stat)
                            TS("dve", xb[:], x_t[:], rs, None, ALU.mult, None, [x_t, stat], [xb])
                            qctx[s] = xb

                        q0(0)
                        for s in range(NQ):
                            qs = slice(s * 128, (s + 1) * 128)
                            if s + 1 < NQ:
                                q0(s + 1)
                            xb = qctx.pop(s)
                            hT = hTr.next()
                            for g in range(2):
                                pt = ptr_ring.next()
                                for j in range(8):
                                    c = g * 8 + j
                                    TRN(pt[:, j, :], xb[:, c * 128:(c + 1) * 128], [xb], [pt])
                                CP("act" if g == 0 else "dve", hT[:, g * 8:(g + 1) * 8, :], pt[:], [pt], [hT])
                            if pass_id == 0:
                                pms = []
                                for g, (c0, n) in enumerate(((0, 512), (512, 512), (1024, 512), (1536, 16))):
                                    pm = pm_ring.next()
                                    for c in range(16):
                                        MM(pm[:, 0:n], hT[:, c, :], wq[:, c, c0:c0 + n], c == 0, c == 15, [hT, wq], [pm])
                                    pms.append(pm)
                                TS("dve", wsc[:, s, :], pms[3][:, 0:16], 1.0 / 32.0, None, ALU.mult, None, [pms[3]], [wsc])
                                stat = statr.next()
                                rs = rstd_of(pms[0][:], 512, [pms[0]], junk, stat)
                                cqn = cqn_r.next()
                                TS("dve", cqn[:], pms[0][:], rs, None, ALU.mult, None, [pms[0], stat], [cqn])
                                qi_tok = qi_tok_r.next()
                                for g in range(2):
                                    pm = pms[1 + g]
                                    tmp = tmp_ring.next()
                                    src = pm[:].rearrange("p (h d) -> p h d", h=8)
                                    dst = qi_tok[:, g * 8:(g + 1) * 8, :]
                                    rope(src, dst, 8, 8, cosQ[:, s:s + 1, 0:32:4], sinQ[:, s:s + 1, 0:32:4], tmp,
                                         [pm, cosQ, sinQ], [qi_tok])
                                    CP("act", dst[:, :, 16:64], src[:, :, 16:64], [pm], [qi_tok])
                                cqT = cqT_r.next()
                                pt = ptr_ring.next()
                                for c in range(4):
                                    TRN(pt[:, c, :], cqn[:, c * 128:(c + 1) * 128], [cqn], [pt])
                                CP("dve", cqT[:], pt[:, 0:4, :], [pt], [cqT])
                                qn_tok = qn_tok_r.next()
                                qr_tok = qr_tok_r.next()
                                for g in range(3):
                                    pm = pm_ring.next()
                                    for c in range(4):
                                        MM(pm[:], cqT[:, c, :], wqb[:, c, g * 512:(g + 1) * 512], c == 0, c == 3, [cqT, wqb], [pm])
                                    if g < 2:
                                        CP("act", qn_tok[:, g * 4:(g + 1) * 4, :], pm[:].rearrange("p (h d) -> p h d", h=4),
                                           [pm], [qn_tok])
                                    else:
                                        tmp = tmp_ring.next()
                                        rope(pm[:].rearrange("p (h d) -> p h d", h=8), qr_tok[:], 8, 32,
                                             cosQ[:, s:s + 1, :], sinQ[:, s:s + 1, :], tmp, [pm, cosQ, sinQ], [qr_tok])
                                pt = ptr_ring.next()
                                for h in range(8):
                                    TRN(pt[:, h, :], qn_tok[:, h, :], [qn_tok], [pt])
                                CP("act", QnT[:, :, qs], pt[:], [pt], [QnT])
                                pt = ptr_ring.next()
                                qr2 = qr_tok[:].rearrange("p (a b) d -> p a (b d)", b=2)
                                for h2 in range(4):
                                    TRN(pt[:, h2, :], qr2[:, h2, :], [qr_tok], [pt])
                                CP("dve", QrT_lo[0:64, :, qs], pt[0:64, 0:4, :], [pt], [QrT_lo])
                                CP("dve", QrT_hi[64:128, :, qs], pt[64:128, 0:4, :], [pt], [QrT_hi])
                                pt = ptr_ring.next()
                                qi2 = qi_tok[:].rearrange("p (a b) d -> p a (b d)", b=2)
                                for h2 in range(8):
                                    TRN(pt[:, h2, :], qi2[:, h2, :], [qi_tok], [pt])
                                CP("act", qiT[:, :, qs], pt[:], [pt], [qiT])
                            else:
                                pms = []
                                for g in range(2):
                                    pm = pm_ring.next()
                                    for c in range(16):
                                        MM(pm[:], hT[:, c, :], wq[:, c, g * 512:(g + 1) * 512], c == 0, c == 15, [hT, wq], [pm])
                                    pms.append(pm)
                                qd_tok = qd_tok_r.next()
                                for g in range(2):
                                    pm = pms[g]
                                    tmp = tmp_ring.next()
                                    src = pm[:].rearrange("p (h d) -> p h d", h=4)
                                    dst = qd_tok[:, g * 4:(g + 1) * 4, :]
                                    rope(src, dst, 4, 16, cosQ[:, s:s + 1, 0:32:2], sinQ[:, s:s + 1, 0:32:2], tmp,
                                         [pm, cosQ, sinQ], [qd_tok])
                                    CP("act", dst[:, :, 32:128], src[:, :, 32:128], [pm], [qd_tok])
                                pt = ptr_ring.next()
                                for h in range(8):
                                    TRN(pt[:, h, :], qd_tok[:, h, :], [qd_tok], [pt])
                                CP("dve", QdT[:, :, qs], pt[:], [pt], [QdT])
                        P.barrier()

                qside(0)
                qside(1)
                dump("B1", "QnT", QnT[:], [128, 8, NQ * 128], BF16)
                dump("B1", "QdT", QdT[:], [128, 8, NQ * 128], BF16)
                dump("B1", "qiT", qiT[:], [128, 8, NQ * 128], BF16)
                dump("B1", "wsc", wsc[:], [128, NQ, 16], F32)
                if stop == "B1":
                    P.barrier()
                checkpoint("B1")

                with ExitStack() as st:
                    zb_ring = Ring(st, "zb", 4, [128, 512], F32)
                    pz_ring = Ring(st, "pz", 3, [128, 512], F32, psum=True)
                    psc_ring = Ring(st, "psc", 2, [128, 512], F32, psum=True)
                    ptr_ring = Ring(st, "ptrI", 2, [128, 8, 128], BF16, psum=True)
                    r_ring = Ring(st, "relu", 4, [128, 512], BF16)
                    dg_ring = Ring(st, "dg", 3, [128, 16, 128], BF16)
                    sc_ring = Ring(st, "score", 4, [128, S], F32)
                    junkI_d = alloc(st, "junkI", [128, S], mybir.dt.uint8)
                    junkI_p = alloc(st, "junkIp", [128, S], mybir.dt.uint8)
                    mask_ring = Ring(st, "mask", 1, [128, S], BF16)
                    mT_ring = Ring(st, "mTst", 1, [128, NT, 128], BF16)
                    bs_ring = Ring(st, "bis", 4, [128, 8 + NBIS], F32)
                    blk_of = [0]
                    for s_ in range(NQ):
                        blk_of.append(blk_of[-1] + 4 * (s_ + 1))

                    def front(s):
                        E = 4 * (s + 1)
                        n = E * 128
                        qs = slice(s * 128, (s + 1) * 128)
                        zb_t = zb_ring.next()
                        P.dma("sp", zb_t[:], zb[:, s, :], writes=[zb_t])
                        dg = dg_ring.next()
                        for h in range(16):
                            TS("pool", dg[:, h, :], identb[:], wsc[:, s, h:h + 1], None, ALU.mult, None, [identb, wsc], [dg])
                        score = sc_ring.next()
                        for kb in range(s + 1):
                            ks = slice(kb * 512, (kb + 1) * 512)
                            psc = psc_ring.next()
                            rprev = None
                            for h in range(16):
                                p0_ = (h % 2) * 64
                                pz = pz_ring.next()
                                kix = kidx_lo if h % 2 == 0 else kidx_hi
                                MM(pz[:], qiT[:, h // 2, qs], kix[:, ks], True, True, [qiT, kix], [pz])
                                rb = r_ring.next()
                                ACTV(rb[:], pz[:], AF.Relu, [pz], [rb])
                                if rprev is not None:
                                    MM(psc[:], dg[:, h - 1, :], rprev[:], h - 1 == 0, False, [dg, rprev], [psc])
                                rprev = rb
                            MM(psc[:], dg[:, 15, :], rprev[:], False, True, [dg, rprev], [psc])
                            CP("act", score[:, ks], psc[:], [psc], [score])
                        return score, zb_t

                    def bis(s, score, zb_t, junkI, bs):
                        n = 4 * (s + 1) * 128
                        P.op("dve", lambda e, o_=bs[:, 0:1], i_=score[:, 0:n]: e.tensor_reduce(out=o_, in_=i_, axis=AX.X, op=ALU.max),
                             [score], [bs])
                        yield
                        P.op("dve", lambda e, o_=bs[:, 1:2], i_=score[:, 0:n]: e.tensor_reduce(out=o_, in_=i_, axis=AX.X, op=ALU.min),
                             [score], [bs])
                        yield
                        TTO("pool", score[:, n - 512:n], score[:, n - 512:n], zb_t[:], ALU.add, [score, zb_t, bs], [score])
                        TTO("dve", bs[:, 2:3], bs[:, 0:1], bs[:, 1:2], ALU.subtract, [bs], [bs])
                        yield
                        TS("dve", bs[:, 8:8 + NBIS], pow2_t[:], bs[:, 2:3], None, ALU.mult, None, [pow2_t, bs], [bs])
                        yield
                        mid = bs[:, 3:4]
                        TS("dve", mid, bs[:, 1:2], bs[:, 8:9], None, ALU.add, None, [bs], [bs])
                        yield
                        for i in range(NBIS):
                            Hi = bs[:, 8 + i:9 + i]
                            Hn = bs[:, 9 + i:10 + i] if i + 1 < NBIS else Hi
                            TS("dve", junkI[:, 0:n], score[:, 0:n], mid, 0.0, ALU.is_ge, ALU.add, [score, bs], [junkI, bs],
                               accum_out=bs[:, 4:5])
                            yield
                            STT("dve", bs[:, 5:6], bs[:, 4:5], 255.5, Hi, ALU.is_ge, ALU.mult, [bs], [bs])
                            yield
                            STT("dve", mid, mid, Hn, bs[:, 5:6], ALU.subtract, ALU.add, [bs], [bs])
                            yield

                    def back(s, score, bs):
                        E = 4 * (s + 1)
                        n = E * 128
                        mask = mask_ring.next()
                        TS("dve", mask[:, 0:n], score[:, 0:n], bs[:, 3:4], None, ALU.is_ge, None, [score, bs], [mask])
                        mT = mT_ring.next()
                        for g in range((E + 7) // 8):
                            pt = ptr_ring.next()
                            nb_ = min(8, E - g * 8)
                            for j in range(nb_):
                                kt = g * 8 + j
                                TRN(pt[:, j, :], mask[:, kt * 128:(kt + 1) * 128], [mask], [pt])
                            CP("act", mT[:, g * 8:g * 8 + nb_, :], pt[:, 0:nb_, :], [pt], [mT])
                        P.dma("sp", maskT_scr[:, blk_of[s]:blk_of[s] + E, :], mT[:, 0:E, :], reads=[mT])

                    fronts = {0: front(0), 1: front(1)}
                    for a in range(0, NQ, 2):
                        b = a + 1
                        sa, za = fronts.pop(a)
                        sb_, zb2 = fronts.pop(b)
                        if a + 2 < NQ:
                            fronts[a + 2] = front(a + 2)
                            fronts[a + 3] = front(a + 3)
                        bsa, bsb = bs_ring.next(), bs_ring.next()
                        ga = bis(a, sa, za, junkI_d, bsa)
                        gb = bis(b, sb_, zb2, junkI_p, bsb)
                        live = [ga, gb]
                        while live:
                            for g_ in list(live):
                                try:
                                    next(g_)
                                except StopIteration:
                                    live.remove(g_)
                        back(a, sa, bsa)
                        back(b, sb_, bsb)
                    score, bs = sb_, bsb
                    P.barrier()
                    dump("B2", "maskT", maskT_scr, [128, 144, 128], BF16)
                    dump("B2", "score", score[:], [128, S], F32)
                    dump("B2", "bs", bs[:], [128, 8 + NBIS], F32)
                    if stop == "B2":
                        P.barrier()
                    checkpoint("B2")

            with ExitStack() as st:
                maskT = alloc(st, "maskT", [128, 144, 128], BF16)
                zmT_t = alloc(st, "zmT", [128, NQ, 512], BF16)
                O_all = alloc(st, "O_all", [128, NQ, D], BF16)
                P.dma("sp", maskT[:], maskT_scr, writes=[maskT])
                P.dma("pool", zmT_t[:], zmT, writes=[zmT_t])
                KT_ring = Ring(st, "KT", 2, [128, S], BF16)
                V_ring = Ring(st, "V", 2, [128, NT, 129], BF16)
                pS_ring = Ring(st, "pS", 5, [128, 512], F32, psum=True)
                pO_ring = Ring(st, "pO", 2, [128, 512], F32, psum=True)
                PT_ring = Ring(st, "PT", 6, [128, 512], BF16)
                rs_ring = Ring(st, "rsum", 4, [128, 1], F32)
                blk_off = [0]
                for s in range(NQ):
                    blk_off.append(blk_off[-1] + 4 * (s + 1))
                groups = [(s, kb) for s in range(NQ) for kb in range(s + 1)]
                for hh in range(16):
                    mla = hh < 8
                    h = hh % 8
                    KT = KT_ring.next()
                    V = V_ring.next()
                    if mla:
                        P.dma("sp", KT[:], KnT_scr[:, h, :], writes=[KT])
                        P.dma("sp", V[:], Vm_scr[h], writes=[V])
                        scale = 192.0 ** -0.5
                    else:
                        P.dma("sp", KT[:], KdT_scr[:, h, :], writes=[KT])
                        P.dma("sp", V[:], Vd_scr[h], writes=[V])
                        scale = 128.0 ** -0.5
                    p0_ = (h % 2) * 64

                    def qk(s, kb):
                        qs = slice(s * 128, (s + 1) * 128)
                        pS = pS_ring.next()
                        for j in range(4):
                            kt = 4 * kb + j
                            ks = slice(kt * 128, (kt + 1) * 128)
                            if mla:
                                MM(pS[:, j * 128:(j + 1) * 128], KT[:, ks], QnT[:, h, qs], True, False, [KT, QnT], [pS])
                                QrP = QrT_lo if h % 2 == 0 else QrT_hi
                                MM(pS[:, j * 128:(j + 1) * 128], kropeT[:, ks], QrP[:, h // 2, qs],
                                   False, True, [kropeT, QrP], [pS])
                            else:
                                MM(pS[:, j * 128:(j + 1) * 128], KT[:, ks], QdT[:, h, qs], True, True, [KT, QdT], [pS])
                        return pS

                    LOOK = 3
                    pend = [qk(*groups[i]) for i in range(LOOK)]
                    pO = None
                    for gi, (s, kb) in enumerate(groups):
                        pS = pend.pop(0)
                        if gi + LOOK < len(groups):
                            pend.append(qk(*groups[gi + LOOK]))
                        PT = PT_ring.next()
                        ACTV(PT[:], pS[:], AF.Exp, [pS], [PT], scale=scale)
                        if mla:
                            if kb == s:
                                TTO("dve", PT[:], PT[:], zmT_t[:, s, :], ALU.mult, [PT, zmT_t], [PT])
                        else:
                            b0 = blk_off[s] + 4 * kb
                            TTO("dve" if gi % 4 != 3 else "pool", PT[:], PT[:],
                                maskT[:, b0:b0 + 4, :].rearrange("p a b -> p (a b)"), ALU.mult, [PT, maskT], [PT])
                        if kb == 0:
                            pO = pO_ring.next()
                        E = 4 * (s + 1)
                        for j in range(4):
                            kt = 4 * kb + j
                            MM(pO[:, 0:129], PT[:, j * 128:(j + 1) * 128], V[:, kt, :], kt == 0, kt == E - 1, [PT, V], [pO])
                        if kb == s:
                            rsum = rs_ring.next()
                            P.op("dve", lambda e, o_=rsum[:], i_=pO[:, 128:129]: e.reciprocal(out=o_, in_=i_), [pO], [rsum])
                            col = hh * 128
                            ACTV(O_all[:, s, col:col + 128], pO[:, 0:128], AF.Copy, [pO, rsum], [O_all], scale=rsum[:])
                P.dma("sp", O_scr.rearrange("(s p) d -> p s d", p=128), O_all[:], reads=[O_all])
                P.barrier()
                dump("B3", "O", O_scr, [NQ * 128, D], BF16)
                if stop == "B3":
                    P.barrier()
                checkpoint("B3")

        stK.close()
        with ExitStack() as stR:
            x1 = alloc(stR, "x1", [128, NQ, D], F32)
            with ExitStack() as st:
                wo = alloc(st, "wo", [128, 16, D], BF16)
                load_w(wo, w_out, 16)
                xring = Ring(st, "xc", 2, [128, D], F32)
                Or = Ring(st, "Oc", 2, [128, D], BF16)
                OT_r = Ring(st, "OT", 2, [128, 16, 128], BF16)
                ptr_ring = Ring(st, "ptrC", 2, [128, 8, 128], BF16, psum=True)
                pm_ring = Ring(st, "pmC", 4, [128, 512], F32, psum=True)
                for s in range(NQ):
                    qs = slice(s * 128, (s + 1) * 128)
                    x_t = xring.next()
                    P.dma("sp", x_t[:], xq[qs, :], writes=[x_t])
                    O_t = Or.next()
                    P.dma("sp", O_t[:], O_scr[qs, :], writes=[O_t])
                    OT = OT_r.next()
                    for g in range(2):
                        pt = ptr_ring.next()
                        for j in range(8):
                            c = g * 8 + j
                            TRN(pt[:, j, :], O_t[:, c * 128:(c + 1) * 128], [O_t], [pt])
                        CP("act" if g == 0 else "dve", OT[:, g * 8:(g + 1) * 8, :], pt[:], [pt], [OT])
                    for g in range(4):
                        pm = pm_ring.next()
                        for c in range(16):
                            MM(pm[:], OT[:, c, :], wo[:, c, g * 512:(g + 1) * 512], c == 0, c == 15, [OT, wo], [pm])
                        TTO("dve", x1[:, s, g * 512:(g + 1) * 512], pm[:], x_t[:, g * 512:(g + 1) * 512], ALU.add,
                            [pm, x_t], [x1])
                P.barrier()
                dump("C", "x1C", x1[:], [128, NQ, D], F32)
                if stop == "C":
                    P.barrier()
                checkpoint("C")

            with ExitStack() as st:
                wq_c = alloc(st, "wqc", [128, 16, 512], BF16)
                wk_c = alloc(st, "wkc", [128, 16, 512], BF16)
                wv_c = alloc(st, "wvc", [128, 16, 512], BF16)
                wo_c = alloc(st, "woc", [128, 4, D], BF16)
                gc = load_g(st, "g_cross", g_cross, 16)
                gme = load_g(st, "g_mem", g_mem, 16)
                load_w(wk_c, wkc, 16, gme)
                load_w(wv_c, wvc, 16, gme)
                load_w(wq_c, wqc, 16, gc)
                load_w(wo_c, woc, 4)
                memT = alloc(st, "memT", [128, 16, 256], BF16)
                KcT = alloc(st, "KcT", [128, 4, 256], BF16)
                Vc = alloc(st, "Vc", [128, 2, 4, 129], BF16)
                MEMSET("pool", Vc[:, :, :, 128:129], 1.0, [Vc])
                xring = Ring(st, "xm", 2, [128, D], F32)
                junk = alloc(st, "junkD", [128, D], BF16)
                statr = Ring(st, "statD", 4, [128, 4], F32)
                xbr = Ring(st, "xbD", 1, [128, D], BF16)
                hTr = Ring(st, "hTD", 2, [128, 16, 128], BF16)
                ptr_ring = Ring(st, "ptrD", 2, [128, 8, 128], BF16, psum=True)
                pm_ring = Ring(st, "pmD", 2, [128, 512], F32, psum=True)
                pS_ring = Ring(st, "pSD", 2, [128, 512], F32, psum=True)
                pO_ring = Ring(st, "pOD", 2, [128, 512], F32, psum=True)
                QcT_r = Ring(st, "QcT", 2, [128, 4, 128], BF16)
                PT_ring = Ring(st, "PTD", 4, [128, 512], BF16)
                rs_ring = Ring(st, "rsD", 2, [128, 4], F32)
                Oc_r = Ring(st, "Oc_tok", 2, [128, 512], BF16)
                OcT_r = Ring(st, "OcT", 2, [128, 4, 128], BF16)
                for mt in range(2):
                    x_t = xring.next()
                    P.dma("sp", x_t[:], mem[mt * 128:(mt + 1) * 128, :], writes=[x_t])
                    norm_transpose(x_t[:], x_t, lambda g, mt=mt: memT[:, g * 8:(g + 1) * 8, mt * 128:(mt + 1) * 128], memT,
                                   junk, statr.next(), xbr.next(), ptr_ring)
                for h in range(4):
                    pm = pm_ring.next()
                    for c in range(16):
                        MM(pm[:, 0:256], wk_c[:, c, h * 128:(h + 1) * 128], memT[:, c, :], c == 0, c == 15, [wk_c, memT], [pm])
                    CP("act", KcT[:, h, :], pm[:, 0:256], [pm], [KcT])
                for mt in range(2):
                    pm = pm_ring.next()
                    for c in range(16):
                        MM(pm[:], memT[:, c, mt * 128:(mt + 1) * 128], wv_c[:, c, :], c == 0, c == 15, [memT, wv_c], [pm])
                    CP("dve", Vc[:, mt, :, 0:128], pm[:].rearrange("p (h d) -> p h d", h=4), [pm], [Vc])
                cscale = 128.0 ** -0.5
                dctx = {}
                x1d = [Buf("x1d%d" % s_) for s_ in range(NQ)]

                def d1(s):
                        hT = hTr.next()
                        norm_transpose(x1[:, s, :], x1d[s], lambda g, hT=hT: hT[:, g * 8:(g + 1) * 8, :], hT, junk, statr.next(),
                                       xbr.next(), ptr_ring)
                        pm = pm_ring.next()
                        for h in range(4):
                            for c in range(16):
                                MM(pm[:, h * 128:(h + 1) * 128], wq_c[:, c, h * 128:(h + 1) * 128], hT[:, c, :], c == 0, c == 15,
                                   [wq_c, hT], [pm])
                        QcT = QcT_r.next()
                        CP("act", QcT[:], pm[:].rearrange("p (h d) -> p h d", h=4), [pm], [QcT])
                        dctx[s] = QcT

                def d2(s):
                        QcT = dctx.pop(s)
                        PTs = []
                        for hp in range(2):
                            pS = pS_ring.next()
                            for hl in range(2):
                                h = hp * 2 + hl
                                for mt in range(2):
                                    cs = (hl * 2 + mt) * 128
                                    MM(pS[:, cs:cs + 128], KcT[:, h, mt * 128:(mt + 1) * 128], QcT[:, h, :], True, True, [KcT, QcT], [pS])
                            PT = PT_ring.next()
                            ACTV(PT[:], pS[:], AF.Exp, [pS], [PT], scale=cscale)
                            PTs.append(PT)
                        rsum = rs_ring.next()
                        Oc = Oc_r.next()
                        for hp in range(2):
                            pO = pO_ring.next()
                            PT = PTs[hp]
                            for hl in range(2):
                                h = hp * 2 + hl
                                for mt in range(2):
                                    cs = (hl * 2 + mt) * 128
                                    MM(pO[:, hl * 256:hl * 256 + 129], PT[:, cs:cs + 128], Vc[:, mt, h, :], mt == 0, mt == 1, [PT, Vc], [pO])
                            for hl in range(2):
                                h = hp * 2 + hl
                                P.op("dve", lambda e, o_=rsum[:, h:h + 1], i_=pO[:, hl * 256 + 128:hl * 256 + 129]: e.reciprocal(out=o_, in_=i_), [pO], [rsum])
                                ACTV(Oc[:, h * 128:(h + 1) * 128], pO[:, hl * 256:hl * 256 + 128], AF.Copy, [pO, rsum], [Oc], scale=rsum[:, h:h + 1])
                        OcT = OcT_r.next()
                        pt = ptr_ring.next()
                        for c in range(4):
                            TRN(pt[:, c, :], Oc[:, c * 128:(c + 1) * 128], [Oc], [pt])
                        CP("dve", OcT[:], pt[:, 0:4, :], [pt], [OcT])
                        for g in range(4):
                            pm = pm_ring.next()
                            for c in range(4):
                                MM(pm[:], OcT[:, c, :], wo_c[:, c, g * 512:(g + 1) * 512], c == 0, c == 3, [OcT, wo_c], [pm])
                            TTO("dve", x1[:, s, g * 512:(g + 1) * 512], pm[:], x1[:, s, g * 512:(g + 1) * 512], ALU.add,
                                [pm, x1d[s]], [x1d[s]])

                d1(0)
                for s in range(NQ):
                    if s + 1 < NQ:
                        d1(s + 1)
                    d2(s)
                P.barrier()
                dump("D", "x1D", x1[:], [128, NQ, D], F32)
                if stop == "D":
                    P.barrier()
                checkpoint("D")

            with ExitStack() as st:
                hmT = alloc(st, "hmT", [128, 16, NQ * 128], BF16)
                with ExitStack() as st2:
                    junk = alloc(st2, "junkE", [128, D], BF16)
                    statr = Ring(st2, "statE", 4, [128, 4], F32)
                    xbr = Ring(st2, "xbE", 2, [128, D], BF16)
                    ptr_ring = Ring(st2, "ptrE", 2, [128, 8, 128], BF16, psum=True)
                    for s in range(NQ):
                        norm_transpose(x1[:, s, :], x1, lambda g, s=s: hmT[:, g * 8:(g + 1) * 8, s * 128:(s + 1) * 128], hmT,
                                       junk, statr.next(), xbr.next(), ptr_ring)
                    P.barrier()
                with ExitStack() as st2:
                    gml = load_g(st2, "g_mlp", g_mlp, 16)
                    wu_ring = Ring(st2, "wu", 2, [128, 16, 512], BF16)
                    wd_ring = Ring(st2, "wd", 2, [128, 4, D], BF16)
                    aT_ring = Ring(st2, "aT", 2, [128, 4, NQ * 128], BF16)
                    rt_ring = Ring(st2, "rt", 2, [128, 512], F32)
                    et_ring = Ring(st2, "et", 2, [128, 512], F32)
                    x1s = [[Buf("x1_%d_%d" % (s_, g_)) for g_ in range(4)] for s_ in range(NQ)]
                    pu_ring = Ring(st2, "pu", 3, [128, 512], F32, psum=True)
                    pd_ring = Ring(st2, "pd", 4, [128, 512], F32, psum=True)
                    NCH = DFF // 512

                    def load_chunk(ch):
                        wu = wu_ring.next()
                        wd = wd_ring.next()
                        P.dma("pool", wu[:], w_up[:, ch * 512:(ch + 1) * 512].rearrange("(c p) n -> p c n", p=128), writes=[wu])
                        P.dma("pool", wd[:], w_down[ch * 512:(ch + 1) * 512, :].rearrange("(f p) n -> p f n", p=128), writes=[wd])
                        for c in range(16):
                            TS("dve", wu[:, c, :], wu[:, c, :], gml[:, c:c + 1], None, ALU.mult, None, [wu, gml], [wu])
                        return wu, wd

                    nxt = load_chunk(0)
                    for ch in range(NCH):
                        wu, wd = nxt
                        if ch + 1 < NCH:
                            nxt = load_chunk(ch + 1)
                        aT = aT_ring.next()
                        for fb in range(4):
                            for half in range(2):
                                pu = pu_ring.next()
                                ts_ = slice(half * 512, (half + 1) * 512)
                                for c in range(16):
                                    MM(pu[:], wu[:, c, fb * 128:(fb + 1) * 128], hmT[:, c, ts_], c == 0, c == 15, [wu, hmT], [pu])
                                rt = rt_ring.next()
                                ACTV(rt[:], pu[:], AF.Relu, [pu], [rt])
                                TTO("pool", aT[:, fb, ts_], rt[:], rt[:], ALU.mult, [rt], [aT])
                        for s in range(NQ):
                            for g in range(4):
                                pd = pd_ring.next()
                                for fb in range(4):
                                    MM(pd[:], aT[:, fb, s * 128:(s + 1) * 128], wd[:, fb, g * 512:(g + 1) * 512], fb == 0, fb == 3,
                                       [aT, wd], [pd])
                                xs_ = x1[:, s, g * 512:(g + 1) * 512]
                                if g % 2 == 0:
                                    TTO("dve", xs_, pd[:], xs_, ALU.add, [pd, x1s[s][g]], [x1s[s][g]])
                                else:
                                    et = et_ring.next()
                                    CP("act", et[:], pd[:], [pd], [et])
                                    TTO("pool", xs_, et[:], xs_, ALU.add, [et, x1s[s][g]], [x1s[s][g]])
                    P.barrier()
            with ExitStack() as st:
                gf = alloc(st, "g_fin", [128, D], F32)
                P.dma("sp", gf[:], g_fin, writes=[gf])
                junk = alloc(st, "junkF", [128, D], BF16)
                statr = Ring(st, "statF", 4, [128, 4], F32)
                y_ring = Ring(st, "y", 2, [128, D], F32)
                outs = []
                for s in range(NQ):
                    stat = statr.next()
                    rs = rstd_of(x1[:, s, :], D, [x1], junk, stat)
                    y = y_ring.next()
                    STT("dve", y[:], x1[:, s, :], rs, gf[:], ALU.mult, ALU.mult, [x1, stat, gf], [y])
                    outs.append(P.dma("sp", out[s * 128:(s + 1) * 128, :], y[:], reads=[y]))
        final = [o_ for o_ in outs if o_ is not None]
      except _Stop:
        final = []
      P.emit(nc, final_waits=list(final) + dump_ops)
    return nc


def _qtiles(r):
    return [r, 7 - r, 8 + r, 15 - r, 16 + r, 23 - r, 24 + r, 31 - r]


_NC_CACHE = {}
_PREP_ONLY = [False]


def kernel(x, mem, positions, g_mix, w_in, g_cq, g_ckv, w_qb, w_kvb, w_out, g_cross, g_mem, w_q_cross, w_k_cross,
           w_v_cross, w_o_cross, g_mlp, w_up, w_down, g_final):
    f32 = np.float32
    x = np.asarray(x, f32)
    mem = np.asarray(mem, f32)
    positions = np.asarray(positions, np.int32)
    w_in0 = np.asarray(w_in, f32)[0]
    o = np.cumsum([0, 512, 256, 64, 1024, 1024, 1024, 1024, 64, 16])
    c_q, c_kv, k_rope, q_d, k_d, v_d, q_idx, k_idx, w_idx = [w_in0[:, o[i]:o[i + 1]] for i in range(9)]
    wk_side = np.ascontiguousarray(np.concatenate([c_kv, k_rope, k_idx, k_d, v_d], axis=1))
    wq_side = np.ascontiguousarray(np.concatenate([c_q, q_idx, w_idx, q_d], axis=1))
    wqb0 = np.asarray(w_qb, f32)[0].reshape(512, 8, 192)
    wqb_r = np.ascontiguousarray(np.concatenate([wqb0[:, :, :128].reshape(512, 1024), wqb0[:, :, 128:].reshape(512, 512)], axis=1))

    def gl(g, k):
        return np.ascontiguousarray(np.asarray(g, f32).reshape(k, 128).T)

    common = {
        "invf": np.ascontiguousarray(np.broadcast_to((THETA ** (-np.arange(32, dtype=np.float64) / 32)).astype(f32), (128, 32))),
        "pow2": np.ascontiguousarray(np.broadcast_to((2.0 ** -(np.arange(NBIS) + 1.0)).astype(f32), (128, NBIS))),
        "ident": np.eye(128, dtype=f32),
        "wk_side": wk_side, "wq_side": wq_side,
        "g_mix": gl(np.asarray(g_mix)[0], 16),
        "w_qb": wqb_r, "g_cq": gl(np.asarray(g_cq)[0], 4),
        "w_kvb": np.ascontiguousarray(np.asarray(w_kvb, f32)[0]), "g_ckv": gl(np.asarray(g_ckv)[0], 2),
        "w_out": np.ascontiguousarray(np.asarray(w_out, f32)[0]),
        "g_cross": gl(np.asarray(g_cross)[0], 16), "g_mem": gl(np.asarray(g_mem)[0], 16),
        "wqc": np.ascontiguousarray(np.asarray(w_q_cross, f32)[0]),
        "wkc": np.ascontiguousarray(np.asarray(w_k_cross, f32)[0]),
        "wvc": np.ascontiguousarray(np.asarray(w_v_cross, f32)[0]),
        "woc": np.ascontiguousarray(np.asarray(w_o_cross, f32)[0]),
        "g_mlp": gl(np.asarray(g_mlp)[0], 16),
        "w_up": np.ascontiguousarray(np.asarray(w_up, f32)[0]),
        "w_down": np.ascontiguousarray(np.asarray(w_down, f32)[0]),
        "g_fin": np.ascontiguousarray(np.broadcast_to(np.asarray(g_final, f32).reshape(1, D), (128, D))),
    }
    in_maps = []
    for c in range(8):
        b, r = c // 4, c % 4
        qt = _qtiles(r)
        xq = np.ascontiguousarray(np.concatenate([x[b, t * 128:(t + 1) * 128] for t in qt], axis=0))
        posk = np.ascontiguousarray(positions[b].reshape(NT, 128).T)
        posq = np.ascontiguousarray(np.stack([positions[b, t * 128:(t + 1) * 128] for t in qt], axis=1))
        zmT = np.zeros((128, NQ, 512), f32)
        zbm = np.zeros((128, NQ, 512), f32)
        for s, t in enumerate(qt):
            qpos = t * 128 + np.arange(128)
            for j in range(4):
                kpos = (4 * s + j) * 128 + np.arange(128)
                allowed = (kpos[:, None] // 64) <= (qpos[None, :] // 64)
                zmT[:, s, j * 128:(j + 1) * 128] = allowed
                zbm[:, s, j * 128:(j + 1) * 128] = np.where(allowed.T, 0.0, -BIG)
        m = dict(common)
        m.update({"x_all": np.ascontiguousarray(x[b]), "xq": xq, "posk": posk, "posq": posq,
                  "mem": np.ascontiguousarray(mem[b]), "zmT": zmT, "zb": zbm})
        in_maps.append(m)
    if _PREP_ONLY[0]:
        return in_maps
    if "nc" not in _NC_CACHE:
        _NC_CACHE["nc"] = build_program()
    res = run_bass_kernel_spmd(_NC_CACHE["nc"], in_maps, core_ids=list(range(8)))
    outp = np.zeros((2, S, D), f32)
    for c in range(8):
        b, r = c // 4, c % 4
        o_c = np.asarray(res.results[c]["out"])
        for s, t in enumerate(_qtiles(r)):
            outp[b, t * 128:(t + 1) * 128] = o_c[s * 128:(s + 1) * 128]
    return outp
```

```python
import math
from contextlib import ExitStack

import numpy as np
import concourse.bass as bass
import concourse.mybir as mybir
from concourse.bass_utils import run_bass_kernel_spmd

F32 = mybir.dt.float32
BF16 = mybir.dt.bfloat16
I32 = mybir.dt.int32
AF = mybir.ActivationFunctionType
ALU = mybir.AluOpType
AX = mybir.AxisListType
PI = math.pi

D = 2048
S = 4096
NT = 32
NQ = 8
EPS = 1e-6
THETA = 500000.0
DFF = 8192
NBIS = 18
import os
KVV = int(os.environ.get('KVV', '0'))
BIS_POOL = tuple(int(c) for c in os.environ.get('BIS_POOL', '').split(',') if c != '')
BIG = 1.0e30

ENGS = ("pe", "act", "dve", "pool", "sp")


class Buf:
    __slots__ = ("name", "w", "r", "excl", "strict")

    def __init__(self, name=""):
        self.name = name
        self.w = None
        self.r = []
        self.excl = False
        self.strict = False


class TT:
    def __init__(self, t, name):
        self.t = t
        self.b = Buf(name)

    def __getitem__(self, k):
        return self.t[k]


def _b(x):
    return x.b if isinstance(x, TT) else x


class Op:
    __slots__ = ("eng", "fn", "dma", "deps", "signal", "tok", "slot_wait", "epoch")

    def __init__(self, eng, fn, dma):
        self.eng = eng
        self.fn = fn
        self.dma = dma
        self.deps = []
        self.signal = False
        self.tok = None
        self.slot_wait = None
        self.epoch = 0


class Prog:
    def __init__(self, nslots=6):
        self.ops = []
        self.nslots = nslots
        self.epoch = 0
        self.barriers = []
        self.stopped = False
        self.last = {}

    def op(self, eng, fn, reads=(), writes=(), dma=False):
        if self.stopped:
            return None
        o = Op(eng, fn, dma)
        o.epoch = self.epoch
        deps = {}
        reads = [_b(x) for x in reads]
        writes = [_b(x) for x in writes]
        writes = writes + [b for b in reads if b.excl and b not in writes]
        reads = [b for b in reads if not b.excl]
        for b in reads:
            if b.w is not None:
                deps[id(b.w)] = (b.w, True)
        for b in writes:
            if b.w is not None and id(b.w) not in deps:
                deps[id(b.w)] = (b.w, b.strict)
            for r in b.r:
                if id(r) not in deps:
                    deps[id(r)] = (r, b.strict)
        for d, raw in deps.values():
            if d.epoch != o.epoch:
                continue
            if (not d.dma) and (not dma) and d.eng == eng:
                if eng == "pe" or not raw:
                    continue
            d.signal = True
            o.deps.append(d)
        for b in map(_b, reads):
            if not dma:
                b.r = [r for r in b.r if r.dma or r.eng != eng or r.epoch != o.epoch]
            b.r.append(o)
        for b in map(_b, writes):
            b.w = o
            b.r = []
        self.ops.append(o)
        if not dma:
            self.last[eng] = o
        return o

    def barrier(self):
        if self.stopped:
            return
        for e, o in self.last.items():
            o.signal = True
        self.last = {}
        self.epoch += 1
        self.barriers.append(len(self.ops))

    def dma(self, q, out, in_, reads=(), writes=(), **kw):
        return self.op(q, lambda e: e.dma_start(out=out, in_=in_, **kw), reads, writes, dma=True)

    def emit(self, nc, final_waits=()):
        with ExitStack() as st:
            esem = {e: st.enter_context(nc.semaphore("s_" + e)) for e in ("pe", "act", "dve", "pool")}
            qsem = {q: [st.enter_context(nc.semaphore("d_%s%d" % (q, i))) for i in range(self.nslots)]
                    for q in ("sp", "act", "pool")}
            ecnt = {e: 0 for e in esem}
            qn = {q: 0 for q in qsem}
            qtot = {q: [0] * self.nslots for q in qsem}
            bar_toks = []
            nb = 0
            for i, o in enumerate(self.ops):
                while nb < len(self.barriers) and self.barriers[nb] == i:
                    toks = [(esem[e], ecnt[e]) for e in esem if ecnt[e] > 0]
                    for q in qsem:
                        for s_ in range(self.nslots):
                            if qtot[q][s_] > 0:
                                toks.append((qsem[q][s_], qtot[q][s_]))
                    bar_toks.append(toks)
                    nb += 1
                if o.dma:
                    q = o.eng
                    s_ = qn[q] % self.nslots
                    qn[q] += 1
                    if qtot[q][s_] > 0:
                        o.slot_wait = (qsem[q][s_], qtot[q][s_])
                    qtot[q][s_] += 16
                    o.tok = (qsem[q][s_], qtot[q][s_])
                elif o.signal:
                    ecnt[o.eng] += 1
                    o.tok = (esem[o.eng], ecnt[o.eng])
            while nb < len(self.barriers):
                bar_toks.append([])
                nb += 1
            per = {e: [o for o in self.ops if o.eng == e] for e in ENGS}
            block = st.enter_context(nc.Block())

            def run(e, eng):
                waited = {}
                cur_epoch = 0

                def w(tok):
                    sem, val = tok
                    k = id(sem)
                    if waited.get(k, 0) < val:
                        eng.wait_ge(sem, val)
                        waited[k] = val

                for o in per[e]:
                    while cur_epoch < o.epoch:
                        for t in bar_toks[cur_epoch]:
                            w(t)
                        cur_epoch += 1
                    if o.slot_wait is not None:
                        w(o.slot_wait)
                    for d in o.deps:
                        w(d.tok)
                    inst = o.fn(eng)
                    if o.dma:
                        inst.then_inc(o.tok[0], 16)
                    elif o.signal:
                        inst.then_inc(o.tok[0], 1)
                if e == "sp":
                    for o in final_waits:
                        w(o.tok)

            @block.tensor
            def _(eng):
                run("pe", eng)

            @block.scalar
            def _(eng):
                run("act", eng)

            @block.vector
            def _(eng):
                run("dve", eng)

            @block.gpsimd
            def _(eng):
                run("pool", eng)

            @block.sync
            def _(eng):
                run("sp", eng)


class _Stop(Exception):
    pass


def build_program(stop=None):
    nc = bass.Bass("TRN2", target_bir_lowering=False)
    dump_ops = []

    def dump(ph, name, ap, shape, dt):
        if stop != ph or P.stopped:
            return
        o_ = nc.dram_tensor("dbg_" + name, list(shape), dt, kind="ExternalOutput").ap()
        P.barrier()
        dump_ops.append(P.dma("sp", o_, ap))

    def checkpoint(name):
        if stop == name:
            P.stopped = True

    def din(name, shape, dt=F32):
        return nc.dram_tensor(name, list(shape), dt, kind="ExternalInput").ap()

    x_all = din("x_all", [S, D])
    xq = din("xq", [NQ * 128, D])
    posk = din("posk", [128, NT], I32)
    posq = din("posq", [128, NQ], I32)
    mem = din("mem", [256, D])
    invf = din("invf", [128, 32])
    pow2 = din("pow2", [128, NBIS])
    ident = din("ident", [128, 128])
    zmT = din("zmT", [128, NQ, 512])
    zb = din("zb", [128, NQ, 512])
    wk_side = din("wk_side", [D, 2432])
    wq_side = din("wq_side", [D, 2576])
    g_mix = din("g_mix", [128, 16])
    w_qb = din("w_qb", [512, 1536])
    g_cq = din("g_cq", [128, 4])
    w_kvb = din("w_kvb", [256, 2048])
    g_ckv = din("g_ckv", [128, 2])
    w_out = din("w_out", [D, D])
    g_cross = din("g_cross", [128, 16])
    g_mem = din("g_mem", [128, 16])
    wqc = din("wqc", [D, 512])
    wkc = din("wkc", [D, 512])
    wvc = din("wvc", [D, 512])
    woc = din("woc", [512, D])
    g_mlp = din("g_mlp", [128, 16])
    w_up = din("w_up", [D, DFF])
    w_down = din("w_down", [DFF, D])
    g_fin = din("g_fin", [128, D])
    out = nc.dram_tensor("out", [NQ * 128, D], F32, kind="ExternalOutput").ap()

    def dscr(name, shape):
        return nc.dram_tensor(name, list(shape), BF16, kind="Internal").ap()

    KnT_scr = dscr("KnT_scr", [128, 8, S])
    KdT_scr = dscr("KdT_scr", [128, 8, S])
    Vm_scr = dscr("Vm_scr", [8, 128, NT, 129])
    Vd_scr = dscr("Vd_scr", [8, 128, NT, 129])
    maskT_scr = dscr("maskT_scr", [128, 144, 128])
    O_scr = dscr("O_scr", [NQ * 128, D])

    P = Prog()
    cnt = [0]

    def MM(out_, lhsT, rhs, start, stop, reads, writes):
        P.op("pe", lambda e: e.matmul(out_, lhsT=lhsT, rhs=rhs, start=start, stop=stop), reads, writes)

    def ACTV(out_, in_, func, reads, writes, **kw):
        P.op("act", lambda e: e.activation(out=out_, in_=in_, func=func, **kw), reads, writes)

    def TS(eng, out_, in0, s1, s2, op0, op1, reads, writes, accum_out=None):
        if op1 is None:
            P.op(eng, lambda e: e.tensor_scalar(out=out_, in0=in0, scalar1=s1, scalar2=None, op0=op0), reads, writes)
        elif accum_out is None:
            P.op(eng, lambda e: e.tensor_scalar(out=out_, in0=in0, scalar1=s1, scalar2=s2, op0=op0, op1=op1), reads, writes)
        else:
            P.op(eng, lambda e: e.tensor_scalar(out=out_, in0=in0, scalar1=s1, scalar2=s2, op0=op0, op1=op1,
                                                accum_out=accum_out), reads, writes)

    def TTO(eng, out_, in0, in1, op, reads, writes):
        P.op(eng, lambda e: e.tensor_tensor(out=out_, in0=in0, in1=in1, op=op), reads, writes)

    def STT(eng, out_, in0, scalar, in1, op0, op1, reads, writes):
        P.op(eng, lambda e: e.scalar_tensor_tensor(out=out_, in0=in0, scalar=scalar, in1=in1, op0=op0, op1=op1),
             reads, writes)

    def CP(eng, out_, in_, reads, writes):
        if eng == "act":
            ACTV(out_, in_, AF.Copy, reads, writes)
        else:
            P.op(eng, lambda e: e.tensor_copy(out=out_, in_=in_), reads, writes)

    def MEMSET(eng, ap, val, writes):
        P.op(eng, lambda e: e.memset(ap, val), (), writes)

    with ExitStack() as top:
      try:
        def alloc(st, name, shape, dt):
            cnt[0] += 1
            t_ = TT(st.enter_context(nc.sbuf_tensor("%s_%d" % (name, cnt[0]), list(shape), dt)), name)
            t_.b.strict = name.startswith("junk")
            return t_

        def palloc(st, name, shape, dt):
            cnt[0] += 1
            t_ = TT(st.enter_context(nc.psum_tensor("%s_%d" % (name, cnt[0]), list(shape), dt)), name)
            t_.b.excl = True
            return t_

        class Ring:
            def __init__(self, st, name, n, shape, dt, psum=False):
                self.items = [(palloc if psum else alloc)(st, "%s%d" % (name, i), shape, dt) for i in range(n)]
                self.i = 0

            def next(self):
                t = self.items[self.i % len(self.items)]
                self.i += 1
                return t

        def TRN(out_, in_, reads, writes):
            P.op("pe", lambda e: e.transpose(out=out_, in_=in_, identity=identb[:]), list(reads) + [identb], writes)

        identf = alloc(top, "identf", [128, 128], F32)
        identb = alloc(top, "identb", [128, 128], BF16)
        invf_t = alloc(top, "invf", [128, 32], F32)
        pow2_t = alloc(top, "pow2", [128, NBIS], F32)
        mhalf = alloc(top, "mhalf", [128, 1], F32)
        stK = ExitStack()
        kropeT = alloc(stK, "kropeT", [128, S], BF16)
        kidx_lo = alloc(stK, "kidx_lo", [128, S], BF16)
        kidx_hi = alloc(stK, "kidx_hi", [128, S], BF16)

        P.dma("sp", identf[:], ident, writes=[identf])
        P.dma("sp", invf_t[:], invf, writes=[invf_t])
        P.dma("sp", pow2_t[:], pow2, writes=[pow2_t])
        CP("dve", identb[:], identf[:], [identf], [identb])
        MEMSET("pool", mhalf[:], -0.5, [mhalf])
        MEMSET("pool", kidx_lo[:], 0.0, [kidx_lo])
        MEMSET("pool", kidx_hi[:], 0.0, [kidx_hi])

        def make_tables(pos_dram, n, cos_t, sin_t):
            with ExitStack() as st:
                posi = alloc(st, "posi", [128, n], I32)
                posf = alloc(st, "posf", [128, n], F32)
                ang = alloc(st, "ang", [128, n, 32], F32)
                a2 = alloc(st, "a2", [128, n, 32], F32)
                tp = alloc(st, "tp", [128, n, 32], F32)
                ki = alloc(st, "ki", [128, n, 32], I32)
                P.dma("sp", posi[:], pos_dram, writes=[posi])
                CP("dve", posf[:], posi[:], [posi], [posf])
                for t in range(n):
                    TS("dve", ang[:, t, :], invf_t[:], posf[:, t:t + 1], None, ALU.mult, None, [invf_t, posf], [ang])
                HI = 6.28125
                LO = 2 * PI - 6.28125
                for (dst, shift) in ((sin_t, 0.0), (cos_t, PI / 2)):
                    TS("dve", a2[:], ang[:], shift, None, ALU.add, None, [ang], [a2])
                    TS("dve", tp[:], a2[:], 1.0 / (2 * PI), None, ALU.mult, None, [a2], [tp])
                    CP("dve", ki[:], tp[:], [tp], [ki])
                    CP("dve", tp[:], ki[:], [ki], [tp])
                    STT("dve", a2[:], tp[:], -HI, a2[:], ALU.mult, ALU.add, [tp, a2], [a2])
                    STT("dve", a2[:], tp[:], -LO, a2[:], ALU.mult, ALU.add, [tp, a2], [a2])
                    TS("dve", tp[:], a2[:], PI, None, ALU.is_gt, None, [a2], [tp])
                    STT("dve", a2[:], tp[:], -2 * PI, a2[:], ALU.mult, ALU.add, [tp, a2], [a2])
                    TS("dve", tp[:], a2[:], -PI, None, ALU.is_lt, None, [a2], [tp])
                    STT("dve", a2[:], tp[:], 2 * PI, a2[:], ALU.mult, ALU.add, [tp, a2], [a2])
                    ACTV(dst[:], a2[:], AF.Sin, [a2], [dst])
                P.barrier()

        def rope(src, dst, H, half, cos_ap, sin_ap, tmp, reads, writes):
            cb = cos_ap.to_broadcast([128, H, half])
            sb_ = sin_ap.to_broadcast([128, H, half])
            n = H * half
            ta = tmp[:, 0, 0:n].rearrange("p (h d) -> p h d", h=H)
            tb = tmp[:, 1, 0:n].rearrange("p (h d) -> p h d", h=H)
            tc_ = tmp[:, 2, 0:n].rearrange("p (h d) -> p h d", h=H)
            td = tmp[:, 3, 0:n].rearrange("p (h d) -> p h d", h=H)
            x1_ = src[:, :, 0:half]
            x2_ = src[:, :, half:2 * half]
            TTO("dve", ta, x1_, cb, ALU.mult, reads, [tmp])
            TTO("dve", tb, x2_, sb_, ALU.mult, reads, [tmp])
            TTO("dve", tc_, x2_, cb, ALU.mult, reads, [tmp])
            TTO("dve", td, x1_, sb_, ALU.mult, reads, [tmp])
            TTO("pool", dst[:, :, 0:half], ta, tb, ALU.subtract, [tmp], writes)
            TTO("pool", dst[:, :, half:2 * half], tc_, td, ALU.add, [tmp], writes)

        def rstd_of(src_ap, n, reads, junk, stat):
            ACTV(junk[:, 0:n], src_ap, AF.Square, reads, [junk, stat], accum_out=stat[:, 0:1])
            TS("dve", stat[:, 1:2], stat[:, 0:1], 1.0 / n, EPS, ALU.mult, ALU.add, [stat], [stat])
            TTO("pool", stat[:, 2:3], stat[:, 1:2], mhalf[:], ALU.pow, [stat, mhalf], [stat])
            return stat[:, 2:3]

        def load_w(dst, src, kc, g_t=None, eng_fold="dve"):
            for c in range(kc):
                P.dma("pool", dst[:, c, :], src[c * 128:(c + 1) * 128, :], writes=[dst])
            if g_t is not None:
                for c in range(kc):
                    TS(eng_fold, dst[:, c, :], dst[:, c, :], g_t[:, c:c + 1], None, ALU.mult, None, [dst, g_t], [dst])

        def load_g(st, name, src, k):
            t = alloc(st, name, [128, k], F32)
            P.dma("sp", t[:], src, writes=[t])
            return t

        def norm_transpose(x_ap, x_tt, dstf, dst_tt, junk, stat, xb, ptr_ring):
            rs = rstd_of(x_ap, D, [x_tt], junk, stat)
            TS("dve", xb[:], x_ap, rs, None, ALU.mult, None, [x_tt, stat], [xb])
            for g in range(2):
                pt = ptr_ring.next()
                for j in range(8):
                    c = g * 8 + j
                    TRN(pt[:, j, :], xb[:, c * 128:(c + 1) * 128], [xb], [pt])
                CP("act" if g == 0 else "dve", dstf(g), pt[:], [pt], [dst_tt])

        with ExitStack() as st:
            cosK = alloc(st, "cosK", [128, NT, 32], F32)
            sinK = alloc(st, "sinK", [128, NT, 32], F32)
            make_tables(posk, NT, cosK, sinK)
            dump("T", "cosK", cosK[:], [128, NT, 32], F32)
            dump("T", "sinK", sinK[:], [128, NT, 32], F32)
            checkpoint("T")
            wk = alloc(st, "wk", [128, 16, 2432], BF16)
            wkv = alloc(st, "wkv", [128, 2, 2048], BF16)
            gm = load_g(st, "g_mix", g_mix, 16)
            gk = load_g(st, "g_ckv", g_ckv, 2)
            load_w(wk, wk_side, 16, gm)
            load_w(wkv, w_kvb, 2, gk)
            xring = Ring(st, "xa", 1, [128, D], F32)
            junk = alloc(st, "junkA", [128, 256], BF16)
            statr = Ring(st, "statA", 6, [128, 4], F32)
            xbr = Ring(st, "xbA", 2, [128, D], BF16)
            hTr = Ring(st, "hTA", 2, [128, 16, 128], BF16)
            ptr_ring = Ring(st, "ptrA", 2, [128, 8, 128], BF16, psum=True)
            pm_ring = Ring(st, "pmA", 6, [128, 512], F32, psum=True)
            tmp_ring = Ring(st, "ropetmp", 2, [128, 4, 256], F32)
            ckvn_r = Ring(st, "ckvn", 2, [128, 256], BF16)
            ckvT_r = Ring(st, "ckvT", 2, [128, 2, 128], BF16)
            kr_tok_r = Ring(st, "kr_tok", 2, [128, 128], BF16)
            ki_tok_r = Ring(st, "ki_tok", 2, [128, 128], BF16)
            kd_tok_r = Ring(st, "kd_tok", 2, [128, 8, 128], BF16)
            kn_tok_r = Ring(st, "kn_tok", 2, [128, 8, 128], BF16)
            KnT_st_r = Ring(st, "KnT_st", 1, [128, 8, 512], BF16)
            KdT_st_r = Ring(st, "KdT_st", 1, [128, 8, 512], BF16)
            Vm_st_r = Ring(st, "Vm_st", 1, [128, 8, 4, 129], BF16)
            Vd_st_r = Ring(st, "Vd_st", 2, [128, 8, 4, 129], BF16)
            for rg in (Vm_st_r, Vd_st_r):
                for it in rg.items:
                    MEMSET("pool", it[:, :, :, 128:129], 1.0, [it])
            dump("W", "wk", wk[:, :, 0:512], [128, 16, 512], BF16)
            dump("W", "wkv", wkv[:, :, 0:512], [128, 2, 512], BF16)
            checkpoint("W")

            Vd_groups = {}
            ctx = {}

            def stage0(t):
                x_t = xring.next()
                P.dma("sp", x_t[:], x_all[t * 128:(t + 1) * 128, :], writes=[x_t])
                xb = xbr.next()
                stat = statr.next()
                rs = rstd_of(x_t[:], D, [x_t], xb, stat)
                TS("dve", xb[:], x_t[:], rs, None, ALU.mult, None, [x_t, stat], [xb])
                ctx[("xb", t)] = xb

            def stage1(t):
                tl = t % 4
                g4 = t // 4
                if tl == 0:
                    Vd_groups[g4] = Vd_st_r.next()
                Vd_st = Vd_groups[g4]
                xb = ctx.pop(("xb", t))
                hT = hTr.next()
                for g in range(2):
                    pt = ptr_ring.next()
                    for j in range(8):
                        c = g * 8 + j
                        TRN(pt[:, j, :], xb[:, c * 128:(c + 1) * 128], [xb], [pt])
                    CP("act" if g == 0 else "dve", hT[:, g * 8:(g + 1) * 8, :], pt[:], [pt], [hT])
                pms = []
                for g, (c0, n) in enumerate(((0, 384), (384, 512), (896, 512), (1408, 512), (1920, 512))):
                    pm = pm_ring.next()
                    for c in range(16):
                        MM(pm[:, 0:n], hT[:, c, :], wk[:, c, c0:c0 + n], c == 0, c == 15, [hT, wk], [pm])
                    pms.append(pm)
                p0 = pms[0]
                stat = statr.next()
                rs = rstd_of(p0[:, 0:256], 256, [p0], junk, stat)
                ckvn = ckvn_r.next()
                TS("dve", ckvn[:], p0[:, 0:256], rs, None, ALU.mult, None, [p0, stat], [ckvn])
                tmp = tmp_ring.next()
                kr_tok = kr_tok_r.next()
                rope(p0[:, 256:320].rearrange("p (h d) -> p h d", h=1), kr_tok[:, 0:64].rearrange("p (h d) -> p h d", h=1),
                     1, 32, cosK[:, t:t + 1, :], sinK[:, t:t + 1, :], tmp, [p0, cosK, sinK], [kr_tok])
                CP("pool", kr_tok[:, 64:128], kr_tok[:, 0:64], [kr_tok], [kr_tok])
                tmp = tmp_ring.next()
                ki_tok = ki_tok_r.next()
                rope(p0[:, 320:384].rearrange("p (h d) -> p h d", h=1), ki_tok[:, 0:64].rearrange("p (h d) -> p h d", h=1),
                     1, 8, cosK[:, t:t + 1, 0:32:4], sinK[:, t:t + 1, 0:32:4], tmp, [p0, cosK, sinK], [ki_tok])
                CP("act", ki_tok[:, 16:64], p0[:, 336:384], [p0], [ki_tok])
                CP("pool", ki_tok[:, 64:128], ki_tok[:, 0:64], [ki_tok], [ki_tok])
                kd_tok = kd_tok_r.next()
                for g in range(2):
                    pm = pms[1 + g]
                    tmp = tmp_ring.next()
                    src = pm[:].rearrange("p (h d) -> p h d", h=4)
                    dst = kd_tok[:, g * 4:(g + 1) * 4, :]
                    rope(src, dst, 4, 16, cosK[:, t:t + 1, 0:32:2], sinK[:, t:t + 1, 0:32:2], tmp, [pm, cosK, sinK], [kd_tok])
                    CP("act", dst[:, :, 32:128], src[:, :, 32:128], [pm], [kd_tok])
                for g in range(2):
                    pm = pms[3 + g]
                    CP("act" if g == 0 else "dve", Vd_st[:, g * 4:(g + 1) * 4, tl, 0:128],
                       pm[:].rearrange("p (h d) -> p h d", h=4), [pm], [Vd_st])
                ctx[t] = (ckvn, kr_tok, ki_tok, kd_tok, Vd_st)

            ctx2, ctx3 = {}, {}

            def s2a(t):
                tl = t % 4
                ckvn, kr_tok, ki_tok, kd_tok, Vd_st = ctx[t]
                KdT_st = KdT_st_r.items[0]
                ckvT = ckvT_r.next()
                pt = ptr_ring.next()
                for c in range(2):
                    TRN(pt[:, c, :], ckvn[:, c * 128:(c + 1) * 128], [ckvn], [pt])
                CP("dve", ckvT[:], pt[:, 0:2, :], [pt], [ckvT])
                pt = ptr_ring.next()
                for h in range(8):
                    TRN(pt[:, h, :], kd_tok[:, h, :], [kd_tok], [pt])
                CP("act", KdT_st[:, :, tl * 128:(tl + 1) * 128], pt[:], [pt], [KdT_st])
                pt = ptr_ring.next()
                TRN(pt[:, 0, :], kr_tok[:], [kr_tok], [pt])
                TRN(pt[:, 1, :], ki_tok[:], [ki_tok], [pt])
                CP("act", kropeT[:, t * 128:(t + 1) * 128], pt[:, 0, :], [pt], [kropeT])
                CP("dve", kidx_lo[0:64, t * 128:(t + 1) * 128], pt[0:64, 1, :], [pt], [kidx_lo])
                CP("dve", kidx_hi[64:128, t * 128:(t + 1) * 128], pt[64:128, 1, :], [pt], [kidx_hi])
                if tl == 3:
                    g4 = t // 4
                    P.dma("sp", KdT_scr[:, :, g4 * 512:(g4 + 1) * 512], KdT_st[:], reads=[KdT_st])
                ctx2[t] = ckvT

            def s2b(t):
                tl = t % 4
                ckvn, kr_tok, ki_tok, kd_tok, Vd_st = ctx.pop(t)
                ckvT = ctx2.pop(t)
                Vm_st = Vm_st_r.items[0]
                kn_tok = kn_tok_r.next()
                for g in range(4):
                    pm = pm_ring.next()
                    for c in range(2):
                        MM(pm[:], ckvT[:, c, :], wkv[:, c, g * 512:(g + 1) * 512], c == 0, c == 1, [ckvT, wkv], [pm])
                    v4 = pm[:].rearrange("p (h e d) -> p h e d", h=2, e=2)
                    CP("act", kn_tok[:, 2 * g:2 * g + 2, :], v4[:, :, 0, :], [pm], [kn_tok])
                    CP("dve", Vm_st[:, 2 * g:2 * g + 2, tl, 0:128], v4[:, :, 1, :], [pm], [Vm_st])
                if tl == 3:
                    g4 = t // 4
                    P.dma("sp", Vm_scr[:, :, g4 * 4:(g4 + 1) * 4, :].rearrange("h p t c -> p h t c"), Vm_st[:],
                          reads=[Vm_st])
                    P.dma("sp", Vd_scr[:, :, g4 * 4:(g4 + 1) * 4, :].rearrange("h p t c -> p h t c"), Vd_st[:],
                          reads=[Vd_st])
                ctx3[t] = kn_tok

            def s2c(t):
                tl = t % 4
                kn_tok = ctx3.pop(t)
                KnT_st = KnT_st_r.items[0]
                pt = ptr_ring.next()
                for h in range(8):
                    TRN(pt[:, h, :], kn_tok[:, h, :], [kn_tok], [pt])
                CP("dve", KnT_st[:, :, tl * 128:(tl + 1) * 128], pt[:], [pt], [KnT_st])
                if tl == 3:
                    g4 = t // 4
                    P.dma("sp", KnT_scr[:, :, g4 * 512:(g4 + 1) * 512], KnT_st[:], reads=[KnT_st])

            stage0(0)
            stage0(1)
            stage1(0)
            for t in range(NT):
                if t + 2 < NT:
                    stage0(t + 2)
                s2a(t)
                if t + 1 < NT:
                    stage1(t + 1)
                s2b(t)
                if t >= 1:
                    s2c(t - 1)
            s2c(NT - 1)
            P.barrier()
            dump("A", "kropeT", kropeT[:], [128, S], BF16)
            dump("A", "KnT", KnT_scr, [128, 8, S], BF16)
            dump("A", "KdT", KdT_scr, [128, 8, S], BF16)
            dump("A", "Vm", Vm_scr, [8, 128, NT, 129], BF16)
            dump("A", "Vd", Vd_scr, [8, 128, NT, 129], BF16)
            if stop == "A":
                P.barrier()
            checkpoint("A")

        with ExitStack() as stB:
            QnT = alloc(stB, "QnT", [128, 8, NQ * 128], BF16)
            QrT_lo = alloc(stB, "QrT_lo", [128, 4, NQ * 128], BF16)
            QrT_hi = alloc(stB, "QrT_hi", [128, 4, NQ * 128], BF16)
            MEMSET("pool", QrT_lo[:], 0.0, [QrT_lo])
            MEMSET("pool", QrT_hi[:], 0.0, [QrT_hi])
            QdT = alloc(stB, "QdT", [128, 8, NQ * 128], BF16)
            with ExitStack() as stI:
                qiT = alloc(stI, "qiT", [128, 8, NQ * 128], BF16)
                wsc = alloc(stI, "wsc", [128, NQ, 16], F32)
                cosQ = alloc(stI, "cosQ", [128, NQ, 32], F32)
                sinQ = alloc(stI, "sinQ", [128, NQ, 32], F32)
                make_tables(posq, NQ, cosQ, sinQ)

                def qside(pass_id):
                    with ExitStack() as st:
                        ncol = 1552 if pass_id == 0 else 1024
                        col0 = 0 if pass_id == 0 else 1552
                        wq = alloc(st, "wq", [128, 16, ncol], BF16)
                        gm = load_g(st, "g_mix2", g_mix, 16)
                        for c in range(16):
                            P.dma("pool", wq[:, c, :], wq_side[c * 128:(c + 1) * 128, col0:col0 + ncol], writes=[wq])
                        for c in range(16):
                            TS("dve", wq[:, c, :], wq[:, c, :], gm[:, c:c + 1], None, ALU.mult, None, [wq, gm], [wq])
                        if pass_id == 0:
                            wqb = alloc(st, "wqb", [128, 4, 1536], BF16)
                            gq = load_g(st, "g_cq", g_cq, 4)
                            load_w(wqb, w_qb, 4, gq)
                        xring = Ring(st, "xb1", 2, [128, D], F32)
                        junk = alloc(st, "junkB", [128, 512], BF16)
                        statr = Ring(st, "statB", 6, [128, 4], F32)
                        xbr = Ring(st, "xbB", 2, [128, D], BF16)
                        hTr = Ring(st, "hTB", 2, [128, 16, 128], BF16)
                        ptr_ring = Ring(st, "ptrB", 2, [128, 8, 128], BF16, psum=True)
                        pm_ring = Ring(st, "pmB", 6, [128, 512], F32, psum=True)
                        tmp_ring = Ring(st, "ropetmpB", 2, [128, 4, 256], F32)
                        if pass_id == 0:
                            cqn_r = Ring(st, "cqn", 2, [128, 512], BF16)
                            cqT_r = Ring(st, "cqT", 2, [128, 4, 128], BF16)
                            qn_tok_r = Ring(st, "qn_tok", 2, [128, 8, 128], BF16)
                            qr_tok_r = Ring(st, "qr_tok", 2, [128, 8, 64], BF16)
                            qi_tok_r = Ring(st, "qi_tok", 2, [128, 16, 64], BF16)
                        else:
                            qd_tok_r = Ring(st, "qd_tok", 2, [128, 8, 128], BF16)
                        qctx = {}

                        def q0(s):
                            x_t = xring.next()
                            P.dma("sp", x_t[:], xq[s * 128:(s + 1) * 128, :], writes=[x_t])
                            xb = xbr.next()
                            stat = statr.next()
                            rs = rstd_of(x_t[:], D, [x_t], xb, stat)
                            TS("dve", xb[:], x_t[:], rs, None, ALU.mult, None, [x_t, stat], [xb])
                            qctx[s] = xb

                        q0(0)
                        for s in range(NQ):
                            qs = slice(s * 128, (s + 1) * 128)
                            if s + 1 < NQ:
                                q0(s + 1)
                            xb = qctx.pop(s)
                            hT = hTr.next()
                            for g in range(2):
                                pt = ptr_ring.next()
                                for j in range(8):
                                    c = g * 8 + j
                                    TRN(pt[:, j, :], xb[:, c * 128:(c + 1) * 128], [xb], [pt])
                                CP("act" if g == 0 else "dve", hT[:, g * 8:(g + 1) * 8, :], pt[:], [pt], [hT])
                            if pass_id == 0:
                                pms = []
                                for g, (c0, n) in enumerate(((0, 512), (512, 512), (1024, 512), (1536, 16))):
                                    pm = pm_ring.next()
                                    for c in range(16):
                                        MM(pm[:, 0:n], hT[:, c, :], wq[:, c, c0:c0 + n], c == 0, c == 15, [hT, wq], [pm])
                                    pms.append(pm)
                                TS("dve", wsc[:, s, :], pms[3][:, 0:16], 1.0 / 32.0, None, ALU.mult, None, [pms[3]], [wsc])
                                stat = statr.next()
                                rs = rstd_of(pms[0][:], 512, [pms[0]], junk, stat)
                                cqn = cqn_r.next()
                                TS("dve", cqn[:], pms[0][:], rs, None, ALU.mult, None, [pms[0], stat], [cqn])
                                qi_tok = qi_tok_r.next()
                                for g in range(2):
                                    pm = pms[1 + g]
                                    tmp = tmp_ring.next()
                                    src = pm[:].rearrange("p (h d) -> p h d", h=8)
                                    dst = qi_tok[:, g * 8:(g + 1) * 8, :]
                                    rope(src, dst, 8, 8, cosQ[:, s:s + 1, 0:32:4], sinQ[:, s:s + 1, 0:32:4], tmp,
                                         [pm, cosQ, sinQ], [qi_tok])
                                    CP("act", dst[:, :, 16:64], src[:, :, 16:64], [pm], [qi_tok])
                                cqT = cqT_r.next()
                                pt = ptr_ring.next()
                                for c in range(4):
                                    TRN(pt[:, c, :], cqn[:, c * 128:(c + 1) * 128], [cqn], [pt])
                                CP("dve", cqT[:], pt[:, 0:4, :], [pt], [cqT])
                                qn_tok = qn_tok_r.next()
                                qr_tok = qr_tok_r.next()
                                for g in range(3):
                                    pm = pm_ring.next()
                                    for c in range(4):
                                        MM(pm[:], cqT[:, c, :], wqb[:, c, g * 512:(g + 1) * 512], c == 0, c == 3, [cqT, wqb], [pm])
                                    if g < 2:
                                        CP("act", qn_tok[:, g * 4:(g + 1) * 4, :], pm[:].rearrange("p (h d) -> p h d", h=4),
                                           [pm], [qn_tok])
                                    else:
                                        tmp = tmp_ring.next()
                                        rope(pm[:].rearrange("p (h d) -> p h d", h=8), qr_tok[:], 8, 32,
                                             cosQ[:, s:s + 1, :], sinQ[:, s:s + 1, :], tmp, [pm, cosQ, sinQ], [qr_tok])
                                pt = ptr_ring.next()
                                for h in range(8):
                                    TRN(pt[:, h, :], qn_tok[:, h, :], [qn_tok], [pt])
                                CP("act", QnT[:, :, qs], pt[:], [pt], [QnT])
                                pt = ptr_ring.next()
                                qr2 = qr_tok[:].rearrange("p (a b) d -> p a (b d)", b=2)
                                for h2 in range(4):
                                    TRN(pt[:, h2, :], qr2[:, h2, :], [qr_tok], [pt])
                                CP("dve", QrT_lo[0:64, :, qs], pt[0:64, 0:4, :], [pt], [QrT_lo])
                                CP("dve", QrT_hi[64:128, :, qs], pt[64:128, 0:4, :], [pt], [QrT_hi])
                                pt = ptr_ring.next()
                                qi2 = qi_tok[:].rearrange("p (a b) d -> p a (b d)", b=2)
                                for h2 in range(8):
                                    TRN(pt[:, h2, :], qi2[:, h2, :], [qi_tok], [pt])
                                CP("act", qiT[:, :, qs], pt[:], [pt], [qiT])
                            else:
                                pms = []
                                for g in range(2):
                                    pm = pm_ring.next()
                                    for c in range(16):
                                        MM(pm[:], hT[:, c, :], wq[:, c, g * 512:(g + 1) * 512], c == 0, c == 15, [hT, wq], [pm])
                                    pms.append(pm)
                                qd_tok = qd_tok_r.next()
                                for g in range(2):
                                    pm = pms[g]
                                    tmp = tmp_ring.next()
                                    src = pm[:].rearrange("p (h d) -> p h d", h=4)
                                    dst = qd_tok[:, g * 4:(g + 1) * 4, :]
                                    rope(src, dst, 4, 16, cosQ[:, s:s + 1, 0:32:2], sinQ[:, s:s + 1, 0:32:2], tmp,
                                         [pm, cosQ, sinQ], [qd_tok])
                                    CP("act", dst[:, :, 32:128], src[:, :, 32:128], [pm], [qd_tok])
                                pt = ptr_ring.next()
                                for h in range(8):
                                    TRN(pt[:, h, :], qd_tok[:, h, :], [qd_tok], [pt])
                                CP("dve", QdT[:, :, qs], pt[:], [pt], [QdT])
                        P.barrier()

                qside(0)
                qside(1)
                dump("B1", "QnT", QnT[:], [128, 8, NQ * 128], BF16)
                dump("B1", "QdT", QdT[:], [128, 8, NQ * 128], BF16)
                dump("B1", "qiT", qiT[:], [128, 8, NQ * 128], BF16)
                dump("B1", "wsc", wsc[:], [128, NQ, 16], F32)
                if stop == "B1":
                    P.barrier()
                checkpoint("B1")

                with ExitStack() as st:
                    zb_ring = Ring(st, "zb", 4, [128, 512], F32)
                    pz_ring = Ring(st, "pz", 3, [128, 512], F32, psum=True)
                    psc_ring = Ring(st, "psc", 2, [128, 512], F32, psum=True)
                    ptr_ring = Ring(st, "ptrI", 2, [128, 8, 128], BF16, psum=True)
                    r_ring = Ring(st, "relu", 4, [128, 512], BF16)
                    dg_ring = Ring(st, "dg", 3, [128, 16, 128], BF16)
                    sc_ring = Ring(st, "score", 4, [128, S], F32)
                    junkI_d = alloc(st, "junkI", [128, S], mybir.dt.uint8)
                    junkI_p = alloc(st, "junkIp", [128, S], mybir.dt.uint8)
                    mask_ring = Ring(st, "mask", 1, [128, S], BF16)
                    mT_ring = Ring(st, "mTst", 1, [128, NT, 128], BF16)
                    bs_ring = Ring(st, "bis", 4, [128, 8 + NBIS], F32)
                    blk_of = [0]
                    for s_ in range(NQ):
                        blk_of.append(blk_of[-1] + 4 * (s_ + 1))

                    def front(s):
                        E = 4 * (s + 1)
                        n = E * 128
                        qs = slice(s * 128, (s + 1) * 128)
                        zb_t = zb_ring.next()
                        P.dma("sp", zb_t[:], zb[:, s, :], writes=[zb_t])
                        dg = dg_ring.next()
                        for h in range(16):
                            TS("pool", dg[:, h, :], identb[:], wsc[:, s, h:h + 1], None, ALU.mult, None, [identb, wsc], [dg])
                        score = sc_ring.next()
                        for kb in range(s + 1):
                            ks = slice(kb * 512, (kb + 1) * 512)
                            psc = psc_ring.next()
                            rprev = None
                            for h in range(16):
                                p0_ = (h % 2) * 64
                                pz = pz_ring.next()
                                kix = kidx_lo if h % 2 == 0 else kidx_hi
                                MM(pz[:], qiT[:, h // 2, qs], kix[:, ks], True, True, [qiT, kix], [pz])
                                rb = r_ring.next()
                                ACTV(rb[:], pz[:], AF.Relu, [pz], [rb])
                                if rprev is not None:
                                    MM(psc[:], dg[:, h - 1, :], rprev[:], h - 1 == 0, False, [dg, rprev], [psc])
                                rprev = rb
                            MM(psc[:], dg[:, 15, :], rprev[:], False, True, [dg, rprev], [psc])
                            CP("act", score[:, ks], psc[:], [psc], [score])
                        return score, zb_t

                    def bis(s, score, zb_t, junkI, bs):
                        n = 4 * (s + 1) * 128
                        P.op("dve", lambda e, o_=bs[:, 0:1], i_=score[:, 0:n]: e.tensor_reduce(out=o_, in_=i_, axis=AX.X, op=ALU.max),
                             [score], [bs])
                        yield
                        P.op("dve", lambda e, o_=bs[:, 1:2], i_=score[:, 0:n]: e.tensor_reduce(out=o_, in_=i_, axis=AX.X, op=ALU.min),
                             [score], [bs])
                        yield
                        TTO("pool", score[:, n - 512:n], score[:, n - 512:n], zb_t[:], ALU.add, [score, zb_t, bs], [score])
                        TTO("dve", bs[:, 2:3], bs[:, 0:1], bs[:, 1:2], ALU.subtract, [bs], [bs])
                        yield
                        TS("dve", bs[:, 8:8 + NBIS], pow2_t[:], bs[:, 2:3], None, ALU.mult, None, [pow2_t, bs], [bs])
                        yield
                        mid = bs[:, 3:4]
                        TS("dve", mid, bs[:, 1:2], bs[:, 8:9], None, ALU.add, None, [bs], [bs])
                        yield
                        for i in range(NBIS):
                            Hi = bs[:, 8 + i:9 + i]
                            Hn = bs[:, 9 + i:10 + i] if i + 1 < NBIS else Hi
                            TS("dve", junkI[:, 0:n], score[:, 0:n], mid, 0.0, ALU.is_ge, ALU.add, [score, bs], [junkI, bs],
                               accum_out=bs[:, 4:5])
                            yield
                            STT("dve", bs[:, 5:6], bs[:, 4:5], 255.5, Hi, ALU.is_ge, ALU.mult, [bs], [bs])
                            yield
                            STT("dve", mid, mid, Hn, bs[:, 5:6], ALU.subtract, ALU.add, [bs], [bs])
                            yield

                    def back(s, score, bs):
                        E = 4 * (s + 1)
                        n = E * 128
                        mask = mask_ring.next()
                        TS("dve", mask[:, 0:n], score[:, 0:n], bs[:, 3:4], None, ALU.is_ge, None, [score, bs], [mask])
                        mT = mT_ring.next()
                        for g in range((E + 7) // 8):
                            pt = ptr_ring.next()
                            nb_ = min(8, E - g * 8)
                            for j in range(nb_):
                                kt = g * 8 + j
                                TRN(pt[:, j, :], mask[:, kt * 128:(kt + 1) * 128], [mask], [pt])
                            CP("act", mT[:, g * 8:g * 8 + nb_, :], pt[:, 0:nb_, :], [pt], [mT])
                        P.dma("sp", maskT_scr[:, blk_of[s]:blk_of[s] + E, :], mT[:, 0:E, :], reads=[mT])

                    fronts = {0: front(0), 1: front(1)}
                    for a in range(0, NQ, 2):
                        b = a + 1
                        sa, za = fronts.pop(a)
                        sb_, zb2 = fronts.pop(b)
                        if a + 2 < NQ:
                            fronts[a + 2] = front(a + 2)
                            fronts[a + 3] = front(a + 3)
                        bsa, bsb = bs_ring.next(), bs_ring.next()
                        ga = bis(a, sa, za, junkI_d, bsa)
                        gb = bis(b, sb_, zb2, junkI_p, bsb)
                        live = [ga, gb]
                        while live:
                            for g_ in list(live):
                                try:
                                    next(g_)
                                except StopIteration:
                                    live.remove(g_)
                        back(a, sa, bsa)
                        back(b, sb_, bsb)
                    score, bs = sb_, bsb
                    P.barrier()
                    dump("B2", "maskT", maskT_scr, [128, 144, 128], BF16)
                    dump("B2", "score", score[:], [128, S], F32)
                    dump("B2", "bs", bs[:], [128, 8 + NBIS], F32)
                    if stop == "B2":
                        P.barrier()
                    checkpoint("B2")

            with ExitStack() as st:
                maskT = alloc(st, "maskT", [128, 144, 128], BF16)
                zmT_t = alloc(st, "zmT", [128, NQ, 512], BF16)
                O_all = alloc(st, "O_all", [128, NQ, D], BF16)
                P.dma("sp", maskT[:], maskT_scr, writes=[maskT])
                P.dma("pool", zmT_t[:], zmT, writes=[zmT_t])
                KT_ring = Ring(st, "KT", 2, [128, S], BF16)
                V_ring = Ring(st, "V", 2, [128, NT, 129], BF16)
                pS_ring = Ring(st, "pS", 5, [128, 512], F32, psum=True)
                pO_ring = Ring(st, "pO", 2, [128, 512], F32, psum=True)
                PT_ring = Ring(st, "PT", 6, [128, 512], BF16)
                rs_ring = Ring(st, "rsum", 4, [128, 1], F32)
                blk_off = [0]
                for s in range(NQ):
                    blk_off.append(blk_off[-1] + 4 * (s + 1))
                groups = [(s, kb) for s in range(NQ) for kb in range(s + 1)]
                for hh in range(16):
                    mla = hh < 8
                    h = hh % 8
                    KT = KT_ring.next()
                    V = V_ring.next()
                    if mla:
                        P.dma("sp", KT[:], KnT_scr[:, h, :], writes=[KT])
                        P.dma("sp", V[:], Vm_scr[h], writes=[V])
                        scale = 192.0 ** -0.5
                    else:
                        P.dma("sp", KT[:], KdT_scr[:, h, :], writes=[KT])
                        P.dma("sp", V[:], Vd_scr[h], writes=[V])
                        scale = 128.0 ** -0.5
                    p0_ = (h % 2) * 64

                    def qk(s, kb):
                        qs = slice(s * 128, (s + 1) * 128)
                        pS = pS_ring.next()
                        for j in range(4):
                            kt = 4 * kb + j
                            ks = slice(kt * 128, (kt + 1) * 128)
                            if mla:
                                MM(pS[:, j * 128:(j + 1) * 128], KT[:, ks], QnT[:, h, qs], True, False, [KT, QnT], [pS])
                                QrP = QrT_lo if h % 2 == 0 else QrT_hi
                                MM(pS[:, j * 128:(j + 1) * 128], kropeT[:, ks], QrP[:, h // 2, qs],
                                   False, True, [kropeT, QrP], [pS])
                            else:
                                MM(pS[:, j * 128:(j + 1) * 128], KT[:, ks], QdT[:, h, qs], True, True, [KT, QdT], [pS])
                        return pS

                    LOOK = 3
                    pend = [qk(*groups[i]) for i in range(LOOK)]
                    pO = None
                    deferred = None
                    for gi, (s, kb) in enumerate(groups):
                        pS = pend.pop(0)
                        if gi + LOOK < len(groups):
                            pend.append(qk(*groups[gi + LOOK]))
                        PT = PT_ring.next()
                        ACTV(PT[:], pS[:], AF.Exp, [pS], [PT], scale=scale)
                        if mla:
                            if kb == s:
                                TTO("dve", PT[:], PT[:], zmT_t[:, s, :], ALU.mult, [PT, zmT_t], [PT])
                        else:
                            b0 = blk_off[s] + 4 * kb
                            TTO("dve" if gi % 4 != 3 else "pool", PT[:], PT[:],
                                maskT[:, b0:b0 + 4, :].rearrange("p a b -> p (a b)"), ALU.mult, [PT, maskT], [PT])
                        if kb == 0:
                            pO = pO_ring.next()
                        E = 4 * (s + 1)
                        for j in range(4):
                            kt = 4 * kb + j
                            MM(pO[:, 0:129], PT[:, j * 128:(j + 1) * 128], V[:, kt, :], kt == 0, kt == E - 1, [PT, V], [pO])
                        if deferred is not None:
                            deferred()
                            deferred = None
                        if kb == s:
                            def deferred(pO=pO, s=s, hh=hh):
                                rsum = rs_ring.next()
                                P.op("dve", lambda e, o_=rsum[:], i_=pO[:, 128:129]: e.reciprocal(out=o_, in_=i_), [pO], [rsum])
                                col = hh * 128
                                ACTV(O_all[:, s, col:col + 128], pO[:, 0:128], AF.Copy, [pO, rsum], [O_all], scale=rsum[:])
                    if deferred is not None:
                        deferred()
                        deferred = None
                P.dma("sp", O_scr.rearrange("(s p) d -> p s d", p=128), O_all[:], reads=[O_all])
                P.barrier()
                dump("B3", "O", O_scr, [NQ * 128, D], BF16)
                if stop == "B3":
                    P.barrier()
                checkpoint("B3")

        stK.close()
        with ExitStack() as stR:
            x1 = alloc(stR, "x1", [128, NQ, D], F32)
            with ExitStack() as st:
                wo = alloc(st, "wo", [128, 16, D], BF16)
                load_w(wo, w_out, 16)
                xring = Ring(st, "xc", 2, [128, D], F32)
                Or = Ring(st, "Oc", 2, [128, D], BF16)
                OT_r = Ring(st, "OT", 2, [128, 16, 128], BF16)
                ptr_ring = Ring(st, "ptrC", 2, [128, 8, 128], BF16, psum=True)
                pm_ring = Ring(st, "pmC", 4, [128, 512], F32, psum=True)
                for s in range(NQ):
                    qs = slice(s * 128, (s + 1) * 128)
                    x_t = xring.next()
                    P.dma("sp", x_t[:], xq[qs, :], writes=[x_t])
                    O_t = Or.next()
                    P.dma("sp", O_t[:], O_scr[qs, :], writes=[O_t])
                    OT = OT_r.next()
                    for g in range(2):
                        pt = ptr_ring.next()
                        for j in range(8):
                            c = g * 8 + j
                            TRN(pt[:, j, :], O_t[:, c * 128:(c + 1) * 128], [O_t], [pt])
                        CP("act" if g == 0 else "dve", OT[:, g * 8:(g + 1) * 8, :], pt[:], [pt], [OT])
                    for g in range(4):
                        pm = pm_ring.next()
                        for c in range(16):
                            MM(pm[:], OT[:, c, :], wo[:, c, g * 512:(g + 1) * 512], c == 0, c == 15, [OT, wo], [pm])
                        TTO("dve", x1[:, s, g * 512:(g + 1) * 512], pm[:], x_t[:, g * 512:(g + 1) * 512], ALU.add,
                            [pm, x_t], [x1])
                P.barrier()
                dump("C", "x1C", x1[:], [128, NQ, D], F32)
                if stop == "C":
                    P.barrier()
                checkpoint("C")

            with ExitStack() as st:
                wq_c = alloc(st, "wqc", [128, 16, 512], BF16)
                wk_c = alloc(st, "wkc", [128, 16, 512], BF16)
                wv_c = alloc(st, "wvc", [128, 16, 512], BF16)
                wo_c = alloc(st, "woc", [128, 4, D], BF16)
                gc = load_g(st, "g_cross", g_cross, 16)
                gme = load_g(st, "g_mem", g_mem, 16)
                load_w(wk_c, wkc, 16, gme)
                load_w(wv_c, wvc, 16, gme)
                load_w(wq_c, wqc, 16, gc)
                load_w(wo_c, woc, 4)
                memT = alloc(st, "memT", [128, 16, 256], BF16)
                KcT = alloc(st, "KcT", [128, 4, 256], BF16)
                Vc = alloc(st, "Vc", [128, 2, 4, 129], BF16)
                MEMSET("pool", Vc[:, :, :, 128:129], 1.0, [Vc])
                xring = Ring(st, "xm", 2, [128, D], F32)
                junk = alloc(st, "junkD", [128, D], BF16)
                statr = Ring(st, "statD", 4, [128, 4], F32)
                xbr = Ring(st, "xbD", 1, [128, D], BF16)
                hTr = Ring(st, "hTD", 2, [128, 16, 128], BF16)
                ptr_ring = Ring(st, "ptrD", 2, [128, 8, 128], BF16, psum=True)
                pm_ring = Ring(st, "pmD", 2, [128, 512], F32, psum=True)
                pS_ring = Ring(st, "pSD", 2, [128, 512], F32, psum=True)
                pO_ring = Ring(st, "pOD", 2, [128, 512], F32, psum=True)
                QcT_r = Ring(st, "QcT", 2, [128, 4, 128], BF16)
                PT_ring = Ring(st, "PTD", 4, [128, 512], BF16)
                rs_ring = Ring(st, "rsD", 2, [128, 4], F32)
                Oc_r = Ring(st, "Oc_tok", 2, [128, 512], BF16)
                OcT_r = Ring(st, "OcT", 2, [128, 4, 128], BF16)
                for mt in range(2):
                    x_t = xring.next()
                    P.dma("sp", x_t[:], mem[mt * 128:(mt + 1) * 128, :], writes=[x_t])
                    norm_transpose(x_t[:], x_t, lambda g, mt=mt: memT[:, g * 8:(g + 1) * 8, mt * 128:(mt + 1) * 128], memT,
                                   junk, statr.next(), xbr.next(), ptr_ring)
                for h in range(4):
                    pm = pm_ring.next()
                    for c in range(16):
                        MM(pm[:, 0:256], wk_c[:, c, h * 128:(h + 1) * 128], memT[:, c, :], c == 0, c == 15, [wk_c, memT], [pm])
                    CP("act", KcT[:, h, :], pm[:, 0:256], [pm], [KcT])
                for mt in range(2):
                    pm = pm_ring.next()
                    for c in range(16):
                        MM(pm[:], memT[:, c, mt * 128:(mt + 1) * 128], wv_c[:, c, :], c == 0, c == 15, [memT, wv_c], [pm])
                    CP("dve", Vc[:, mt, :, 0:128], pm[:].rearrange("p (h d) -> p h d", h=4), [pm], [Vc])
                cscale = 128.0 ** -0.5
                dctx = {}
                x1d = [Buf("x1d%d" % s_) for s_ in range(NQ)]

                def d1(s):
                        hT = hTr.next()
                        norm_transpose(x1[:, s, :], x1d[s], lambda g, hT=hT: hT[:, g * 8:(g + 1) * 8, :], hT, junk, statr.next(),
                                       xbr.next(), ptr_ring)
                        pm = pm_ring.next()
                        for h in range(4):
                            for c in range(16):
                                MM(pm[:, h * 128:(h + 1) * 128], wq_c[:, c, h * 128:(h + 1) * 128], hT[:, c, :], c == 0, c == 15,
                                   [wq_c, hT], [pm])
                        QcT = QcT_r.next()
                        CP("act", QcT[:], pm[:].rearrange("p (h d) -> p h d", h=4), [pm], [QcT])
                        dctx[s] = QcT

                def d2(s):
                        QcT = dctx.pop(s)
                        PTs = []
                        for hp in range(2):
                            pS = pS_ring.next()
                            for hl in range(2):
                                h = hp * 2 + hl
                                for mt in range(2):
                                    cs = (hl * 2 + mt) * 128
                                    MM(pS[:, cs:cs + 128], KcT[:, h, mt * 128:(mt + 1) * 128], QcT[:, h, :], True, True, [KcT, QcT], [pS])
                            PT = PT_ring.next()
                            ACTV(PT[:], pS[:], AF.Exp, [pS], [PT], scale=cscale)
                            PTs.append(PT)
                        rsum = rs_ring.next()
                        Oc = Oc_r.next()
                        for hp in range(2):
                            pO = pO_ring.next()
                            PT = PTs[hp]
                            for hl in range(2):
                                h = hp * 2 + hl
                                for mt in range(2):
                                    cs = (hl * 2 + mt) * 128
                                    MM(pO[:, hl * 256:hl * 256 + 129], PT[:, cs:cs + 128], Vc[:, mt, h, :], mt == 0, mt == 1, [PT, Vc], [pO])
                            for hl in range(2):
                                h = hp * 2 + hl
                                P.op("dve", lambda e, o_=rsum[:, h:h + 1], i_=pO[:, hl * 256 + 128:hl * 256 + 129]: e.reciprocal(out=o_, in_=i_), [pO], [rsum])
                                ACTV(Oc[:, h * 128:(h + 1) * 128], pO[:, hl * 256:hl * 256 + 128], AF.Copy, [pO, rsum], [Oc], scale=rsum[:, h:h + 1])
                        OcT = OcT_r.next()
                        pt = ptr_ring.next()
                        for c in range(4):
                            TRN(pt[:, c, :], Oc[:, c * 128:(c + 1) * 128], [Oc], [pt])
                        CP("dve", OcT[:], pt[:, 0:4, :], [pt], [OcT])
                        for g in range(4):
                            pm = pm_ring.next()
                            for c in range(4):
                                MM(pm[:], OcT[:, c, :], wo_c[:, c, g * 512:(g + 1) * 512], c == 0, c == 3, [OcT, wo_c], [pm])
                            TTO("dve", x1[:, s, g * 512:(g + 1) * 512], pm[:], x1[:, s, g * 512:(g + 1) * 512], ALU.add,
                                [pm, x1d[s]], [x1d[s]])

                d1(0)
                for s in range(NQ):
                    if s + 1 < NQ:
                        d1(s + 1)
                    d2(s)
                P.barrier()
                dump("D", "x1D", x1[:], [128, NQ, D], F32)
                if stop == "D":
                    P.barrier()
                checkpoint("D")

            with ExitStack() as st:
                hmT = alloc(st, "hmT", [128, 16, NQ * 128], BF16)
                with ExitStack() as st2:
                    junk = alloc(st2, "junkE", [128, D], BF16)
                    statr = Ring(st2, "statE", 4, [128, 4], F32)
                    xbr = Ring(st2, "xbE", 2, [128, D], BF16)
                    ptr_ring = Ring(st2, "ptrE", 2, [128, 8, 128], BF16, psum=True)
                    for s in range(NQ):
                        norm_transpose(x1[:, s, :], x1, lambda g, s=s: hmT[:, g * 8:(g + 1) * 8, s * 128:(s + 1) * 128], hmT,
                                       junk, statr.next(), xbr.next(), ptr_ring)
                    P.barrier()
                with ExitStack() as st2:
                    gml = load_g(st2, "g_mlp", g_mlp, 16)
                    wu_ring = Ring(st2, "wu", 2, [128, 16, 512], BF16)
                    wd_ring = Ring(st2, "wd", 2, [128, 4, D], BF16)
                    aT_ring = Ring(st2, "aT", 2, [128, 4, NQ * 128], BF16)
                    rt_ring = Ring(st2, "rt", 2, [128, 512], F32)
                    et_ring = Ring(st2, "et", 2, [128, 512], F32)
                    x1s = [[Buf("x1_%d_%d" % (s_, g_)) for g_ in range(4)] for s_ in range(NQ)]
                    pu_ring = Ring(st2, "pu", 3, [128, 512], F32, psum=True)
                    pd_ring = Ring(st2, "pd", 4, [128, 512], F32, psum=True)
                    NCH = DFF // 512

                    def load_chunk(ch):
                        wu = wu_ring.next()
                        wd = wd_ring.next()
                        P.dma("pool", wu[:], w_up[:, ch * 512:(ch + 1) * 512].rearrange("(c p) n -> p c n", p=128), writes=[wu])
                        P.dma("pool", wd[:], w_down[ch * 512:(ch + 1) * 512, :].rearrange("(f p) n -> p f n", p=128), writes=[wd])
                        for c in range(16):
                            TS("dve", wu[:, c, :], wu[:, c, :], gml[:, c:c + 1], None, ALU.mult, None, [wu, gml], [wu])
                        return wu, wd

                    nxt = load_chunk(0)
                    for ch in range(NCH):
                        wu, wd = nxt
                        if ch + 1 < NCH:
                            nxt = load_chunk(ch + 1)
                        aT = aT_ring.next()
                        for fb in range(4):
                            for half in range(2):
                                pu = pu_ring.next()
                                ts_ = slice(half * 512, (half + 1) * 512)
                                for c in range(16):
                                    MM(pu[:], wu[:, c, fb * 128:(fb + 1) * 128], hmT[:, c, ts_], c == 0, c == 15, [wu, hmT], [pu])
                                rt = rt_ring.next()
                                ACTV(rt[:], pu[:], AF.Relu, [pu], [rt])
                                TTO("pool", aT[:, fb, ts_], rt[:], rt[:], ALU.mult, [rt], [aT])
                        for s in range(NQ):
                            for g in range(4):
                                pd = pd_ring.next()
                                for fb in range(4):
                                    MM(pd[:], aT[:, fb, s * 128:(s + 1) * 128], wd[:, fb, g * 512:(g + 1) * 512], fb == 0, fb == 3,
                                       [aT, wd], [pd])
                                xs_ = x1[:, s, g * 512:(g + 1) * 512]
                                if g % 2 == 0:
                                    TTO("dve", xs_, pd[:], xs_, ALU.add, [pd, x1s[s][g]], [x1s[s][g]])
                                else:
                                    et = et_ring.next()
                                    CP("act", et[:], pd[:], [pd], [et])
                                    TTO("pool", xs_, et[:], xs_, ALU.add, [et, x1s[s][g]], [x1s[s][g]])
                    P.barrier()
            with ExitStack() as st:
                gf = alloc(st, "g_fin", [128, D], F32)
                P.dma("sp", gf[:], g_fin, writes=[gf])
                junk = alloc(st, "junkF", [128, D], BF16)
                statr = Ring(st, "statF", 4, [128, 4], F32)
                y_ring = Ring(st, "y", 2, [128, D], F32)
                outs = []
                for s in range(NQ):
                    stat = statr.next()
                    rs = rstd_of(x1[:, s, :], D, [x1], junk, stat)
                    y = y_ring.next()
                    STT("dve", y[:], x1[:, s, :], rs, gf[:], ALU.mult, ALU.mult, [x1, stat, gf], [y])
                    outs.append(P.dma("sp", out[s * 128:(s + 1) * 128, :], y[:], reads=[y]))
        final = [o_ for o_ in outs if o_ is not None]
      except _Stop:
        final = []
      P.emit(nc, final_waits=list(final) + dump_ops)
    return nc


def _qtiles(r):
    return [r, 7 - r, 8 + r, 15 - r, 16 + r, 23 - r, 24 + r, 31 - r]


_NC_CACHE = {}
_PREP_ONLY = [False]


def kernel(x, mem, positions, g_mix, w_in, g_cq, g_ckv, w_qb, w_kvb, w_out, g_cross, g_mem, w_q_cross, w_k_cross,
           w_v_cross, w_o_cross, g_mlp, w_up, w_down, g_final):
    f32 = np.float32
    x = np.asarray(x, f32)
    mem = np.asarray(mem, f32)
    positions = np.asarray(positions, np.int32)
    w_in0 = np.asarray(w_in, f32)[0]
    o = np.cumsum([0, 512, 256, 64, 1024, 1024, 1024, 1024, 64, 16])
    c_q, c_kv, k_rope, q_d, k_d, v_d, q_idx, k_idx, w_idx = [w_in0[:, o[i]:o[i + 1]] for i in range(9)]
    wk_side = np.ascontiguousarray(np.concatenate([c_kv, k_rope, k_idx, k_d, v_d], axis=1))
    wq_side = np.ascontiguousarray(np.concatenate([c_q, q_idx, w_idx, q_d], axis=1))
    wqb0 = np.asarray(w_qb, f32)[0].reshape(512, 8, 192)
    wqb_r = np.ascontiguousarray(np.concatenate([wqb0[:, :, :128].reshape(512, 1024), wqb0[:, :, 128:].reshape(512, 512)], axis=1))

    def gl(g, k):
        return np.ascontiguousarray(np.asarray(g, f32).reshape(k, 128).T)

    common = {
        "invf": np.ascontiguousarray(np.broadcast_to((THETA ** (-np.arange(32, dtype=np.float64) / 32)).astype(f32), (128, 32))),
        "pow2": np.ascontiguousarray(np.broadcast_to((2.0 ** -(np.arange(NBIS) + 1.0)).astype(f32), (128, NBIS))),
        "ident": np.eye(128, dtype=f32),
        "wk_side": wk_side, "wq_side": wq_side,
        "g_mix": gl(np.asarray(g_mix)[0], 16),
        "w_qb": wqb_r, "g_cq": gl(np.asarray(g_cq)[0], 4),
        "w_kvb": np.ascontiguousarray(np.asarray(w_kvb, f32)[0]), "g_ckv": gl(np.asarray(g_ckv)[0], 2),
        "w_out": np.ascontiguousarray(np.asarray(w_out, f32)[0]),
        "g_cross": gl(np.asarray(g_cross)[0], 16), "g_mem": gl(np.asarray(g_mem)[0], 16),
        "wqc": np.ascontiguousarray(np.asarray(w_q_cross, f32)[0]),
        "wkc": np.ascontiguousarray(np.asarray(w_k_cross, f32)[0]),
        "wvc": np.ascontiguousarray(np.asarray(w_v_cross, f32)[0]),
        "woc": np.ascontiguousarray(np.asarray(w_o_cross, f32)[0]),
        "g_mlp": gl(np.asarray(g_mlp)[0], 16),
        "w_up": np.ascontiguousarray(np.asarray(w_up, f32)[0]),
        "w_down": np.ascontiguousarray(np.asarray(w_down, f32)[0]),
        "g_fin": np.ascontiguousarray(np.broadcast_to(np.asarray(g_final, f32).reshape(1, D), (128, D))),
    }
    in_maps = []
    for c in range(8):
        b, r = c // 4, c % 4
        qt = _qtiles(r)
        xq = np.ascontiguousarray(np.concatenate([x[b, t * 128:(t + 1) * 128] for t in qt], axis=0))
        posk = np.ascontiguousarray(positions[b].reshape(NT, 128).T)
        posq = np.ascontiguousarray(np.stack([positions[b, t * 128:(t + 1) * 128] for t in qt], axis=1))
        zmT = np.zeros((128, NQ, 512), f32)
        zbm = np.zeros((128, NQ, 512), f32)
        for s, t in enumerate(qt):
            qpos = t * 128 + np.arange(128)
            for j in range(4):
                kpos = (4 * s + j) * 128 + np.arange(128)
                allowed = (kpos[:, None] // 64) <= (qpos[None, :] // 64)
                zmT[:, s, j * 128:(j + 1) * 128] = allowed
                zbm[:, s, j * 128:(j + 1) * 128] = np.where(allowed.T, 0.0, -BIG)
        m = dict(common)
        m.update({"x_all": np.ascontiguousarray(x[b]), "xq": xq, "posk": posk, "posq": posq,
                  "mem": np.ascontiguousarray(mem[b]), "zmT": zmT, "zb": zbm})
        in_maps.append(m)
    if _PREP_ONLY[0]:
        return in_maps
    if "nc" not in _NC_CACHE:
        _NC_CACHE["nc"] = build_program()
    res = run_bass_kernel_spmd(_NC_CACHE["nc"], in_maps, core_ids=list(range(8)))
    outp = np.zeros((2, S, D), f32)
    for c in range(8):
        b, r = c // 4, c % 4
        o_c = np.asarray(res.results[c]["out"])
        for s, t in enumerate(_qtiles(r)):
            outp[b, t * 128:(t + 1) * 128] = o_c[s * 128:(s + 1) * 128]
    return outp
```

```python
import math
from contextlib import ExitStack

import numpy as np
import concourse.bass as bass
import concourse.mybir as mybir
from concourse.bass_utils import run_bass_kernel_spmd

F32 = mybir.dt.float32
BF16 = mybir.dt.bfloat16
I32 = mybir.dt.int32
AF = mybir.ActivationFunctionType
ALU = mybir.AluOpType
AX = mybir.AxisListType
PI = math.pi

D = 2048
S = 4096
NT = 32
NQ = 8
EPS = 1e-6
THETA = 500000.0
DFF = 8192
NBIS = 18
import os
KVV = int(os.environ.get('KVV', '0'))
BIS_POOL = tuple(int(c) for c in os.environ.get('BIS_POOL', '').split(',') if c != '')
BIG = 1.0e30

ENGS = ("pe", "act", "dve", "pool", "sp")


class Buf:
    __slots__ = ("name", "w", "r", "excl", "strict")

    def __init__(self, name=""):
        self.name = name
        self.w = None
        self.r = []
        self.excl = False
        self.strict = False


class TT:
    def __init__(self, t, name):
        self.t = t
        self.b = Buf(name)

    def __getitem__(self, k):
        return self.t[k]


def _b(x):
    return x.b if isinstance(x, TT) else x


class Op:
    __slots__ = ("eng", "fn", "dma", "deps", "signal", "tok", "slot_wait", "epoch")

    def __init__(self, eng, fn, dma):
        self.eng = eng
        self.fn = fn
        self.dma = dma
        self.deps = []
        self.signal = False
        self.tok = None
        self.slot_wait = None
        self.epoch = 0


class Prog:
    def __init__(self, nslots=6):
        self.ops = []
        self.nslots = nslots
        self.epoch = 0
        self.barriers = []
        self.stopped = False
        self.last = {}

    def op(self, eng, fn, reads=(), writes=(), dma=False):
        if self.stopped:
            return None
        o = Op(eng, fn, dma)
        o.epoch = self.epoch
        deps = {}
        reads = [_b(x) for x in reads]
        writes = [_b(x) for x in writes]
        writes = writes + [b for b in reads if b.excl and b not in writes]
        reads = [b for b in reads if not b.excl]
        for b in reads:
            if b.w is not None:
                deps[id(b.w)] = (b.w, True)
        for b in writes:
            if b.w is not None and id(b.w) not in deps:
                deps[id(b.w)] = (b.w, b.strict)
            for r in b.r:
                if id(r) not in deps:
                    deps[id(r)] = (r, b.strict)
        for d, raw in deps.values():
            if d.epoch != o.epoch:
                continue
            if (not d.dma) and (not dma) and d.eng == eng:
                if eng == "pe" or not raw:
                    continue
            d.signal = True
            o.deps.append(d)
        for b in map(_b, reads):
            if not dma:
                b.r = [r for r in b.r if r.dma or r.eng != eng or r.epoch != o.epoch]
            b.r.append(o)
        for b in map(_b, writes):
            b.w = o
            b.r = []
        self.ops.append(o)
        if not dma:
            self.last[eng] = o
        return o

    def barrier(self):
        if self.stopped:
            return
        for e, o in self.last.items():
            o.signal = True
        self.last = {}
        self.epoch += 1
        self.barriers.append(len(self.ops))

    def dma(self, q, out, in_, reads=(), writes=(), **kw):
        return self.op(q, lambda e: e.dma_start(out=out, in_=in_, **kw), reads, writes, dma=True)

    def emit(self, nc, final_waits=()):
        with ExitStack() as st:
            esem = {e: st.enter_context(nc.semaphore("s_" + e)) for e in ("pe", "act", "dve", "pool")}
            qsem = {q: [st.enter_context(nc.semaphore("d_%s%d" % (q, i))) for i in range(self.nslots)]
                    for q in ("sp", "act", "pool")}
            ecnt = {e: 0 for e in esem}
            qn = {q: 0 for q in qsem}
            qtot = {q: [0] * self.nslots for q in qsem}
            bar_toks = []
            nb = 0
            for i, o in enumerate(self.ops):
                while nb < len(self.barriers) and self.barriers[nb] == i:
                    toks = [(esem[e], ecnt[e]) for e in esem if ecnt[e] > 0]
                    for q in qsem:
                        for s_ in range(self.nslots):
                            if qtot[q][s_] > 0:
                                toks.append((qsem[q][s_], qtot[q][s_]))
                    bar_toks.append(toks)
                    nb += 1
                if o.dma:
                    q = o.eng
                    s_ = qn[q] % self.nslots
                    qn[q] += 1
                    if qtot[q][s_] > 0:
                        o.slot_wait = (qsem[q][s_], qtot[q][s_])
                    qtot[q][s_] += 16
                    o.tok = (qsem[q][s_], qtot[q][s_])
                elif o.signal:
                    ecnt[o.eng] += 1
                    o.tok = (esem[o.eng], ecnt[o.eng])
            while nb < len(self.barriers):
                bar_toks.append([])
                nb += 1
            per = {e: [o for o in self.ops if o.eng == e] for e in ENGS}
            block = st.enter_context(nc.Block())

            def run(e, eng):
                waited = {}
                cur_epoch = 0

                def w(tok):
                    sem, val = tok
                    k = id(sem)
                    if waited.get(k, 0) < val:
                        eng.wait_ge(sem, val)
                        waited[k] = val

                for o in per[e]:
                    while cur_epoch < o.epoch:
                        for t in bar_toks[cur_epoch]:
                            w(t)
                        cur_epoch += 1
                    if o.slot_wait is not None:
                        w(o.slot_wait)
                    for d in o.deps:
                        w(d.tok)
                    inst = o.fn(eng)
                    if o.dma:
                        inst.then_inc(o.tok[0], 16)
                    elif o.signal:
                        inst.then_inc(o.tok[0], 1)
                if e == "sp":
                    for o in final_waits:
                        w(o.tok)

            @block.tensor
            def _(eng):
                run("pe", eng)

            @block.scalar
            def _(eng):
                run("act", eng)

            @block.vector
            def _(eng):
                run("dve", eng)

            @block.gpsimd
            def _(eng):
                run("pool", eng)

            @block.sync
            def _(eng):
                run("sp", eng)


class _Stop(Exception):
    pass


def build_program(stop=None):
    nc = bass.Bass("TRN2", target_bir_lowering=False)
    dump_ops = []

    def dump(ph, name, ap, shape, dt):
        if stop != ph or P.stopped:
            return
        o_ = nc.dram_tensor("dbg_" + name, list(shape), dt, kind="ExternalOutput").ap()
        P.barrier()
        dump_ops.append(P.dma("sp", o_, ap))

    def checkpoint(name):
        if stop == name:
            P.stopped = True

    def din(name, shape, dt=F32):
        return nc.dram_tensor(name, list(shape), dt, kind="ExternalInput").ap()

    x_all = din("x_all", [S, D])
    xq = din("xq", [NQ * 128, D])
    posk = din("posk", [128, NT], I32)
    posq = din("posq", [128, NQ], I32)
    mem = din("mem", [256, D])
    invf = din("invf", [128, 32])
    pow2 = din("pow2", [128, NBIS])
    ident = din("ident", [128, 128])
    zmT = din("zmT", [128, NQ, 512])
    zb = din("zb", [128, NQ, 512])
    wk_side = din("wk_side", [D, 2432])
    wq_side = din("wq_side", [D, 2576])
    g_mix = din("g_mix", [128, 16])
    w_qb = din("w_qb", [512, 1536])
    g_cq = din("g_cq", [128, 4])
    w_kvb = din("w_kvb", [256, 2048])
    g_ckv = din("g_ckv", [128, 2])
    w_out = din("w_out", [D, D])
    g_cross = din("g_cross", [128, 16])
    g_mem = din("g_mem", [128, 16])
    wqc = din("wqc", [D, 512])
    wkc = din("wkc", [D, 512])
    wvc = din("wvc", [D, 512])
    woc = din("woc", [512, D])
    g_mlp = din("g_mlp", [128, 16])
    w_up = din("w_up", [D, DFF])
    w_down = din("w_down", [DFF, D])
    g_fin = din("g_fin", [128, D])
    out = nc.dram_tensor("out", [NQ * 128, D], F32, kind="ExternalOutput").ap()

    def dscr(name, shape):
        return nc.dram_tensor(name, list(shape), BF16, kind="Internal").ap()

    KnT_scr = dscr("KnT_scr", [128, 8, S])
    KdT_scr = dscr("KdT_scr", [128, 8, S])
    Vm_scr = dscr("Vm_scr", [8, 128, NT, 129])
    Vd_scr = dscr("Vd_scr", [8, 128, NT, 129])
    maskT_scr = dscr("maskT_scr", [128, 144, 128])
    O_scr = dscr("O_scr", [NQ * 128, D])

    P = Prog()
    cnt = [0]

    def MM(out_, lhsT, rhs, start, stop, reads, writes):
        P.op("pe", lambda e: e.matmul(out_, lhsT=lhsT, rhs=rhs, start=start, stop=stop), reads, writes)

    def ACTV(out_, in_, func, reads, writes, **kw):
        P.op("act", lambda e: e.activation(out=out_, in_=in_, func=func, **kw), reads, writes)

    def TS(eng, out_, in0, s1, s2, op0, op1, reads, writes, accum_out=None):
        if op1 is None:
            P.op(eng, lambda e: e.tensor_scalar(out=out_, in0=in0, scalar1=s1, scalar2=None, op0=op0), reads, writes)
        elif accum_out is None:
            P.op(eng, lambda e: e.tensor_scalar(out=out_, in0=in0, scalar1=s1, scalar2=s2, op0=op0, op1=op1), reads, writes)
        else:
            P.op(eng, lambda e: e.tensor_scalar(out=out_, in0=in0, scalar1=s1, scalar2=s2, op0=op0, op1=op1,
                                                accum_out=accum_out), reads, writes)

    def TTO(eng, out_, in0, in1, op, reads, writes):
        P.op(eng, lambda e: e.tensor_tensor(out=out_, in0=in0, in1=in1, op=op), reads, writes)

    def STT(eng, out_, in0, scalar, in1, op0, op1, reads, writes):
        P.op(eng, lambda e: e.scalar_tensor_tensor(out=out_, in0=in0, scalar=scalar, in1=in1, op0=op0, op1=op1),
             reads, writes)

    def CP(eng, out_, in_, reads, writes):
        if eng == "act":
            ACTV(out_, in_, AF.Copy, reads, writes)
        else:
            P.op(eng, lambda e: e.tensor_copy(out=out_, in_=in_), reads, writes)

    def MEMSET(eng, ap, val, writes):
        P.op(eng, lambda e: e.memset(ap, val), (), writes)

    with ExitStack() as top:
      try:
        def alloc(st, name, shape, dt):
            cnt[0] += 1
            t_ = TT(st.enter_context(nc.sbuf_tensor("%s_%d" % (name, cnt[0]), list(shape), dt)), name)
            t_.b.strict = name.startswith("junk")
            return t_

        def palloc(st, name, shape, dt):
            cnt[0] += 1
            t_ = TT(st.enter_context(nc.psum_tensor("%s_%d" % (name, cnt[0]), list(shape), dt)), name)
            t_.b.excl = True
            return t_

        class Ring:
            def __init__(self, st, name, n, shape, dt, psum=False):
                self.items = [(palloc if psum else alloc)(st, "%s%d" % (name, i), shape, dt) for i in range(n)]
                self.i = 0

            def next(self):
                t = self.items[self.i % len(self.items)]
                self.i += 1
                return t

        def TRN(out_, in_, reads, writes):
            P.op("pe", lambda e: e.transpose(out=out_, in_=in_, identity=identb[:]), list(reads) + [identb], writes)

        identf = alloc(top, "identf", [128, 128], F32)
        identb = alloc(top, "identb", [128, 128], BF16)
        invf_t = alloc(top, "invf", [128, 32], F32)
        pow2_t = alloc(top, "pow2", [128, NBIS], F32)
        mhalf = alloc(top, "mhalf", [128, 1], F32)
        stK = ExitStack()
        kropeT = alloc(stK, "kropeT", [128, S], BF16)
        kidx_lo = alloc(stK, "kidx_lo", [128, S], BF16)
        kidx_hi = alloc(stK, "kidx_hi", [128, S], BF16)

        P.dma("sp", identf[:], ident, writes=[identf])
        P.dma("sp", invf_t[:], invf, writes=[invf_t])
        P.dma("sp", pow2_t[:], pow2, writes=[pow2_t])
        CP("dve", identb[:], identf[:], [identf], [identb])
        MEMSET("pool", mhalf[:], -0.5, [mhalf])
        MEMSET("pool", kidx_lo[:], 0.0, [kidx_lo])
        MEMSET("pool", kidx_hi[:], 0.0, [kidx_hi])

        def make_tables(pos_dram, n, cos_t, sin_t):
            with ExitStack() as st:
                posi = alloc(st, "posi", [128, n], I32)
                posf = alloc(st, "posf", [128, n], F32)
                ang = alloc(st, "ang", [128, n, 32], F32)
                a2 = alloc(st, "a2", [128, n, 32], F32)
                tp = alloc(st, "tp", [128, n, 32], F32)
                ki = alloc(st, "ki", [128, n, 32], I32)
                P.dma("sp", posi[:], pos_dram, writes=[posi])
                CP("dve", posf[:], posi[:], [posi], [posf])
                for t in range(n):
                    TS("dve", ang[:, t, :], invf_t[:], posf[:, t:t + 1], None, ALU.mult, None, [invf_t, posf], [ang])
                HI = 6.28125
                LO = 2 * PI - 6.28125
                for (dst, shift) in ((sin_t, 0.0), (cos_t, PI / 2)):
                    TS("dve", a2[:], ang[:], shift, None, ALU.add, None, [ang], [a2])
                    TS("dve", tp[:], a2[:], 1.0 / (2 * PI), None, ALU.mult, None, [a2], [tp])
                    CP("dve", ki[:], tp[:], [tp], [ki])
                    CP("dve", tp[:], ki[:], [ki], [tp])
                    STT("dve", a2[:], tp[:], -HI, a2[:], ALU.mult, ALU.add, [tp, a2], [a2])
                    STT("dve", a2[:], tp[:], -LO, a2[:], ALU.mult, ALU.add, [tp, a2], [a2])
                    TS("dve", tp[:], a2[:], PI, None, ALU.is_gt, None, [a2], [tp])
                    STT("dve", a2[:], tp[:], -2 * PI, a2[:], ALU.mult, ALU.add, [tp, a2], [a2])
                    TS("dve", tp[:], a2[:], -PI, None, ALU.is_lt, None, [a2], [tp])
                    STT("dve", a2[:], tp[:], 2 * PI, a2[:], ALU.mult, ALU.add, [tp, a2], [a2])
                    ACTV(dst[:], a2[:], AF.Sin, [a2], [dst])
                P.barrier()

        def rope(src, dst, H, half, cos_ap, sin_ap, tmp, reads, writes):
            cb = cos_ap.to_broadcast([128, H, half])
            sb_ = sin_ap.to_broadcast([128, H, half])
            n = H * half
            ta = tmp[:, 0, 0:n].rearrange("p (h d) -> p h d", h=H)
            tb = tmp[:, 1, 0:n].rearrange("p (h d) -> p h d", h=H)
            tc_ = tmp[:, 2, 0:n].rearrange("p (h d) -> p h d", h=H)
            td = tmp[:, 3, 0:n].rearrange("p (h d) -> p h d", h=H)
            x1_ = src[:, :, 0:half]
            x2_ = src[:, :, half:2 * half]
            TTO("dve", ta, x1_, cb, ALU.mult, reads, [tmp])
            TTO("dve", tb, x2_, sb_, ALU.mult, reads, [tmp])
            TTO("dve", tc_, x2_, cb, ALU.mult, reads, [tmp])
            TTO("dve", td, x1_, sb_, ALU.mult, reads, [tmp])
            TTO("pool", dst[:, :, 0:half], ta, tb, ALU.subtract, [tmp], writes)
            TTO("pool", dst[:, :, half:2 * half], tc_, td, ALU.add, [tmp], writes)

        def rstd_of(src_ap, n, reads, junk, stat):
            ACTV(junk[:, 0:n], src_ap, AF.Square, reads, [junk, stat], accum_out=stat[:, 0:1])
            TS("dve", stat[:, 1:2], stat[:, 0:1], 1.0 / n, EPS, ALU.mult, ALU.add, [stat], [stat])
            TTO("pool", stat[:, 2:3], stat[:, 1:2], mhalf[:], ALU.pow, [stat, mhalf], [stat])
            return stat[:, 2:3]

        def load_w(dst, src, kc, g_t=None, eng_fold="dve"):
            for c in range(kc):
                P.dma("pool", dst[:, c, :], src[c * 128:(c + 1) * 128, :], writes=[dst])
            if g_t is not None:
                for c in range(kc):
                    TS(eng_fold, dst[:, c, :], dst[:, c, :], g_t[:, c:c + 1], None, ALU.mult, None, [dst, g_t], [dst])

        def load_g(st, name, src, k):
            t = alloc(st, name, [128, k], F32)
            P.dma("sp", t[:], src, writes=[t])
            return t

        def norm_transpose(x_ap, x_tt, dstf, dst_tt, junk, stat, xb, ptr_ring):
            rs = rstd_of(x_ap, D, [x_tt], junk, stat)
            TS("dve", xb[:], x_ap, rs, None, ALU.mult, None, [x_tt, stat], [xb])
            for g in range(2):
                pt = ptr_ring.next()
                for j in range(8):
                    c = g * 8 + j
                    TRN(pt[:, j, :], xb[:, c * 128:(c + 1) * 128], [xb], [pt])
                CP("act" if g == 0 else "dve", dstf(g), pt[:], [pt], [dst_tt])

        with ExitStack() as st:
            cosK = alloc(st, "cosK", [128, NT, 32], F32)
            sinK = alloc(st, "sinK", [128, NT, 32], F32)
            make_tables(posk, NT, cosK, sinK)
            dump("T", "cosK", cosK[:], [128, NT, 32], F32)
            dump("T", "sinK", sinK[:], [128, NT, 32], F32)
            checkpoint("T")
            wk = alloc(st, "wk", [128, 16, 2432], BF16)
            wkv = alloc(st, "wkv", [128, 2, 2048], BF16)
            gm = load_g(st, "g_mix", g_mix, 16)
            gk = load_g(st, "g_ckv", g_ckv, 2)
            load_w(wk, wk_side, 16, gm)
            load_w(wkv, w_kvb, 2, gk)
            xring = Ring(st, "xa", 2, [128, D], F32)
            junk = alloc(st, "junkA", [128, 256], BF16)
            statr = Ring(st, "statA", 6, [128, 4], F32)
            xbr = Ring(st, "xbA", 2, [128, D], BF16)
            hTr = Ring(st, "hTA", 2, [128, 16, 128], BF16)
            ptr_ring = Ring(st, "ptrA", 2, [128, 8, 128], BF16, psum=True)
            pm_ring = Ring(st, "pmA", 6, [128, 512], F32, psum=True)
            tmp_ring = Ring(st, "ropetmp", 2, [128, 4, 256], F32)
            ckvn_r = Ring(st, "ckvn", 2, [128, 256], BF16)
            ckvT_r = Ring(st, "ckvT", 2, [128, 2, 128], BF16)
            kr_tok_r = Ring(st, "kr_tok", 2, [128, 128], BF16)
            ki_tok_r = Ring(st, "ki_tok", 2, [128, 128], BF16)
            kd_tok_r = Ring(st, "kd_tok", 2, [128, 8, 128], BF16)
            kn_tok_r = Ring(st, "kn_tok", 2, [128, 8, 128], BF16)
            KnT_st_r = Ring(st, "KnT_st", 1, [128, 8, 512], BF16)
            KdT_st_r = Ring(st, "KdT_st", 1, [128, 8, 512], BF16)
            Vm_st_r = Ring(st, "Vm_st", 1, [128, 8, 4, 129], BF16)
            Vd_st_r = Ring(st, "Vd_st", 1, [128, 8, 4, 129], BF16)
            for rg in (Vm_st_r, Vd_st_r):
                for it in rg.items:
                    MEMSET("pool", it[:, :, :, 128:129], 1.0, [it])
            dump("W", "wk", wk[:, :, 0:512], [128, 16, 512], BF16)
            dump("W", "wkv", wkv[:, :, 0:512], [128, 2, 512], BF16)
            checkpoint("W")

            Vd_groups = {}
            ctx = {}

            def stage0(t):
                x_t = xring.next()
                P.dma("sp", x_t[:], x_all[t * 128:(t + 1) * 128, :], writes=[x_t])
                xb = xbr.next()
                stat = statr.next()
                rs = rstd_of(x_t[:], D, [x_t], xb, stat)
                TS("dve", xb[:], x_t[:], rs, None, ALU.mult, None, [x_t, stat], [xb])
                ctx[("xb", t)] = xb

            def stage1(t):
                tl = t % 4
                g4 = t // 4
                if tl == 0:
                    Vd_groups[g4] = Vd_st_r.next()
                Vd_st = Vd_groups[g4]
                xb = ctx.pop(("xb", t))
                hT = hTr.next()
                for g in range(2):
                    pt = ptr_ring.next()
                    for j in range(8):
                        c = g * 8 + j
                        TRN(pt[:, j, :], xb[:, c * 128:(c + 1) * 128], [xb], [pt])
                    CP("act" if g == 0 else "dve", hT[:, g * 8:(g + 1) * 8, :], pt[:], [pt], [hT])
                pms = []
                for g, (c0, n) in enumerate(((0, 384), (384, 512), (896, 512), (1408, 512), (1920, 512))):
                    pm = pm_ring.next()
                    for c in range(16):
                        MM(pm[:, 0:n], hT[:, c, :], wk[:, c, c0:c0 + n], c == 0, c == 15, [hT, wk], [pm])
                    pms.append(pm)
                p0 = pms[0]
                stat = statr.next()
                rs = rstd_of(p0[:, 0:256], 256, [p0], junk, stat)
                ckvn = ckvn_r.next()
                TS("dve", ckvn[:], p0[:, 0:256], rs, None, ALU.mult, None, [p0, stat], [ckvn])
                tmp = tmp_ring.next()
                kr_tok = kr_tok_r.next()
                rope(p0[:, 256:320].rearrange("p (h d) -> p h d", h=1), kr_tok[:, 0:64].rearrange("p (h d) -> p h d", h=1),
                     1, 32, cosK[:, t:t + 1, :], sinK[:, t:t + 1, :], tmp, [p0, cosK, sinK], [kr_tok])
                CP("pool", kr_tok[:, 64:128], kr_tok[:, 0:64], [kr_tok], [kr_tok])
                tmp = tmp_ring.next()
                ki_tok = ki_tok_r.next()
                rope(p0[:, 320:384].rearrange("p (h d) -> p h d", h=1), ki_tok[:, 0:64].rearrange("p (h d) -> p h d", h=1),
                     1, 8, cosK[:, t:t + 1, 0:32:4], sinK[:, t:t + 1, 0:32:4], tmp, [p0, cosK, sinK], [ki_tok])
                CP("act", ki_tok[:, 16:64], p0[:, 336:384], [p0], [ki_tok])
                CP("pool", ki_tok[:, 64:128], ki_tok[:, 0:64], [ki_tok], [ki_tok])
                kd_tok = kd_tok_r.next()
                for g in range(2):
                    pm = pms[1 + g]
                    tmp = tmp_ring.next()
                    src = pm[:].rearrange("p (h d) -> p h d", h=4)
                    dst = kd_tok[:, g * 4:(g + 1) * 4, :]
                    rope(src, dst, 4, 16, cosK[:, t:t + 1, 0:32:2], sinK[:, t:t + 1, 0:32:2], tmp, [pm, cosK, sinK], [kd_tok])
                    CP("act", dst[:, :, 32:128], src[:, :, 32:128], [pm], [kd_tok])
                for g in range(2):
                    pm = pms[3 + g]
                    CP("act" if g == 0 else "dve", Vd_st[:, g * 4:(g + 1) * 4, tl, 0:128],
                       pm[:].rearrange("p (h d) -> p h d", h=4), [pm], [Vd_st])
                if tl == 3:
                    P.dma("sp", Vd_scr[:, :, g4 * 4:(g4 + 1) * 4, :].rearrange("h p t c -> p h t c"), Vd_st[:],
                          reads=[Vd_st])
                ctx[t] = (ckvn, kr_tok, ki_tok, kd_tok, Vd_st)

            ctx2, ctx3 = {}, {}

            def s2a(t):
                tl = t % 4
                ckvn, kr_tok, ki_tok, kd_tok, Vd_st = ctx[t]
                KdT_st = KdT_st_r.items[0]
                ckvT = ckvT_r.next()
                pt = ptr_ring.next()
                for c in range(2):
                    TRN(pt[:, c, :], ckvn[:, c * 128:(c + 1) * 128], [ckvn], [pt])
                CP("dve", ckvT[:], pt[:, 0:2, :], [pt], [ckvT])
                pt = ptr_ring.next()
                for h in range(8):
                    TRN(pt[:, h, :], kd_tok[:, h, :], [kd_tok], [pt])
                CP("act", KdT_st[:, :, tl * 128:(tl + 1) * 128], pt[:], [pt], [KdT_st])
                pt = ptr_ring.next()
                TRN(pt[:, 0, :], kr_tok[:], [kr_tok], [pt])
                TRN(pt[:, 1, :], ki_tok[:], [ki_tok], [pt])
                CP("act", kropeT[:, t * 128:(t + 1) * 128], pt[:, 0, :], [pt], [kropeT])
                CP("dve", kidx_lo[0:64, t * 128:(t + 1) * 128], pt[0:64, 1, :], [pt], [kidx_lo])
                CP("dve", kidx_hi[64:128, t * 128:(t + 1) * 128], pt[64:128, 1, :], [pt], [kidx_hi])
                if tl == 3:
                    g4 = t // 4
                    P.dma("sp", KdT_scr[:, :, g4 * 512:(g4 + 1) * 512], KdT_st[:], reads=[KdT_st])
                ctx2[t] = ckvT

            def s2b(t):
                tl = t % 4
                ckvn, kr_tok, ki_tok, kd_tok, Vd_st = ctx.pop(t)
                ckvT = ctx2.pop(t)
                Vm_st = Vm_st_r.items[0]
                kn_tok = kn_tok_r.next()
                for g in range(4):
                    pm = pm_ring.next()
                    for c in range(2):
                        MM(pm[:], ckvT[:, c, :], wkv[:, c, g * 512:(g + 1) * 512], c == 0, c == 1, [ckvT, wkv], [pm])
                    v4 = pm[:].rearrange("p (h e d) -> p h e d", h=2, e=2)
                    CP("act", kn_tok[:, 2 * g:2 * g + 2, :], v4[:, :, 0, :], [pm], [kn_tok])
                    CP("dve", Vm_st[:, 2 * g:2 * g + 2, tl, 0:128], v4[:, :, 1, :], [pm], [Vm_st])
                if tl == 3:
                    g4 = t // 4
                    P.dma("sp", Vm_scr[:, :, g4 * 4:(g4 + 1) * 4, :].rearrange("h p t c -> p h t c"), Vm_st[:],
                          reads=[Vm_st])
                ctx3[t] = kn_tok

            def s2c(t):
                tl = t % 4
                kn_tok = ctx3.pop(t)
                KnT_st = KnT_st_r.items[0]
                pt = ptr_ring.next()
                for h in range(8):
                    TRN(pt[:, h, :], kn_tok[:, h, :], [kn_tok], [pt])
                CP("dve", KnT_st[:, :, tl * 128:(tl + 1) * 128], pt[:], [pt], [KnT_st])
                if tl == 3:
                    g4 = t // 4
                    P.dma("sp", KnT_scr[:, :, g4 * 512:(g4 + 1) * 512], KnT_st[:], reads=[KnT_st])

            stage0(0)
            stage0(1)
            stage1(0)
            for t in range(NT):
                if t + 2 < NT:
                    stage0(t + 2)
                s2a(t)
                if t + 1 < NT:
                    stage1(t + 1)
                s2b(t)
                if t >= 1:
                    s2c(t - 1)
            s2c(NT - 1)
            P.barrier()
            dump("A", "kropeT", kropeT[:], [128, S], BF16)
            dump("A", "KnT", KnT_scr, [128, 8, S], BF16)
            dump("A", "KdT", KdT_scr, [128, 8, S], BF16)
            dump("A", "Vm", Vm_scr, [8, 128, NT, 129], BF16)
            dump("A", "Vd", Vd_scr, [8, 128, NT, 129], BF16)
            if stop == "A":
                P.barrier()
            checkpoint("A")

        with ExitStack() as stB:
            QnT = alloc(stB, "QnT", [128, 8, NQ * 128], BF16)
            QrT_lo = alloc(stB, "QrT_lo", [128, 4, NQ * 128], BF16)
            QrT_hi = alloc(stB, "QrT_hi", [128, 4, NQ * 128], BF16)
            MEMSET("pool", QrT_lo[:], 0.0, [QrT_lo])
            MEMSET("pool", QrT_hi[:], 0.0, [QrT_hi])
            QdT = alloc(stB, "QdT", [128, 8, NQ * 128], BF16)
            with ExitStack() as stI:
                qiT = alloc(stI, "qiT", [128, 8, NQ * 128], BF16)
                wsc = alloc(stI, "wsc", [128, NQ, 16], F32)
                cosQ = alloc(stI, "cosQ", [128, NQ, 32], F32)
                sinQ = alloc(stI, "sinQ", [128, NQ, 32], F32)
                make_tables(posq, NQ, cosQ, sinQ)

                def qside(pass_id):
                    with ExitStack() as st:
                        ncol = 1552 if pass_id == 0 else 1024
                        col0 = 0 if pass_id == 0 else 1552
                        wq = alloc(st, "wq", [128, 16, ncol], BF16)
                        gm = load_g(st, "g_mix2", g_mix, 16)
                        for c in range(16):
                            P.dma("pool", wq[:, c, :], wq_side[c * 128:(c + 1) * 128, col0:col0 + ncol], writes=[wq])
                        for c in range(16):
                            TS("dve", wq[:, c, :], wq[:, c, :], gm[:, c:c + 1], None, ALU.mult, None, [wq, gm], [wq])
                        if pass_id == 0:
                            wqb = alloc(st, "wqb", [128, 4, 1536], BF16)
                            gq = load_g(st, "g_cq", g_cq, 4)
                            load_w(wqb, w_qb, 4, gq)
                        xring = Ring(st, "xb1", 2, [128, D], F32)
                        junk = alloc(st, "junkB", [128, 512], BF16)
                        statr = Ring(st, "statB", 6, [128, 4], F32)
                        xbr = Ring(st, "xbB", 2, [128, D], BF16)
                        hTr = Ring(st, "hTB", 2, [128, 16, 128], BF16)
                        ptr_ring = Ring(st, "ptrB", 2, [128, 8, 128], BF16, psum=True)
                        pm_ring = Ring(st, "pmB", 6, [128, 512], F32, psum=True)
                        tmp_ring = Ring(st, "ropetmpB", 2, [128, 4, 256], F32)
                        if pass_id == 0:
                            cqn_r = Ring(st, "cqn", 2, [128, 512], BF16)
                            cqT_r = Ring(st, "cqT", 2, [128, 4, 128], BF16)
                            qn_tok_r = Ring(st, "qn_tok", 2, [128, 8, 128], BF16)
                            qr_tok_r = Ring(st, "qr_tok", 2, [128, 8, 64], BF16)
                            qi_tok_r = Ring(st, "qi_tok", 2, [128, 16, 64], BF16)
                        else:
                            qd_tok_r = Ring(st, "qd_tok", 2, [128, 8, 128], BF16)
                        qctx = {}

                        def q0(s):
                            x_t = xring.next()
                            P.dma("sp", x_t[:], xq[s * 128:(s + 1) * 128, :], writes=[x_t])
                            xb = xbr.next()
                            stat = statr.next()
                            rs = rstd_of(x_t[:], D, [x_t], xb, stat)
                            TS("dve", xb[:], x_t[:], rs, None, ALU.mult, None, [x_t, stat], [xb])
                            qctx[s] = xb

                        q0(0)
                        for s in range(NQ):
                            qs = slice(s * 128, (s + 1) * 128)
                            if s + 1 < NQ:
                                q0(s + 1)
                            xb = qctx.pop(s)
                            hT = hTr.next()
                            for g in range(2):
                                pt = ptr_ring.next()
                                for j in range(8):
                                    c = g * 8 + j
                                    TRN(pt[:, j, :], xb[:, c * 128:(c + 1) * 128], [xb], [pt])
                                CP("act" if g == 0 else "dve", hT[:, g * 8:(g + 1) * 8, :], pt[:], [pt], [hT])
                            if pass_id == 0:
                                pms = []
                                for g, (c0, n) in enumerate(((0, 512), (512, 512), (1024, 512), (1536, 16))):
                                    pm = pm_ring.next()
                                    for c in range(16):
                                        MM(pm[:, 0:n], hT[:, c, :], wq[:, c, c0:c0 + n], c == 0, c == 15, [hT, wq], [pm])
                                    pms.append(pm)
                                TS("dve", wsc[:, s, :], pms[3][:, 0:16], 1.0 / 32.0, None, ALU.mult, None, [pms[3]], [wsc])
                                stat = statr.next()
                                rs = rstd_of(pms[0][:], 512, [pms[0]], junk, stat)
                                cqn = cqn_r.next()
                                TS("dve", cqn[:], pms[0][:], rs, None, ALU.mult, None, [pms[0], stat], [cqn])
                                qi_tok = qi_tok_r.next()
                                for g in range(2):
                                    pm = pms[1 + g]
                                    tmp = tmp_ring.next()
                                    src = pm[:].rearrange("p (h d) -> p h d", h=8)
                                    dst = qi_tok[:, g * 8:(g + 1) * 8, :]
                                    rope(src, dst, 8, 8, cosQ[:, s:s + 1, 0:32:4], sinQ[:, s:s + 1, 0:32:4], tmp,
                                         [pm, cosQ, sinQ], [qi_tok])
                                    CP("act", dst[:, :, 16:64], src[:, :, 16:64], [pm], [qi_tok])
                                cqT = cqT_r.next()
                                pt = ptr_ring.next()
                                for c in range(4):
                                    TRN(pt[:, c, :], cqn[:, c * 128:(c + 1) * 128], [cqn], [pt])
                                CP("dve", cqT[:], pt[:, 0:4, :], [pt], [cqT])
                                qn_tok = qn_tok_r.next()
                                qr_tok = qr_tok_r.next()
                                for g in range(3):
                                    pm = pm_ring.next()
                                    for c in range(4):
                                        MM(pm[:], cqT[:, c, :], wqb[:, c, g * 512:(g + 1) * 512], c == 0, c == 3, [cqT, wqb], [pm])
                                    if g < 2:
                                        CP("act", qn_tok[:, g * 4:(g + 1) * 4, :], pm[:].rearrange("p (h d) -> p h d", h=4),
                                           [pm], [qn_tok])
                                    else:
                                        tmp = tmp_ring.next()
                                        rope(pm[:].rearrange("p (h d) -> p h d", h=8), qr_tok[:], 8, 32,
                                             cosQ[:, s:s + 1, :], sinQ[:, s:s + 1, :], tmp, [pm, cosQ, sinQ], [qr_tok])
                                pt = ptr_ring.next()
                                for h in range(8):
                                    TRN(pt[:, h, :], qn_tok[:, h, :], [qn_tok], [pt])
                                CP("act", QnT[:, :, qs], pt[:], [pt], [QnT])
                                pt = ptr_ring.next()
                                qr2 = qr_tok[:].rearrange("p (a b) d -> p a (b d)", b=2)
                                for h2 in range(4):
                                    TRN(pt[:, h2, :], qr2[:, h2, :], [qr_tok], [pt])
                                CP("dve", QrT_lo[0:64, :, qs], pt[0:64, 0:4, :], [pt], [QrT_lo])
                                CP("dve", QrT_hi[64:128, :, qs], pt[64:128, 0:4, :], [pt], [QrT_hi])
                                pt = ptr_ring.next()
                                qi2 = qi_tok[:].rearrange("p (a b) d -> p a (b d)", b=2)
                                for h2 in range(8):
                                    TRN(pt[:, h2, :], qi2[:, h2, :], [qi_tok], [pt])
                                CP("act", qiT[:, :, qs], pt[:], [pt], [qiT])
                            else:
                                pms = []
                                for g in range(2):
                                    pm = pm_ring.next()
                                    for c in range(16):
                                        MM(pm[:], hT[:, c, :], wq[:, c, g * 512:(g + 1) * 512], c == 0, c == 15, [hT, wq], [pm])
                                    pms.append(pm)
                                qd_tok = qd_tok_r.next()
                                for g in range(2):
                                    pm = pms[g]
                                    tmp = tmp_ring.next()
                                    src = pm[:].rearrange("p (h d) -> p h d", h=4)
                                    dst = qd_tok[:, g * 4:(g + 1) * 4, :]
                                    rope(src, dst, 4, 16, cosQ[:, s:s + 1, 0:32:2], sinQ[:, s:s + 1, 0:32:2], tmp,
                                         [pm, cosQ, sinQ], [qd_tok])
                                    CP("act", dst[:, :, 32:128], src[:, :, 32:128], [pm], [qd_tok])
                                pt = ptr_ring.next()
                                for h in range(8):
                                    TRN(pt[:, h, :], qd_tok[:, h, :], [qd_tok], [pt])
                                CP("dve", QdT[:, :, qs], pt[:], [pt], [QdT])
                        P.barrier()

                qside(0)
                qside(1)
                dump("B1", "QnT", QnT[:], [128, 8, NQ * 128], BF16)
                dump("B1", "QdT", QdT[:], [128, 8, NQ * 128], BF16)
                dump("B1", "qiT", qiT[:], [128, 8, NQ * 128], BF16)
                dump("B1", "wsc", wsc[:], [128, NQ, 16], F32)
                if stop == "B1":
                    P.barrier()
                checkpoint("B1")

                with ExitStack() as st:
                    zb_ring = Ring(st, "zb", 4, [128, 512], F32)
                    pz_ring = Ring(st, "pz", 3, [128, 512], F32, psum=True)
                    psc_ring = Ring(st, "psc", 2, [128, 512], F32, psum=True)
                    ptr_ring = Ring(st, "ptrI", 2, [128, 8, 128], BF16, psum=True)
                    r_ring = Ring(st, "relu", 4, [128, 512], BF16)
                    dg_ring = Ring(st, "dg", 3, [128, 16, 128], BF16)
                    sc_ring = Ring(st, "score", 4, [128, S], F32)
                    junkI_d = alloc(st, "junkI", [128, S], mybir.dt.uint8)
                    junkI_p = alloc(st, "junkIp", [128, S], mybir.dt.uint8)
                    mask_ring = Ring(st, "mask", 1, [128, S], BF16)
                    mT_ring = Ring(st, "mTst", 1, [128, NT, 128], BF16)
                    bs_ring = Ring(st, "bis", 4, [128, 8 + NBIS], F32)
                    blk_of = [0]
                    for s_ in range(NQ):
                        blk_of.append(blk_of[-1] + 4 * (s_ + 1))

                    def front(s):
                        E = 4 * (s + 1)
                        n = E * 128
                        qs = slice(s * 128, (s + 1) * 128)
                        zb_t = zb_ring.next()
                        P.dma("sp", zb_t[:], zb[:, s, :], writes=[zb_t])
                        dg = dg_ring.next()
                        for h in range(16):
                            TS("pool", dg[:, h, :], identb[:], wsc[:, s, h:h + 1], None, ALU.mult, None, [identb, wsc], [dg])
                        score = sc_ring.next()
                        for kb in range(s + 1):
                            ks = slice(kb * 512, (kb + 1) * 512)
                            psc = psc_ring.next()
                            rprev = None
                            for h in range(16):
                                p0_ = (h % 2) * 64
                                pz = pz_ring.next()
                                kix = kidx_lo if h % 2 == 0 else kidx_hi
                                MM(pz[:], qiT[:, h // 2, qs], kix[:, ks], True, True, [qiT, kix], [pz])
                                rb = r_ring.next()
                                ACTV(rb[:], pz[:], AF.Relu, [pz], [rb])
                                if rprev is not None:
                                    MM(psc[:], dg[:, h - 1, :], rprev[:], h - 1 == 0, False, [dg, rprev], [psc])
                                rprev = rb
                            MM(psc[:], dg[:, 15, :], rprev[:], False, True, [dg, rprev], [psc])
                            CP("act", score[:, ks], psc[:], [psc], [score])
                        return score, zb_t

                    def bis(s, score, zb_t, junkI, bs):
                        n = 4 * (s + 1) * 128
                        P.op("dve", lambda e, o_=bs[:, 0:1], i_=score[:, 0:n]: e.tensor_reduce(out=o_, in_=i_, axis=AX.X, op=ALU.max),
                             [score], [bs])
                        yield
                        P.op("dve", lambda e, o_=bs[:, 1:2], i_=score[:, 0:n]: e.tensor_reduce(out=o_, in_=i_, axis=AX.X, op=ALU.min),
                             [score], [bs])
                        yield
                        TTO("pool", score[:, n - 512:n], score[:, n - 512:n], zb_t[:], ALU.add, [score, zb_t, bs], [score])
                        TTO("dve", bs[:, 2:3], bs[:, 0:1], bs[:, 1:2], ALU.subtract, [bs], [bs])
                        yield
                        TS("dve", bs[:, 8:8 + NBIS], pow2_t[:], bs[:, 2:3], None, ALU.mult, None, [pow2_t, bs], [bs])
                        yield
                        mid = bs[:, 3:4]
                        TS("dve", mid, bs[:, 1:2], bs[:, 8:9], None, ALU.add, None, [bs], [bs])
                        yield
                        for i in range(NBIS):
                            Hi = bs[:, 8 + i:9 + i]
                            Hn = bs[:, 9 + i:10 + i] if i + 1 < NBIS else Hi
                            TS("dve", junkI[:, 0:n], score[:, 0:n], mid, 0.0, ALU.is_ge, ALU.add, [score, bs], [junkI, bs],
                               accum_out=bs[:, 4:5])
                            yield
                            STT("dve", bs[:, 5:6], bs[:, 4:5], 255.5, Hi, ALU.is_ge, ALU.mult, [bs], [bs])
                            yield
                            STT("dve", mid, mid, Hn, bs[:, 5:6], ALU.subtract, ALU.add, [bs], [bs])
                            yield

                    def back(s, score, bs):
                        E = 4 * (s + 1)
                        n = E * 128
                        mask = mask_ring.next()
                        TS("dve", mask[:, 0:n], score[:, 0:n], bs[:, 3:4], None, ALU.is_ge, None, [score, bs], [mask])
                        mT = mT_ring.next()
                        for g in range((E + 7) // 8):
                            pt = ptr_ring.next()
                            nb_ = min(8, E - g * 8)
                            for j in range(nb_):
                                kt = g * 8 + j
                                TRN(pt[:, j, :], mask[:, kt * 128:(kt + 1) * 128], [mask], [pt])
                            CP("act", mT[:, g * 8:g * 8 + nb_, :], pt[:, 0:nb_, :], [pt], [mT])
                        P.dma("sp", maskT_scr[:, blk_of[s]:blk_of[s] + E, :], mT[:, 0:E, :], reads=[mT])

                    fronts = {0: front(0), 1: front(1)}
                    for a in range(0, NQ, 2):
                        b = a + 1
                        sa, za = fronts.pop(a)
                        sb_, zb2 = fronts.pop(b)
                        if a + 2 < NQ:
                            fronts[a + 2] = front(a + 2)
                            fronts[a + 3] = front(a + 3)
                        bsa, bsb = bs_ring.next(), bs_ring.next()
                        ga = bis(a, sa, za, junkI_d, bsa)
                        gb = bis(b, sb_, zb2, junkI_p, bsb)
                        live = [ga, gb]
                        while live:
                            for g_ in list(live):
                                try:
                                    next(g_)
                                except StopIteration:
                                    live.remove(g_)
                        back(a, sa, bsa)
                        back(b, sb_, bsb)
                    score, bs = sb_, bsb
                    P.barrier()
                    dump("B2", "maskT", maskT_scr, [128, 144, 128], BF16)
                    dump("B2", "score", score[:], [128, S], F32)
                    dump("B2", "bs", bs[:], [128, 8 + NBIS], F32)
                    if stop == "B2":
                        P.barrier()
                    checkpoint("B2")

            with ExitStack() as st:
                maskT = alloc(st, "maskT", [128, 144, 128], BF16)
                zmT_t = alloc(st, "zmT", [128, NQ, 512], BF16)
                O_all = alloc(st, "O_all", [128, NQ, D], BF16)
                P.dma("sp", maskT[:], maskT_scr, writes=[maskT])
                P.dma("pool", zmT_t[:], zmT, writes=[zmT_t])
                KT_ring = Ring(st, "KT", 2, [128, S], BF16)
                V_ring = Ring(st, "V", 2, [128, NT, 129], BF16)
                pS_ring = Ring(st, "pS", 5, [128, 512], F32, psum=True)
                pO_ring = Ring(st, "pO", 2, [128, 512], F32, psum=True)
                PT_ring = Ring(st, "PT", 6, [128, 512], BF16)
                rs_ring = Ring(st, "rsum", 4, [128, 1], F32)
                blk_off = [0]
                for s in range(NQ):
                    blk_off.append(blk_off[-1] + 4 * (s + 1))
                groups = [(s, kb) for s in range(NQ) for kb in range(s + 1)]
                for hh in range(16):
                    mla = hh < 8
                    h = hh % 8
                    KT = KT_ring.next()
                    V = V_ring.next()
                    if mla:
                        P.dma("sp", KT[:], KnT_scr[:, h, :], writes=[KT])
                        P.dma("sp", V[:], Vm_scr[h], writes=[V])
                        scale = 192.0 ** -0.5
                    else:
                        P.dma("sp", KT[:], KdT_scr[:, h, :], writes=[KT])
                        P.dma("sp", V[:], Vd_scr[h], writes=[V])
                        scale = 128.0 ** -0.5
                    p0_ = (h % 2) * 64

                    def qk(s, kb):
                        qs = slice(s * 128, (s + 1) * 128)
                        pS = pS_ring.next()
                        for j in range(4):
                            kt = 4 * kb + j
                            ks = slice(kt * 128, (kt + 1) * 128)
                            if mla:
                                MM(pS[:, j * 128:(j + 1) * 128], KT[:, ks], QnT[:, h, qs], True, False, [KT, QnT], [pS])
                                QrP = QrT_lo if h % 2 == 0 else QrT_hi
                                MM(pS[:, j * 128:(j + 1) * 128], kropeT[:, ks], QrP[:, h // 2, qs],
                                   False, True, [kropeT, QrP], [pS])
                            else:
                                MM(pS[:, j * 128:(j + 1) * 128], KT[:, ks], QdT[:, h, qs], True, True, [KT, QdT], [pS])
                        return pS

                    LOOK = 3
                    pend = [qk(*groups[i]) for i in range(LOOK)]
                    pO = None
                    deferred = None
                    for gi, (s, kb) in enumerate(groups):
                        pS = pend.pop(0)
                        if gi + LOOK < len(groups):
                            pend.append(qk(*groups[gi + LOOK]))
                        PT = PT_ring.next()
                        ACTV(PT[:], pS[:], AF.Exp, [pS], [PT], scale=scale)
                        if mla:
                            if kb == s:
                                TTO("dve", PT[:], PT[:], zmT_t[:, s, :], ALU.mult, [PT, zmT_t], [PT])
                        else:
                            b0 = blk_off[s] + 4 * kb
                            TTO("dve" if gi % 4 != 3 else "pool", PT[:], PT[:],
                                maskT[:, b0:b0 + 4, :].rearrange("p a b -> p (a b)"), ALU.mult, [PT, maskT], [PT])
                        if kb == 0:
                            pO = pO_ring.next()
                        E = 4 * (s + 1)
                        for j in range(4):
                            kt = 4 * kb + j
                            MM(pO[:, 0:129], PT[:, j * 128:(j + 1) * 128], V[:, kt, :], kt == 0, kt == E - 1, [PT, V], [pO])
                        if deferred is not None:
                            deferred()
                            deferred = None
                        if kb == s:
                            def deferred(pO=pO, s=s, hh=hh):
                                rsum = rs_ring.next()
                                P.op("dve", lambda e, o_=rsum[:], i_=pO[:, 128:129]: e.reciprocal(out=o_, in_=i_), [pO], [rsum])
                                col = hh * 128
                                ACTV(O_all[:, s, col:col + 128], pO[:, 0:128], AF.Copy, [pO, rsum], [O_all], scale=rsum[:])
                    if deferred is not None:
                        deferred()
                        deferred = None
                P.dma("sp", O_scr.rearrange("(s p) d -> p s d", p=128), O_all[:], reads=[O_all])
                P.barrier()
                dump("B3", "O", O_scr, [NQ * 128, D], BF16)
                if stop == "B3":
                    P.barrier()
                checkpoint("B3")

        stK.close()
        with ExitStack() as stR:
            x1 = alloc(stR, "x1", [128, NQ, D], F32)
            with ExitStack() as st:
                wo = alloc(st, "wo", [128, 16, D], BF16)
                load_w(wo, w_out, 16)
                xring = Ring(st, "xc", 2, [128, D], F32)
                Or = Ring(st, "Oc", 2, [128, D], BF16)
                OT_r = Ring(st, "OT", 2, [128, 16, 128], BF16)
                ptr_ring = Ring(st, "ptrC", 2, [128, 8, 128], BF16, psum=True)
                pm_ring = Ring(st, "pmC", 4, [128, 512], F32, psum=True)
                for s in range(NQ):
                    qs = slice(s * 128, (s + 1) * 128)
                    x_t = xring.next()
                    P.dma("sp", x_t[:], xq[qs, :], writes=[x_t])
                    O_t = Or.next()
                    P.dma("sp", O_t[:], O_scr[qs, :], writes=[O_t])
                    OT = OT_r.next()
                    for g in range(2):
                        pt = ptr_ring.next()
                        for j in range(8):
                            c = g * 8 + j
                            TRN(pt[:, j, :], O_t[:, c * 128:(c + 1) * 128], [O_t], [pt])
                        CP("act" if g == 0 else "dve", OT[:, g * 8:(g + 1) * 8, :], pt[:], [pt], [OT])
                    for g in range(4):
                        pm = pm_ring.next()
                        for c in range(16):
                            MM(pm[:], OT[:, c, :], wo[:, c, g * 512:(g + 1) * 512], c == 0, c == 15, [OT, wo], [pm])
                        TTO("dve", x1[:, s, g * 512:(g + 1) * 512], pm[:], x_t[:, g * 512:(g + 1) * 512], ALU.add,
                            [pm, x_t], [x1])
                P.barrier()
                dump("C", "x1C", x1[:], [128, NQ, D], F32)
                if stop == "C":
                    P.barrier()
                checkpoint("C")

            with ExitStack() as st:
                wq_c = alloc(st, "wqc", [128, 16, 512], BF16)
                wk_c = alloc(st, "wkc", [128, 16, 512], BF16)
                wv_c = alloc(st, "wvc", [128, 16, 512], BF16)
                wo_c = alloc(st, "woc", [128, 4, D], BF16)
                gc = load_g(st, "g_cross", g_cross, 16)
                gme = load_g(st, "g_mem", g_mem, 16)
                load_w(wk_c, wkc, 16, gme)
                load_w(wv_c, wvc, 16, gme)
                load_w(wq_c, wqc, 16, gc)
                load_w(wo_c, woc, 4)
                memT = alloc(st, "memT", [128, 16, 256], BF16)
                KcT = alloc(st, "KcT", [128, 4, 256], BF16)
                Vc = alloc(st, "Vc", [128, 2, 4, 129], BF16)
                MEMSET("pool", Vc[:, :, :, 128:129], 1.0, [Vc])
                xring = Ring(st, "xm", 2, [128, D], F32)
                junk = alloc(st, "junkD", [128, D], BF16)
                statr = Ring(st, "statD", 4, [128, 4], F32)
                xbr = Ring(st, "xbD", 1, [128, D], BF16)
                hTr = Ring(st, "hTD", 2, [128, 16, 128], BF16)
                ptr_ring = Ring(st, "ptrD", 2, [128, 8, 128], BF16, psum=True)
                pm_ring = Ring(st, "pmD", 2, [128, 512], F32, psum=True)
                pS_ring = Ring(st, "pSD", 2, [128, 512], F32, psum=True)
                pO_ring = Ring(st, "pOD", 2, [128, 512], F32, psum=True)
                QcT_r = Ring(st, "QcT", 2, [128, 4, 128], BF16)
                PT_ring = Ring(st, "PTD", 4, [128, 512], BF16)
                rs_ring = Ring(st, "rsD", 2, [128, 4], F32)
                Oc_r = Ring(st, "Oc_tok", 2, [128, 512], BF16)
                OcT_r = Ring(st, "OcT", 2, [128, 4, 128], BF16)
                for mt in range(2):
                    x_t = xring.next()
                    P.dma("sp", x_t[:], mem[mt * 128:(mt + 1) * 128, :], writes=[x_t])
                    norm_transpose(x_t[:], x_t, lambda g, mt=mt: memT[:, g * 8:(g + 1) * 8, mt * 128:(mt + 1) * 128], memT,
                                   junk, statr.next(), xbr.next(), ptr_ring)
                for h in range(4):
                    pm = pm_ring.next()
                    for c in range(16):
                        MM(pm[:, 0:256], wk_c[:, c, h * 128:(h + 1) * 128], memT[:, c, :], c == 0, c == 15, [wk_c, memT], [pm])
                    CP("act", KcT[:, h, :], pm[:, 0:256], [pm], [KcT])
                for mt in range(2):
                    pm = pm_ring.next()
                    for c in range(16):
                        MM(pm[:], memT[:, c, mt * 128:(mt + 1) * 128], wv_c[:, c, :], c == 0, c == 15, [memT, wv_c], [pm])
                    CP("dve", Vc[:, mt, :, 0:128], pm[:].rearrange("p (h d) -> p h d", h=4), [pm], [Vc])
                cscale = 128.0 ** -0.5
                dctx = {}
                x1d = [Buf("x1d%d" % s_) for s_ in range(NQ)]

                def d1(s):
                        hT = hTr.next()
                        norm_transpose(x1[:, s, :], x1d[s], lambda g, hT=hT: hT[:, g * 8:(g + 1) * 8, :], hT, junk, statr.next(),
                                       xbr.next(), ptr_ring)
                        pm = pm_ring.next()
                        for h in range(4):
                            for c in range(16):
                                MM(pm[:, h * 128:(h + 1) * 128], wq_c[:, c, h * 128:(h + 1) * 128], hT[:, c, :], c == 0, c == 15,
                                   [wq_c, hT], [pm])
                        QcT = QcT_r.next()
                        CP("act", QcT[:], pm[:].rearrange("p (h d) -> p h d", h=4), [pm], [QcT])
                        dctx[s] = QcT

                def d2(s):
                        QcT = dctx.pop(s)
                        PTs = []
                        for hp in range(2):
                            pS = pS_ring.next()
                            for hl in range(2):
                                h = hp * 2 + hl
                                for mt in range(2):
                                    cs = (hl * 2 + mt) * 128
                                    MM(pS[:, cs:cs + 128], KcT[:, h, mt * 128:(mt + 1) * 128], QcT[:, h, :], True, True, [KcT, QcT], [pS])
                            PT = PT_ring.next()
                            ACTV(PT[:], pS[:], AF.Exp, [pS], [PT], scale=cscale)
                            PTs.append(PT)
                        rsum = rs_ring.next()
                        Oc = Oc_r.next()
                        for hp in range(2):
                            pO = pO_ring.next()
                            PT = PTs[hp]
                            for hl in range(2):
                                h = hp * 2 + hl
                                for mt in range(2):
                                    cs = (hl * 2 + mt) * 128
                                    MM(pO[:, hl * 256:hl * 256 + 129], PT[:, cs:cs + 128], Vc[:, mt, h, :], mt == 0, mt == 1, [PT, Vc], [pO])
                            for hl in range(2):
                                h = hp * 2 + hl
                                P.op("dve", lambda e, o_=rsum[:, h:h + 1], i_=pO[:, hl * 256 + 128:hl * 256 + 129]: e.reciprocal(out=o_, in_=i_), [pO], [rsum])
                                ACTV(Oc[:, h * 128:(h + 1) * 128], pO[:, hl * 256:hl * 256 + 128], AF.Copy, [pO, rsum], [Oc], scale=rsum[:, h:h + 1])
                        OcT = OcT_r.next()
                        pt = ptr_ring.next()
                        for c in range(4):
                            TRN(pt[:, c, :], Oc[:, c * 128:(c + 1) * 128], [Oc], [pt])
                        CP("dve", OcT[:], pt[:, 0:4, :], [pt], [OcT])
                        for g in range(4):
                            pm = pm_ring.next()
                            for c in range(4):
                                MM(pm[:], OcT[:, c, :], wo_c[:, c, g * 512:(g + 1) * 512], c == 0, c == 3, [OcT, wo_c], [pm])
                            TTO("dve", x1[:, s, g * 512:(g + 1) * 512], pm[:], x1[:, s, g * 512:(g + 1) * 512], ALU.add,
                                [pm, x1d[s]], [x1d[s]])

                d1(0)
                for s in range(NQ):
                    if s + 1 < NQ:
                        d1(s + 1)
                    d2(s)
                P.barrier()
                dump("D", "x1D", x1[:], [128, NQ, D], F32)
                if stop == "D":
                    P.barrier()
                checkpoint("D")

            with ExitStack() as st:
                hmT = alloc(st, "hmT", [128, 16, NQ * 128], BF16)
                with ExitStack() as st2:
                    junk = alloc(st2, "junkE", [128, D], BF16)
                    statr = Ring(st2, "statE", 4, [128, 4], F32)
                    xbr = Ring(st2, "xbE", 2, [128, D], BF16)
                    ptr_ring = Ring(st2, "ptrE", 2, [128, 8, 128], BF16, psum=True)
                    for s in range(NQ):
                        norm_transpose(x1[:, s, :], x1, lambda g, s=s: hmT[:, g * 8:(g + 1) * 8, s * 128:(s + 1) * 128], hmT,
                                       junk, statr.next(), xbr.next(), ptr_ring)
                    P.barrier()
                with ExitStack() as st2:
                    gml = load_g(st2, "g_mlp", g_mlp, 16)
                    wu_ring = Ring(st2, "wu", 2, [128, 16, 512], BF16)
                    wd_ring = Ring(st2, "wd", 2, [128, 4, D], BF16)
                    aT_ring = Ring(st2, "aT", 2, [128, 4, NQ * 128], BF16)
                    rt_ring = Ring(st2, "rt", 2, [128, 512], F32)
                    et_ring = Ring(st2, "et", 2, [128, 512], F32)
                    x1s = [[Buf("x1_%d_%d" % (s_, g_)) for g_ in range(4)] for s_ in range(NQ)]
                    pu_ring = Ring(st2, "pu", 3, [128, 512], F32, psum=True)
                    pd_ring = Ring(st2, "pd", 4, [128, 512], F32, psum=True)
                    NCH = DFF // 512

                    def load_chunk(ch):
                        wu = wu_ring.next()
                        wd = wd_ring.next()
                        P.dma("pool", wu[:], w_up[:, ch * 512:(ch + 1) * 512].rearrange("(c p) n -> p c n", p=128), writes=[wu])
                        P.dma("pool", wd[:], w_down[ch * 512:(ch + 1) * 512, :].rearrange("(f p) n -> p f n", p=128), writes=[wd])
                        for c in range(16):
                            TS("dve", wu[:, c, :], wu[:, c, :], gml[:, c:c + 1], None, ALU.mult, None, [wu, gml], [wu])
                        return wu, wd

                    nxt = load_chunk(0)
                    for ch in range(NCH):
                        wu, wd = nxt
                        if ch + 1 < NCH:
                            nxt = load_chunk(ch + 1)
                        aT = aT_ring.next()
                        for fb in range(4):
                            for half in range(2):
                                pu = pu_ring.next()
                                ts_ = slice(half * 512, (half + 1) * 512)
                                for c in range(16):
                                    MM(pu[:], wu[:, c, fb * 128:(fb + 1) * 128], hmT[:, c, ts_], c == 0, c == 15, [wu, hmT], [pu])
                                rt = rt_ring.next()
                                ACTV(rt[:], pu[:], AF.Relu, [pu], [rt])
                                TTO("pool", aT[:, fb, ts_], rt[:], rt[:], ALU.mult, [rt], [aT])
                        for s in range(NQ):
                            for g in range(4):
                                pd = pd_ring.next()
                                for fb in range(4):
                                    MM(pd[:], aT[:, fb, s * 128:(s + 1) * 128], wd[:, fb, g * 512:(g + 1) * 512], fb == 0, fb == 3,
                                       [aT, wd], [pd])
                                xs_ = x1[:, s, g * 512:(g + 1) * 512]
                                if g % 2 == 0:
                                    TTO("dve", xs_, pd[:], xs_, ALU.add, [pd, x1s[s][g]], [x1s[s][g]])
                                else:
                                    et = et_ring.next()
                                    CP("act", et[:], pd[:], [pd], [et])
                                    TTO("pool", xs_, et[:], xs_, ALU.add, [et, x1s[s][g]], [x1s[s][g]])
                    P.barrier()
            with ExitStack() as st:
                gf = alloc(st, "g_fin", [128, D], F32)
                P.dma("sp", gf[:], g_fin, writes=[gf])
                junk = alloc(st, "junkF", [128, D], BF16)
                statr = Ring(st, "statF", 4, [128, 4], F32)
                y_ring = Ring(st, "y", 2, [128, D], F32)
                outs = []
                for s in range(NQ):
                    stat = statr.next()
                    rs = rstd_of(x1[:, s, :], D, [x1], junk, stat)
                    y = y_ring.next()
                    STT("dve", y[:], x1[:, s, :], rs, gf[:], ALU.mult, ALU.mult, [x1, stat, gf], [y])
                    outs.append(P.dma("sp", out[s * 128:(s + 1) * 128, :], y[:], reads=[y]))
        final = [o_ for o_ in outs if o_ is not None]
      except _Stop:
        final = []
      P.emit(nc, final_waits=list(final) + dump_ops)
    return nc


def _qtiles(r):
    return [r, 7 - r, 8 + r, 15 - r, 16 + r, 23 - r, 24 + r, 31 - r]


_NC_CACHE = {}
_PREP_ONLY = [False]


def kernel(x, mem, positions, g_mix, w_in, g_cq, g_ckv, w_qb, w_kvb, w_out, g_cross, g_mem, w_q_cross, w_k_cross,
           w_v_cross, w_o_cross, g_mlp, w_up, w_down, g_final):
    f32 = np.float32
    x = np.asarray(x, f32)
    mem = np.asarray(mem, f32)
    positions = np.asarray(positions, np.int32)
    w_in0 = np.asarray(w_in, f32)[0]
    o = np.cumsum([0, 512, 256, 64, 1024, 1024, 1024, 1024, 64, 16])
    c_q, c_kv, k_rope, q_d, k_d, v_d, q_idx, k_idx, w_idx = [w_in0[:, o[i]:o[i + 1]] for i in range(9)]
    wk_side = np.ascontiguousarray(np.concatenate([c_kv, k_rope, k_idx, k_d, v_d], axis=1))
    wq_side = np.ascontiguousarray(np.concatenate([c_q, q_idx, w_idx, q_d], axis=1))
    wqb0 = np.asarray(w_qb, f32)[0].reshape(512, 8, 192)
    wqb_r = np.ascontiguousarray(np.concatenate([wqb0[:, :, :128].reshape(512, 1024), wqb0[:, :, 128:].reshape(512, 512)], axis=1))

    def gl(g, k):
        return np.ascontiguousarray(np.asarray(g, f32).reshape(k, 128).T)

    common = {
        "invf": np.ascontiguousarray(np.broadcast_to((THETA ** (-np.arange(32, dtype=np.float64) / 32)).astype(f32), (128, 32))),
        "pow2": np.ascontiguousarray(np.broadcast_to((2.0 ** -(np.arange(NBIS) + 1.0)).astype(f32), (128, NBIS))),
        "ident": np.eye(128, dtype=f32),
        "wk_side": wk_side, "wq_side": wq_side,
        "g_mix": gl(np.asarray(g_mix)[0], 16),
        "w_qb": wqb_r, "g_cq": gl(np.asarray(g_cq)[0], 4),
        "w_kvb": np.ascontiguousarray(np.asarray(w_kvb, f32)[0]), "g_ckv": gl(np.asarray(g_ckv)[0], 2),
        "w_out": np.ascontiguousarray(np.asarray(w_out, f32)[0]),
        "g_cross": gl(np.asarray(g_cross)[0], 16), "g_mem": gl(np.asarray(g_mem)[0], 16),
        "wqc": np.ascontiguousarray(np.asarray(w_q_cross, f32)[0]),
        "wkc": np.ascontiguousarray(np.asarray(w_k_cross, f32)[0]),
        "wvc": np.ascontiguousarray(np.asarray(w_v_cross, f32)[0]),
        "woc": np.ascontiguousarray(np.asarray(w_o_cross, f32)[0]),
        "g_mlp": gl(np.asarray(g_mlp)[0], 16),
        "w_up": np.ascontiguousarray(np.asarray(w_up, f32)[0]),
        "w_down": np.ascontiguousarray(np.asarray(w_down, f32)[0]),
        "g_fin": np.ascontiguousarray(np.broadcast_to(np.asarray(g_final, f32).reshape(1, D), (128, D))),
    }
    in_maps = []
    for c in range(8):
        b, r = c // 4, c % 4
        qt = _qtiles(r)
        xq = np.ascontiguousarray(np.concatenate([x[b, t * 128:(t + 1) * 128] for t in qt], axis=0))
        posk = np.ascontiguousarray(positions[b].reshape(NT, 128).T)
        posq = np.ascontiguousarray(np.stack([positions[b, t * 128:(t + 1) * 128] for t in qt], axis=1))
        zmT = np.zeros((128, NQ, 512), f32)
        zbm = np.zeros((128, NQ, 512), f32)
        for s, t in enumerate(qt):
            qpos = t * 128 + np.arange(128)
            for j in range(4):
                kpos = (4 * s + j) * 128 + np.arange(128)
                allowed = (kpos[:, None] // 64) <= (qpos[None, :] // 64)
                zmT[:, s, j * 128:(j + 1) * 128] = allowed
                zbm[:, s, j * 128:(j + 1) * 128] = np.where(allowed.T, 0.0, -BIG)
        m = dict(common)
        m.update({"x_all": np.ascontiguousarray(x[b]), "xq": xq, "posk": posk, "posq": posq,
                  "mem": np.ascontiguousarray(mem[b]), "zmT": zmT, "zb": zbm})
        in_maps.append(m)
    if _PREP_ONLY[0]:
        return in_maps
    if "nc" not in _NC_CACHE:
        _NC_CACHE["nc"] = build_program()
    res = run_bass_kernel_spmd(_NC_CACHE["nc"], in_maps, core_ids=list(range(8)))
    outp = np.zeros((2, S, D), f32)
    for c in range(8):
        b, r = c // 4, c % 4
        o_c = np.asarray(res.results[c]["out"])
        for s, t in enumerate(_qtiles(r)):
            outp[b, t * 128:(t + 1) * 128] = o_c[s * 128:(s + 1) * 128]
    return outp
```

```python
import math
from contextlib import ExitStack

import numpy as np
import concourse.bass as bass
import concourse.mybir as mybir
from concourse.bass_utils import run_bass_kernel_spmd

F32 = mybir.dt.float32
BF16 = mybir.dt.bfloat16
I32 = mybir.dt.int32
AF = mybir.ActivationFunctionType
ALU = mybir.AluOpType
AX = mybir.AxisListType
PI = math.pi

D = 2048
S = 4096
NT = 32
NQ = 8
EPS = 1e-6
THETA = 500000.0
DFF = 8192
NBIS = 18
import os
KVV = int(os.environ.get('KVV', '0'))
BIS_POOL = tuple(int(c) for c in os.environ.get('BIS_POOL', '').split(',') if c != '')
BIG = 1.0e30

ENGS = ("pe", "act", "dve", "pool", "sp")


class Buf:
    __slots__ = ("name", "w", "r", "excl", "strict")

    def __init__(self, name=""):
        self.name = name
        self.w = None
        self.r = []
        self.excl = False
        self.strict = False


class TT:
    def __init__(self, t, name):
        self.t = t
        self.b = Buf(name)

    def __getitem__(self, k):
        return self.t[k]


def _b(x):
    return x.b if isinstance(x, TT) else x


class Op:
    __slots__ = ("eng", "fn", "dma", "deps", "signal", "tok", "slot_wait", "epoch")

    def __init__(self, eng, fn, dma):
        self.eng = eng
        self.fn = fn
        self.dma = dma
        self.deps = []
        self.signal = False
        self.tok = None
        self.slot_wait = None
        self.epoch = 0


class Prog:
    def __init__(self, nslots=6):
        self.ops = []
        self.nslots = nslots
        self.epoch = 0
        self.barriers = []
        self.stopped = False
        self.last = {}

    def op(self, eng, fn, reads=(), writes=(), dma=False):
        if self.stopped:
            return None
        o = Op(eng, fn, dma)
        o.epoch = self.epoch
        deps = {}
        reads = [_b(x) for x in reads]
        writes = [_b(x) for x in writes]
        writes = writes + [b for b in reads if b.excl and b not in writes]
        reads = [b for b in reads if not b.excl]
        for b in reads:
            if b.w is not None:
                deps[id(b.w)] = (b.w, True)
        for b in writes:
            if b.w is not None and id(b.w) not in deps:
                deps[id(b.w)] = (b.w, b.strict)
            for r in b.r:
                if id(r) not in deps:
                    deps[id(r)] = (r, b.strict)
        for d, raw in deps.values():
            if d.epoch != o.epoch:
                continue
            if (not d.dma) and (not dma) and d.eng == eng:
                if eng == "pe" or not raw:
                    continue
            d.signal = True
            o.deps.append(d)
        for b in map(_b, reads):
            if not dma:
                b.r = [r for r in b.r if r.dma or r.eng != eng or r.epoch != o.epoch]
            b.r.append(o)
        for b in map(_b, writes):
            b.w = o
            b.r = []
        self.ops.append(o)
        if not dma:
            self.last[eng] = o
        return o

    def barrier(self):
        if self.stopped:
            return
        for e, o in self.last.items():
            o.signal = True
        self.last = {}
        self.epoch += 1
        self.barriers.append(len(self.ops))

    def dma(self, q, out, in_, reads=(), writes=(), **kw):
        return self.op(q, lambda e: e.dma_start(out=out, in_=in_, **kw), reads, writes, dma=True)

    def emit(self, nc, final_waits=()):
        with ExitStack() as st:
            esem = {e: st.enter_context(nc.semaphore("s_" + e)) for e in ("pe", "act", "dve", "pool")}
            qsem = {q: [st.enter_context(nc.semaphore("d_%s%d" % (q, i))) for i in range(self.nslots)]
                    for q in ("sp", "act", "pool")}
            ecnt = {e: 0 for e in esem}
            qn = {q: 0 for q in qsem}
            qtot = {q: [0] * self.nslots for q in qsem}
            bar_toks = []
            nb = 0
            for i, o in enumerate(self.ops):
                while nb < len(self.barriers) and self.barriers[nb] == i:
                    toks = [(esem[e], ecnt[e]) for e in esem if ecnt[e] > 0]
                    for q in qsem:
                        for s_ in range(self.nslots):
                            if qtot[q][s_] > 0:
                                toks.append((qsem[q][s_], qtot[q][s_]))
                    bar_toks.append(toks)
                    nb += 1
                if o.dma:
                    q = o.eng
                    s_ = qn[q] % self.nslots
                    qn[q] += 1
                    if qtot[q][s_] > 0:
                        o.slot_wait = (qsem[q][s_], qtot[q][s_])
                    qtot[q][s_] += 16
                    o.tok = (qsem[q][s_], qtot[q][s_])
                elif o.signal:
                    ecnt[o.eng] += 1
                    o.tok = (esem[o.eng], ecnt[o.eng])
            while nb < len(self.barriers):
                bar_toks.append([])
                nb += 1
            per = {e: [o for o in self.ops if o.eng == e] for e in ENGS}
            block = st.enter_context(nc.Block())

            def run(e, eng):
                waited = {}
                cur_epoch = 0

                def w(tok):
                    sem, val = tok
                    k = id(sem)
                    if waited.get(k, 0) < val:
                        eng.wait_ge(sem, val)
                        waited[k] = val

                for o in per[e]:
                    while cur_epoch < o.epoch:
                        for t in bar_toks[cur_epoch]:
                            w(t)
                        cur_epoch += 1
                    if o.slot_wait is not None:
                        w(o.slot_wait)
                    for d in o.deps:
                        w(d.tok)
                    inst = o.fn(eng)
                    if o.dma:
                        inst.then_inc(o.tok[0], 16)
                    elif o.signal:
                        inst.then_inc(o.tok[0], 1)
                if e == "sp":
                    for o in final_waits:
                        w(o.tok)

            @block.tensor
            def _(eng):
                run("pe", eng)

            @block.scalar
            def _(eng):
                run("act", eng)

            @block.vector
            def _(eng):
                run("dve", eng)

            @block.gpsimd
            def _(eng):
                run("pool", eng)

            @block.sync
            def _(eng):
                run("sp", eng)


class _Stop(Exception):
    pass


def build_program(stop=None):
    nc = bass.Bass("TRN2", target_bir_lowering=False)
    dump_ops = []

    def dump(ph, name, ap, shape, dt):
        if stop != ph or P.stopped:
            return
        o_ = nc.dram_tensor("dbg_" + name, list(shape), dt, kind="ExternalOutput").ap()
        P.barrier()
        dump_ops.append(P.dma("sp", o_, ap))

    def checkpoint(name):
        if stop == name:
            P.stopped = True

    def din(name, shape, dt=F32):
        return nc.dram_tensor(name, list(shape), dt, kind="ExternalInput").ap()

    x_all = din("x_all", [S, D])
    xq = din("xq", [NQ * 128, D])
    posk = din("posk", [128, NT], I32)
    posq = din("posq", [128, NQ], I32)
    mem = din("mem", [256, D])
    invf = din("invf", [128, 32])
    pow2 = din("pow2", [128, NBIS])
    ident = din("ident", [128, 128])
    zmT = din("zmT", [128, NQ, 512])
    zb = din("zb", [128, NQ, 512])
    wk_side = din("wk_side", [D, 2432])
    wq_side = din("wq_side", [D, 2576])
    g_mix = din("g_mix", [128, 16])
    w_qb = din("w_qb", [512, 1536])
    g_cq = din("g_cq", [128, 4])
    w_kvb = din("w_kvb", [256, 2048])
    g_ckv = din("g_ckv", [128, 2])
    w_out = din("w_out", [D, D])
    g_cross = din("g_cross", [128, 16])
    g_mem = din("g_mem", [128, 16])
    wqc = din("wqc", [D, 512])
    wkc = din("wkc", [D, 512])
    wvc = din("wvc", [D, 512])
    woc = din("woc", [512, D])
    g_mlp = din("g_mlp", [128, 16])
    w_up = din("w_up", [D, DFF])
    w_down = din("w_down", [DFF, D])
    g_fin = din("g_fin", [128, D])
    out = nc.dram_tensor("out", [NQ * 128, D], F32, kind="ExternalOutput").ap()

    def dscr(name, shape):
        return nc.dram_tensor(name, list(shape), BF16, kind="Internal").ap()

    KnT_scr = dscr("KnT_scr", [128, 8, S])
    KdT_scr = dscr("KdT_scr", [128, 8, S])
    Vm_scr = dscr("Vm_scr", [8, 128, NT, 129])
    Vd_scr = dscr("Vd_scr", [8, 128, NT, 129])
    maskT_scr = dscr("maskT_scr", [128, 144, 128])
    O_scr = dscr("O_scr", [NQ * 128, D])

    P = Prog()
    cnt = [0]

    def MM(out_, lhsT, rhs, start, stop, reads, writes):
        P.op("pe", lambda e: e.matmul(out_, lhsT=lhsT, rhs=rhs, start=start, stop=stop), reads, writes)

    def ACTV(out_, in_, func, reads, writes, **kw):
        P.op("act", lambda e: e.activation(out=out_, in_=in_, func=func, **kw), reads, writes)

    def TS(eng, out_, in0, s1, s2, op0, op1, reads, writes, accum_out=None):
        if op1 is None:
            P.op(eng, lambda e: e.tensor_scalar(out=out_, in0=in0, scalar1=s1, scalar2=None, op0=op0), reads, writes)
        elif accum_out is None:
            P.op(eng, lambda e: e.tensor_scalar(out=out_, in0=in0, scalar1=s1, scalar2=s2, op0=op0, op1=op1), reads, writes)
        else:
            P.op(eng, lambda e: e.tensor_scalar(out=out_, in0=in0, scalar1=s1, scalar2=s2, op0=op0, op1=op1,
                                                accum_out=accum_out), reads, writes)

    def TTO(eng, out_, in0, in1, op, reads, writes):
        P.op(eng, lambda e: e.tensor_tensor(out=out_, in0=in0, in1=in1, op=op), reads, writes)

    def STT(eng, out_, in0, scalar, in1, op0, op1, reads, writes):
        P.op(eng, lambda e: e.scalar_tensor_tensor(out=out_, in0=in0, scalar=scalar, in1=in1, op0=op0, op1=op1),
             reads, writes)

    def CP(eng, out_, in_, reads, writes):
        if eng == "act":
            ACTV(out_, in_, AF.Copy, reads, writes)
        else:
            P.op(eng, lambda e: e.tensor_copy(out=out_, in_=in_), reads, writes)

    def MEMSET(eng, ap, val, writes):
        P.op(eng, lambda e: e.memset(ap, val), (), writes)

    with ExitStack() as top:
      try:
        def alloc(st, name, shape, dt):
            cnt[0] += 1
            t_ = TT(st.enter_context(nc.sbuf_tensor("%s_%d" % (name, cnt[0]), list(shape), dt)), name)
            t_.b.strict = name.startswith("junk")
            return t_

        def palloc(st, name, shape, dt):
            cnt[0] += 1
            t_ = TT(st.enter_context(nc.psum_tensor("%s_%d" % (name, cnt[0]), list(shape), dt)), name)
            t_.b.excl = True
            return t_

        class Ring:
            def __init__(self, st, name, n, shape, dt, psum=False):
                self.items = [(palloc if psum else alloc)(st, "%s%d" % (name, i), shape, dt) for i in range(n)]
                self.i = 0

            def next(self):
                t = self.items[self.i % len(self.items)]
                self.i += 1
                return t

        def TRN(out_, in_, reads, writes):
            P.op("pe", lambda e: e.transpose(out=out_, in_=in_, identity=identb[:]), list(reads) + [identb], writes)

        identf = alloc(top, "identf", [128, 128], F32)
        identb = alloc(top, "identb", [128, 128], BF16)
        invf_t = alloc(top, "invf", [128, 32], F32)
        pow2_t = alloc(top, "pow2", [128, NBIS], F32)
        mhalf = alloc(top, "mhalf", [128, 1], F32)
        stK = ExitStack()
        kropeT = alloc(stK, "kropeT", [128, S], BF16)
        kidx_lo = alloc(stK, "kidx_lo", [128, S], BF16)
        kidx_hi = alloc(stK, "kidx_hi", [128, S], BF16)

        P.dma("sp", identf[:], ident, writes=[identf])
        P.dma("sp", invf_t[:], invf, writes=[invf_t])
        P.dma("sp", pow2_t[:], pow2, writes=[pow2_t])
        CP("dve", identb[:], identf[:], [identf], [identb])
        MEMSET("pool", mhalf[:], -0.5, [mhalf])
        MEMSET("pool", kidx_lo[:], 0.0, [kidx_lo])
        MEMSET("pool", kidx_hi[:], 0.0, [kidx_hi])

        def make_tables(pos_dram, n, cos_t, sin_t):
            with ExitStack() as st:
                posi = alloc(st, "posi", [128, n], I32)
                posf = alloc(st, "posf", [128, n], F32)
                ang = alloc(st, "ang", [128, n, 32], F32)
                a2 = alloc(st, "a2", [128, n, 32], F32)
                tp = alloc(st, "tp", [128, n, 32], F32)
                ki = alloc(st, "ki", [128, n, 32], I32)
                P.dma("sp", posi[:], pos_dram, writes=[posi])
                CP("dve", posf[:], posi[:], [posi], [posf])
                for t in range(n):
                    TS("dve", ang[:, t, :], invf_t[:], posf[:, t:t + 1], None, ALU.mult, None, [invf_t, posf], [ang])
                HI = 6.28125
                LO = 2 * PI - 6.28125
                for (dst, shift) in ((sin_t, 0.0), (cos_t, PI / 2)):
                    TS("dve", a2[:], ang[:], shift, None, ALU.add, None, [ang], [a2])
                    TS("dve", tp[:], a2[:], 1.0 / (2 * PI), None, ALU.mult, None, [a2], [tp])
                    CP("dve", ki[:], tp[:], [tp], [ki])
                    CP("dve", tp[:], ki[:], [ki], [tp])
                    STT("dve", a2[:], tp[:], -HI, a2[:], ALU.mult, ALU.add, [tp, a2], [a2])
                    STT("dve", a2[:], tp[:], -LO, a2[:], ALU.mult, ALU.add, [tp, a2], [a2])
                    TS("dve", tp[:], a2[:], PI, None, ALU.is_gt, None, [a2], [tp])
                    STT("dve", a2[:], tp[:], -2 * PI, a2[:], ALU.mult, ALU.add, [tp, a2], [a2])
                    TS("dve", tp[:], a2[:], -PI, None, ALU.is_lt, None, [a2], [tp])
                    STT("dve", a2[:], tp[:], 2 * PI, a2[:], ALU.mult, ALU.add, [tp, a2], [a2])
                    ACTV(dst[:], a2[:], AF.Sin, [a2], [dst])
                P.barrier()

        def rope(src, dst, H, half, cos_ap, sin_ap, tmp, reads, writes):
            cb = cos_ap.to_broadcast([128, H, half])
            sb_ = sin_ap.to_broadcast([128, H, half])
            n = H * half
            ta = tmp[:, 0, 0:n].rearrange("p (h d) -> p h d", h=H)
            tb = tmp[:, 1, 0:n].rearrange("p (h d) -> p h d", h=H)
            tc_ = tmp[:, 2, 0:n].rearrange("p (h d) -> p h d", h=H)
            td = tmp[:, 3, 0:n].rearrange("p (h d) -> p h d", h=H)
            x1_ = src[:, :, 0:half]
            x2_ = src[:, :, half:2 * half]
            TTO("dve", ta, x1_, cb, ALU.mult, reads, [tmp])
            TTO("dve", tb, x2_, sb_, ALU.mult, reads, [tmp])
            TTO("dve", tc_, x2_, cb, ALU.mult, reads, [tmp])
            TTO("dve", td, x1_, sb_, ALU.mult, reads, [tmp])
            TTO("pool", dst[:, :, 0:half], ta, tb, ALU.subtract, [tmp], writes)
            TTO("pool", dst[:, :, half:2 * half], tc_, td, ALU.add, [tmp], writes)

        def rstd_of(src_ap, n, reads, junk, stat):
            ACTV(junk[:, 0:n], src_ap, AF.Square, reads, [junk, stat], accum_out=stat[:, 0:1])
            TS("dve", stat[:, 1:2], stat[:, 0:1], 1.0 / n, EPS, ALU.mult, ALU.add, [stat], [stat])
            TTO("pool", stat[:, 2:3], stat[:, 1:2], mhalf[:], ALU.pow, [stat, mhalf], [stat])
            return stat[:, 2:3]

        def load_w(dst, src, kc, g_t=None, eng_fold="dve"):
            for c in range(kc):
                P.dma("pool", dst[:, c, :], src[c * 128:(c + 1) * 128, :], writes=[dst])
            if g_t is not None:
                for c in range(kc):
                    TS(eng_fold, dst[:, c, :], dst[:, c, :], g_t[:, c:c + 1], None, ALU.mult, None, [dst, g_t], [dst])

        def load_g(st, name, src, k):
            t = alloc(st, name, [128, k], F32)
            P.dma("sp", t[:], src, writes=[t])
            return t

        def norm_transpose(x_ap, x_tt, dstf, dst_tt, junk, stat, xb, ptr_ring):
            rs = rstd_of(x_ap, D, [x_tt], junk, stat)
            TS("dve", xb[:], x_ap, rs, None, ALU.mult, None, [x_tt, stat], [xb])
            for g in range(2):
                pt = ptr_ring.next()
                for j in range(8):
                    c = g * 8 + j
                    TRN(pt[:, j, :], xb[:, c * 128:(c + 1) * 128], [xb], [pt])
                CP("act" if g == 0 else "dve", dstf(g), pt[:], [pt], [dst_tt])

        with ExitStack() as st:
            cosK = alloc(st, "cosK", [128, NT, 32], F32)
            sinK = alloc(st, "sinK", [128, NT, 32], F32)
            make_tables(posk, NT, cosK, sinK)
            dump("T", "cosK", cosK[:], [128, NT, 32], F32)
            dump("T", "sinK", sinK[:], [128, NT, 32], F32)
            checkpoint("T")
            wk = alloc(st, "wk", [128, 16, 2432], BF16)
            wkv = alloc(st, "wkv", [128, 2, 2048], BF16)
            gm = load_g(st, "g_mix", g_mix, 16)
            gk = load_g(st, "g_ckv", g_ckv, 2)
            load_w(wk, wk_side, 16, gm)
            load_w(wkv, w_kvb, 2, gk)
            xring = Ring(st, "xa", 2, [128, D], F32)
            junk = alloc(st, "junkA", [128, 256], BF16)
            statr = Ring(st, "statA", 6, [128, 4], F32)
            xbr = Ring(st, "xbA", 2, [128, D], BF16)
            hTr = Ring(st, "hTA", 2, [128, 16, 128], BF16)
            ptr_ring = Ring(st, "ptrA", 2, [128, 8, 128], BF16, psum=True)
            pm_ring = Ring(st, "pmA", 6, [128, 512], F32, psum=True)
            tmp_ring = Ring(st, "ropetmp", 2, [128, 4, 256], F32)
            ckvn_r = Ring(st, "ckvn", 2, [128, 256], BF16)
            ckvT_r = Ring(st, "ckvT", 2, [128, 2, 128], BF16)
            kr_tok_r = Ring(st, "kr_tok", 2, [128, 128], BF16)
            ki_tok_r = Ring(st, "ki_tok", 2, [128, 128], BF16)
            kd_tok_r = Ring(st, "kd_tok", 2, [128, 8, 128], BF16)
            kn_tok_r = Ring(st, "kn_tok", 2, [128, 8, 128], BF16)
            KnT_st_r = Ring(st, "KnT_st", 1, [128, 8, 512], BF16)
            KdT_st_r = Ring(st, "KdT_st", 1, [128, 8, 512], BF16)
            Vm_st_r = Ring(st, "Vm_st", 1, [128, 8, 4, 129], BF16)
            Vd_st_r = Ring(st, "Vd_st", 1, [128, 8, 4, 129], BF16)
            for rg in (Vm_st_r, Vd_st_r):
                for it in rg.items:
                    MEMSET("pool", it[:, :, :, 128:129], 1.0, [it])
            dump("W", "wk", wk[:, :, 0:512], [128, 16, 512], BF16)
            dump("W", "wkv", wkv[:, :, 0:512], [128, 2, 512], BF16)
            checkpoint("W")

            Vd_groups = {}
            ctx = {}

            def stage0(t):
                x_t = xring.next()
                P.dma("act", x_t[:], x_all[t * 128:(t + 1) * 128, :], writes=[x_t])
                xb = xbr.next()
                stat = statr.next()
                rs = rstd_of(x_t[:], D, [x_t], xb, stat)
                TS("dve", xb[:], x_t[:], rs, None, ALU.mult, None, [x_t, stat], [xb])
                ctx[("xb", t)] = xb

            def stage1(t):
                tl = t % 4
                g4 = t // 4
                if tl == 0:
                    Vd_groups[g4] = Vd_st_r.next()
                Vd_st = Vd_groups[g4]
                xb = ctx.pop(("xb", t))
                hT = hTr.next()
                for g in range(2):
                    pt = ptr_ring.next()
                    for j in range(8):
                        c = g * 8 + j
                        TRN(pt[:, j, :], xb[:, c * 128:(c + 1) * 128], [xb], [pt])
                    CP("act" if g == 0 else "dve", hT[:, g * 8:(g + 1) * 8, :], pt[:], [pt], [hT])
                pms = []
                for g, (c0, n) in enumerate(((0, 384), (384, 512), (896, 512), (1408, 512), (1920, 512))):
                    pm = pm_ring.next()
                    for c in range(16):
                        MM(pm[:, 0:n], hT[:, c, :], wk[:, c, c0:c0 + n], c == 0, c == 15, [hT, wk], [pm])
                    pms.append(pm)
                p0 = pms[0]
                stat = statr.next()
                rs = rstd_of(p0[:, 0:256], 256, [p0], junk, stat)
                ckvn = ckvn_r.next()
                TS("dve", ckvn[:], p0[:, 0:256], rs, None, ALU.mult, None, [p0, stat], [ckvn])
                tmp = tmp_ring.next()
                kr_tok = kr_tok_r.next()
                rope(p0[:, 256:320].rearrange("p (h d) -> p h d", h=1), kr_tok[:, 0:64].rearrange("p (h d) -> p h d", h=1),
                     1, 32, cosK[:, t:t + 1, :], sinK[:, t:t + 1, :], tmp, [p0, cosK, sinK], [kr_tok])
                CP("pool", kr_tok[:, 64:128], kr_tok[:, 0:64], [kr_tok], [kr_tok])
                tmp = tmp_ring.next()
                ki_tok = ki_tok_r.next()
                rope(p0[:, 320:384].rearrange("p (h d) -> p h d", h=1), ki_tok[:, 0:64].rearrange("p (h d) -> p h d", h=1),
                     1, 8, cosK[:, t:t + 1, 0:32:4], sinK[:, t:t + 1, 0:32:4], tmp, [p0, cosK, sinK], [ki_tok])
                CP("act", ki_tok[:, 16:64], p0[:, 336:384], [p0], [ki_tok])
                CP("pool", ki_tok[:, 64:128], ki_tok[:, 0:64], [ki_tok], [ki_tok])
                kd_tok = kd_tok_r.next()
                for g in range(2):
                    pm = pms[1 + g]
                    tmp = tmp_ring.next()
                    src = pm[:].rearrange("p (h d) -> p h d", h=4)
                    dst = kd_tok[:, g * 4:(g + 1) * 4, :]
                    rope(src, dst, 4, 16, cosK[:, t:t + 1, 0:32:2], sinK[:, t:t + 1, 0:32:2], tmp, [pm, cosK, sinK], [kd_tok])
                    CP("act", dst[:, :, 32:128], src[:, :, 32:128], [pm], [kd_tok])
                for g in range(2):
                    pm = pms[3 + g]
                    CP("act" if g == 0 else "dve", Vd_st[:, g * 4:(g + 1) * 4, tl, 0:128],
                       pm[:].rearrange("p (h d) -> p h d", h=4), [pm], [Vd_st])
                if tl == 3:
                    P.dma("sp", Vd_scr[:, :, g4 * 4:(g4 + 1) * 4, :].rearrange("h p t c -> p h t c"), Vd_st[:],
                          reads=[Vd_st])
                ctx[t] = (ckvn, kr_tok, ki_tok, kd_tok, Vd_st)

            ctx2, ctx3 = {}, {}

            def s2a(t):
                tl = t % 4
                ckvn, kr_tok, ki_tok, kd_tok, Vd_st = ctx[t]
                KdT_st = KdT_st_r.items[0]
                ckvT = ckvT_r.next()
                pt = ptr_ring.next()
                for c in range(2):
                    TRN(pt[:, c, :], ckvn[:, c * 128:(c + 1) * 128], [ckvn], [pt])
                CP("dve", ckvT[:], pt[:, 0:2, :], [pt], [ckvT])
                pt = ptr_ring.next()
                for h in range(8):
                    TRN(pt[:, h, :], kd_tok[:, h, :], [kd_tok], [pt])
                CP("act", KdT_st[:, :, tl * 128:(tl + 1) * 128], pt[:], [pt], [KdT_st])
                pt = ptr_ring.next()
                TRN(pt[:, 0, :], kr_tok[:], [kr_tok], [pt])
                TRN(pt[:, 1, :], ki_tok[:], [ki_tok], [pt])
                CP("act", kropeT[:, t * 128:(t + 1) * 128], pt[:, 0, :], [pt], [kropeT])
                CP("dve", kidx_lo[0:64, t * 128:(t + 1) * 128], pt[0:64, 1, :], [pt], [kidx_lo])
                CP("dve", kidx_hi[64:128, t * 128:(t + 1) * 128], pt[64:128, 1, :], [pt], [kidx_hi])
                if tl == 3:
                    g4 = t // 4
                    P.dma("sp", KdT_scr[:, :, g4 * 512:(g4 + 1) * 512], KdT_st[:], reads=[KdT_st])
                ctx2[t] = ckvT

            def s2b(t):
                tl = t % 4
                ckvn, kr_tok, ki_tok, kd_tok, Vd_st = ctx.pop(t)
                ckvT = ctx2.pop(t)
                Vm_st = Vm_st_r.items[0]
                kn_tok = kn_tok_r.next()
                for g in range(4):
                    pm = pm_ring.next()
                    for c in range(2):
                        MM(pm[:], ckvT[:, c, :], wkv[:, c, g * 512:(g + 1) * 512], c == 0, c == 1, [ckvT, wkv], [pm])
                    v4 = pm[:].rearrange("p (h e d) -> p h e d", h=2, e=2)
                    CP("act", kn_tok[:, 2 * g:2 * g + 2, :], v4[:, :, 0, :], [pm], [kn_tok])
                    CP("dve", Vm_st[:, 2 * g:2 * g + 2, tl, 0:128], v4[:, :, 1, :], [pm], [Vm_st])
                if tl == 3:
                    g4 = t // 4
                    P.dma("sp", Vm_scr[:, :, g4 * 4:(g4 + 1) * 4, :].rearrange("h p t c -> p h t c"), Vm_st[:],
                          reads=[Vm_st])
                ctx3[t] = kn_tok

            def s2c(t):
                tl = t % 4
                kn_tok = ctx3.pop(t)
                KnT_st = KnT_st_r.items[0]
                pt = ptr_ring.next()
                for h in range(8):
                    TRN(pt[:, h, :], kn_tok[:, h, :], [kn_tok], [pt])
                CP("dve", KnT_st[:, :, tl * 128:(tl + 1) * 128], pt[:], [pt], [KnT_st])
                if tl == 3:
                    g4 = t // 4
                    P.dma("sp", KnT_scr[:, :, g4 * 512:(g4 + 1) * 512], KnT_st[:], reads=[KnT_st])

            stage0(0)
            stage0(1)
            stage1(0)
            for t in range(NT):
                if t + 2 < NT:
                    stage0(t + 2)
                s2a(t)
                if t + 1 < NT:
                    stage1(t + 1)
                s2b(t)
                if t >= 1:
                    s2c(t - 1)
            s2c(NT - 1)
            P.barrier()
            dump("A", "kropeT", kropeT[:], [128, S], BF16)
            dump("A", "KnT", KnT_scr, [128, 8, S], BF16)
            dump("A", "KdT", KdT_scr, [128, 8, S], BF16)
            dump("A", "Vm", Vm_scr, [8, 128, NT, 129], BF16)
            dump("A", "Vd", Vd_scr, [8, 128, NT, 129], BF16)
            if stop == "A":
                P.barrier()
            checkpoint("A")

        with ExitStack() as stB:
            QnT = alloc(stB, "QnT", [128, 8, NQ * 128], BF16)
            QrT_lo = alloc(stB, "QrT_lo", [128, 4, NQ * 128], BF16)
            QrT_hi = alloc(stB, "QrT_hi", [128, 4, NQ * 128], BF16)
            MEMSET("pool", QrT_lo[:], 0.0, [QrT_lo])
            MEMSET("pool", QrT_hi[:], 0.0, [QrT_hi])
            QdT = alloc(stB, "QdT", [128, 8, NQ * 128], BF16)
            with ExitStack() as stI:
                qiT = alloc(stI, "qiT", [128, 8, NQ * 128], BF16)
                wsc = alloc(stI, "wsc", [128, NQ, 16], F32)
                cosQ = alloc(stI, "cosQ", [128, NQ, 32], F32)
                sinQ = alloc(stI, "sinQ", [128, NQ, 32], F32)
                make_tables(posq, NQ, cosQ, sinQ)

                def qside(pass_id):
                    with ExitStack() as st:
                        ncol = 1552 if pass_id == 0 else 1024
                        col0 = 0 if pass_id == 0 else 1552
                        wq = alloc(st, "wq", [128, 16, ncol], BF16)
                        gm = load_g(st, "g_mix2", g_mix, 16)
                        for c in range(16):
                            P.dma("pool", wq[:, c, :], wq_side[c * 128:(c + 1) * 128, col0:col0 + ncol], writes=[wq])
                        for c in range(16):
                            TS("dve", wq[:, c, :], wq[:, c, :], gm[:, c:c + 1], None, ALU.mult, None, [wq, gm], [wq])
                        if pass_id == 0:
                            wqb = alloc(st, "wqb", [128, 4, 1536], BF16)
                            gq = load_g(st, "g_cq", g_cq, 4)
                            load_w(wqb, w_qb, 4, gq)
                        xring = Ring(st, "xb1", 2, [128, D], F32)
                        junk = alloc(st, "junkB", [128, 512], BF16)
                        statr = Ring(st, "statB", 6, [128, 4], F32)
                        xbr = Ring(st, "xbB", 2, [128, D], BF16)
                        hTr = Ring(st, "hTB", 2, [128, 16, 128], BF16)
                        ptr_ring = Ring(st, "ptrB", 2, [128, 8, 128], BF16, psum=True)
                        pm_ring = Ring(st, "pmB", 6, [128, 512], F32, psum=True)
                        tmp_ring = Ring(st, "ropetmpB", 2, [128, 4, 256], F32)
                        if pass_id == 0:
                            cqn_r = Ring(st, "cqn", 2, [128, 512], BF16)
                            cqT_r = Ring(st, "cqT", 2, [128, 4, 128], BF16)
                            qn_tok_r = Ring(st, "qn_tok", 2, [128, 8, 128], BF16)
                            qr_tok_r = Ring(st, "qr_tok", 2, [128, 8, 64], BF16)
                            qi_tok_r = Ring(st, "qi_tok", 2, [128, 16, 64], BF16)
                        else:
                            qd_tok_r = Ring(st, "qd_tok", 2, [128, 8, 128], BF16)
                        qctx = {}

                        def q0(s):
                            x_t = xring.next()
                            P.dma("sp", x_t[:], xq[s * 128:(s + 1) * 128, :], writes=[x_t])
                            xb = xbr.next()
                            stat = statr.next()
                            rs = rstd_of(x_t[:], D, [x_t], xb, stat)
                            TS("dve", xb[:], x_t[:], rs, None, ALU.mult, None, [x_t, stat], [xb])
                            qctx[s] = xb

                        q0(0)
                        for s in range(NQ):
                            qs = slice(s * 128, (s + 1) * 128)
                            if s + 1 < NQ:
                                q0(s + 1)
                            xb = qctx.pop(s)
                            hT = hTr.next()
                            for g in range(2):
                                pt = ptr_ring.next()
                                for j in range(8):
                                    c = g * 8 + j
                                    TRN(pt[:, j, :], xb[:, c * 128:(c + 1) * 128], [xb], [pt])
                                CP("act" if g == 0 else "dve", hT[:, g * 8:(g + 1) * 8, :], pt[:], [pt], [hT])
                            if pass_id == 0:
                                pms = []
                                for g, (c0, n) in enumerate(((0, 512), (512, 512), (1024, 512), (1536, 16))):
                                    pm = pm_ring.next()
                                    for c in range(16):
                                        MM(pm[:, 0:n], hT[:, c, :], wq[:, c, c0:c0 + n], c == 0, c == 15, [hT, wq], [pm])
                                    pms.append(pm)
                                TS("dve", wsc[:, s, :], pms[3][:, 0:16], 1.0 / 32.0, None, ALU.mult, None, [pms[3]], [wsc])
                                stat = statr.next()
                                rs = rstd_of(pms[0][:], 512, [pms[0]], junk, stat)
                                cqn = cqn_r.next()
                                TS("dve", cqn[:], pms[0][:], rs, None, ALU.mult, None, [pms[0], stat], [cqn])
                                qi_tok = qi_tok_r.next()
                                for g in range(2):
                                    pm = pms[1 + g]
                                    tmp = tmp_ring.next()
                                    src = pm[:].rearrange("p (h d) -> p h d", h=8)
                                    dst = qi_tok[:, g * 8:(g + 1) * 8, :]
                                    rope(src, dst, 8, 8, cosQ[:, s:s + 1, 0:32:4], sinQ[:, s:s + 1, 0:32:4], tmp,
                                         [pm, cosQ, sinQ], [qi_tok])
                                    CP("act", dst[:, :, 16:64], src[:, :, 16:64], [pm], [qi_tok])
                                cqT = cqT_r.next()
                                pt = ptr_ring.next()
                                for c in range(4):
                                    TRN(pt[:, c, :], cqn[:, c * 128:(c + 1) * 128], [cqn], [pt])
                                CP("dve", cqT[:], pt[:, 0:4, :], [pt], [cqT])
                                qn_tok = qn_tok_r.next()
                                qr_tok = qr_tok_r.next()
                                for g in range(3):
                                    pm = pm_ring.next()
                                    for c in range(4):
                                        MM(pm[:], cqT[:, c, :], wqb[:, c, g * 512:(g + 1) * 512], c == 0, c == 3, [cqT, wqb], [pm])
                                    if g < 2:
                                        CP("act", qn_tok[:, g * 4:(g + 1) * 4, :], pm[:].rearrange("p (h d) -> p h d", h=4),
                                           [pm], [qn_tok])
                                    else:
                                        tmp = tmp_ring.next()
                                        rope(pm[:].rearrange("p (h d) -> p h d", h=8), qr_tok[:], 8, 32,
                                             cosQ[:, s:s + 1, :], sinQ[:, s:s + 1, :], tmp, [pm, cosQ, sinQ], [qr_tok])
                                pt = ptr_ring.next()
                                for h in range(8):
                                    TRN(pt[:, h, :], qn_tok[:, h, :], [qn_tok], [pt])
                                CP("act", QnT[:, :, qs], pt[:], [pt], [QnT])
                                pt = ptr_ring.next()
                                qr2 = qr_tok[:].rearrange("p (a b) d -> p a (b d)", b=2)
                                for h2 in range(4):
                                    TRN(pt[:, h2, :], qr2[:, h2, :], [qr_tok], [pt])
                                CP("dve", QrT_lo[0:64, :, qs], pt[0:64, 0:4, :], [pt], [QrT_lo])
                                CP("dve", QrT_hi[64:128, :, qs], pt[64:128, 0:4, :], [pt], [QrT_hi])
                                pt = ptr_ring.next()
                                qi2 = qi_tok[:].rearrange("p (a b) d -> p a (b d)", b=2)
                                for h2 in range(8):
                                    TRN(pt[:, h2, :], qi2[:, h2, :], [qi_tok], [pt])
                                CP("act", qiT[:, :, qs], pt[:], [pt], [qiT])
                            else:
                                pms = []
                                for g in range(2):
                                    pm = pm_ring.next()
                                    for c in range(16):
                                        MM(pm[:], hT[:, c, :], wq[:, c, g * 512:(g + 1) * 512], c == 0, c == 15, [hT, wq], [pm])
                                    pms.append(pm)
                                qd_tok = qd_tok_r.next()
                                for g in range(2):
                                    pm = pms[g]
                                    tmp = tmp_ring.next()
                                    src = pm[:].rearrange("p (h d) -> p h d", h=4)
                                    dst = qd_tok[:, g * 4:(g + 1) * 4, :]
                                    rope(src, dst, 4, 16, cosQ[:, s:s + 1, 0:32:2], sinQ[:, s:s + 1, 0:32:2], tmp,
                                         [pm, cosQ, sinQ], [qd_tok])
                                    CP("act", dst[:, :, 32:128], src[:, :, 32:128], [pm], [qd_tok])
                                pt = ptr_ring.next()
                                for h in range(8):
                                    TRN(pt[:, h, :], qd_tok[:, h, :], [qd_tok], [pt])
                                CP("dve", QdT[:, :, qs], pt[:], [pt], [QdT])
                        P.barrier()

                qside(0)
                qside(1)
                dump("B1", "QnT", QnT[:], [128, 8, NQ * 128], BF16)
                dump("B1", "QdT", QdT[:], [128, 8, NQ * 128], BF16)
                dump("B1", "qiT", qiT[:], [128, 8, NQ * 128], BF16)
                dump("B1", "wsc", wsc[:], [128, NQ, 16], F32)
                if stop == "B1":
                    P.barrier()
                checkpoint("B1")

                with ExitStack() as st:
                    zb_ring = Ring(st, "zb", 4, [128, 512], F32)
                    pz_ring = Ring(st, "pz", 3, [128, 512], F32, psum=True)
                    psc_ring = Ring(st, "psc", 2, [128, 512], F32, psum=True)
                    ptr_ring = Ring(st, "ptrI", 2, [128, 8, 128], BF16, psum=True)
                    r_ring = Ring(st, "relu", 4, [128, 512], BF16)
                    dg_ring = Ring(st, "dg", 3, [128, 16, 128], BF16)
                    sc_ring = Ring(st, "score", 4, [128, S], F32)
                    junkI_d = alloc(st, "junkI", [128, S], mybir.dt.uint8)
                    junkI_p = alloc(st, "junkIp", [128, S], mybir.dt.uint8)
                    mask_ring = Ring(st, "mask", 1, [128, S], BF16)
                    mT_ring = Ring(st, "mTst", 1, [128, NT, 128], BF16)
                    bs_ring = Ring(st, "bis", 4, [128, 8 + NBIS], F32)
                    blk_of = [0]
                    for s_ in range(NQ):
                        blk_of.append(blk_of[-1] + 4 * (s_ + 1))

                    def front(s):
                        E = 4 * (s + 1)
                        n = E * 128
                        qs = slice(s * 128, (s + 1) * 128)
                        zb_t = zb_ring.next()
                        P.dma("sp", zb_t[:], zb[:, s, :], writes=[zb_t])
                        dg = dg_ring.next()
                        for h in range(16):
                            TS("pool", dg[:, h, :], identb[:], wsc[:, s, h:h + 1], None, ALU.mult, None, [identb, wsc], [dg])
                        score = sc_ring.next()
                        for kb in range(s + 1):
                            ks = slice(kb * 512, (kb + 1) * 512)
                            psc = psc_ring.next()
                            rprev = None
                            for h in range(16):
                                p0_ = (h % 2) * 64
                                pz = pz_ring.next()
                                kix = kidx_lo if h % 2 == 0 else kidx_hi
                                MM(pz[:], qiT[:, h // 2, qs], kix[:, ks], True, True, [qiT, kix], [pz])
                                rb = r_ring.next()
                                ACTV(rb[:], pz[:], AF.Relu, [pz], [rb])
                                if rprev is not None:
                                    MM(psc[:], dg[:, h - 1, :], rprev[:], h - 1 == 0, False, [dg, rprev], [psc])
                                rprev = rb
                            MM(psc[:], dg[:, 15, :], rprev[:], False, True, [dg, rprev], [psc])
                            CP("act", score[:, ks], psc[:], [psc], [score])
                        return score, zb_t

                    def bis(s, score, zb_t, junkI, bs):
                        n = 4 * (s + 1) * 128
                        P.op("dve", lambda e, o_=bs[:, 0:1], i_=score[:, 0:n]: e.tensor_reduce(out=o_, in_=i_, axis=AX.X, op=ALU.max),
                             [score], [bs])
                        yield
                        P.op("dve", lambda e, o_=bs[:, 1:2], i_=score[:, 0:n]: e.tensor_reduce(out=o_, in_=i_, axis=AX.X, op=ALU.min),
                             [score], [bs])
                        yield
                        TTO("pool", score[:, n - 512:n], score[:, n - 512:n], zb_t[:], ALU.add, [score, zb_t, bs], [score])
                        TTO("dve", bs[:, 2:3], bs[:, 0:1], bs[:, 1:2], ALU.subtract, [bs], [bs])
                        yield
                        TS("dve", bs[:, 8:8 + NBIS], pow2_t[:], bs[:, 2:3], None, ALU.mult, None, [pow2_t, bs], [bs])
                        yield
                        mid = bs[:, 3:4]
                        TS("dve", mid, bs[:, 1:2], bs[:, 8:9], None, ALU.add, None, [bs], [bs])
                        yield
                        for i in range(NBIS):
                            Hi = bs[:, 8 + i:9 + i]
                            Hn = bs[:, 9 + i:10 + i] if i + 1 < NBIS else Hi
                            TS("dve", junkI[:, 0:n], score[:, 0:n], mid, 0.0, ALU.is_ge, ALU.add, [score, bs], [junkI, bs],
                               accum_out=bs[:, 4:5])
                            yield
                            STT("dve", bs[:, 5:6], bs[:, 4:5], 255.5, Hi, ALU.is_ge, ALU.mult, [bs], [bs])
                            yield
                            STT("dve", mid, mid, Hn, bs[:, 5:6], ALU.subtract, ALU.add, [bs], [bs])
                            yield

                    def back(s, score, bs):
                        E = 4 * (s + 1)
                        n = E * 128
                        mask = mask_ring.next()
                        TS("dve", mask[:, 0:n], score[:, 0:n], bs[:, 3:4], None, ALU.is_ge, None, [score, bs], [mask])
                        mT = mT_ring.next()
                        for g in range((E + 7) // 8):
                            pt = ptr_ring.next()
                            nb_ = min(8, E - g * 8)
                            for j in range(nb_):
                                kt = g * 8 + j
                                TRN(pt[:, j, :], mask[:, kt * 128:(kt + 1) * 128], [mask], [pt])
                            CP("act", mT[:, g * 8:g * 8 + nb_, :], pt[:, 0:nb_, :], [pt], [mT])
                        P.dma("sp", maskT_scr[:, blk_of[s]:blk_of[s] + E, :], mT[:, 0:E, :], reads=[mT])

                    fronts = {0: front(0), 1: front(1)}
                    for a in range(0, NQ, 2):
                        b = a + 1
                        sa, za = fronts.pop(a)
                        sb_, zb2 = fronts.pop(b)
                        if a + 2 < NQ:
                            fronts[a + 2] = front(a + 2)
                            fronts[a + 3] = front(a + 3)
                        bsa, bsb = bs_ring.next(), bs_ring.next()
                        ga = bis(a, sa, za, junkI_d, bsa)
                        gb = bis(b, sb_, zb2, junkI_p, bsb)
                        live = [ga, gb]
                        while live:
                            for g_ in list(live):
                                try:
                                    next(g_)
                                except StopIteration:
                                    live.remove(g_)
                        back(a, sa, bsa)
                        back(b, sb_, bsb)
                    score, bs = sb_, bsb
                    P.barrier()
                    dump("B2", "maskT", maskT_scr, [128, 144, 128], BF16)
                    dump("B2", "score", score[:], [128, S], F32)
                    dump("B2", "bs", bs[:], [128, 8 + NBIS], F32)
                    if stop == "B2":
                        P.barrier()
                    checkpoint("B2")

            with ExitStack() as st:
                maskT = alloc(st, "maskT", [128, 144, 128], BF16)
                zmT_t = alloc(st, "zmT", [128, NQ, 512], BF16)
                O_all = alloc(st, "O_all", [128, NQ, D], BF16)
                P.dma("sp", maskT[:], maskT_scr, writes=[maskT])
                P.dma("pool", zmT_t[:], zmT, writes=[zmT_t])
                KT_ring = Ring(st, "KT", 2, [128, S], BF16)
                V_ring = Ring(st, "V", 2, [128, NT, 129], BF16)
                pS_ring = Ring(st, "pS", 5, [128, 512], F32, psum=True)
                pO_ring = Ring(st, "pO", 2, [128, 512], F32, psum=True)
                PT_ring = Ring(st, "PT", 6, [128, 512], BF16)
                rs_ring = Ring(st, "rsum", 4, [128, 1], F32)
                blk_off = [0]
                for s in range(NQ):
                    blk_off.append(blk_off[-1] + 4 * (s + 1))
                groups = [(s, kb) for s in range(NQ) for kb in range(s + 1)]
                for hh in range(16):
                    mla = hh < 8
                    h = hh % 8
                    KT = KT_ring.next()
                    V = V_ring.next()
                    if mla:
                        P.dma("sp", KT[:], KnT_scr[:, h, :], writes=[KT])
                        P.dma("sp", V[:], Vm_scr[h], writes=[V])
                        scale = 192.0 ** -0.5
                    else:
                        P.dma("sp", KT[:], KdT_scr[:, h, :], writes=[KT])
                        P.dma("sp", V[:], Vd_scr[h], writes=[V])
                        scale = 128.0 ** -0.5
                    p0_ = (h % 2) * 64

                    def qk(s, kb):
                        qs = slice(s * 128, (s + 1) * 128)
                        pS = pS_ring.next()
                        for j in range(4):
                            kt = 4 * kb + j
                            ks = slice(kt * 128, (kt + 1) * 128)
                            if mla:
                                MM(pS[:, j * 128:(j + 1) * 128], KT[:, ks], QnT[:, h, qs], True, False, [KT, QnT], [pS])
                                QrP = QrT_lo if h % 2 == 0 else QrT_hi
                                MM(pS[:, j * 128:(j + 1) * 128], kropeT[:, ks], QrP[:, h // 2, qs],
                                   False, True, [kropeT, QrP], [pS])
                            else:
                                MM(pS[:, j * 128:(j + 1) * 128], KT[:, ks], QdT[:, h, qs], True, True, [KT, QdT], [pS])
                        return pS

                    LOOK = 3
                    pend = [qk(*groups[i]) for i in range(LOOK)]
                    pO = None
                    deferred = None
                    for gi, (s, kb) in enumerate(groups):
                        pS = pend.pop(0)
                        if gi + LOOK < len(groups):
                            pend.append(qk(*groups[gi + LOOK]))
                        PT = PT_ring.next()
                        ACTV(PT[:], pS[:], AF.Exp, [pS], [PT], scale=scale)
                        if mla:
                            if kb == s:
                                TTO("dve", PT[:], PT[:], zmT_t[:, s, :], ALU.mult, [PT, zmT_t], [PT])
                        else:
                            b0 = blk_off[s] + 4 * kb
                            TTO("dve" if gi % 4 != 3 else "pool", PT[:], PT[:],
                                maskT[:, b0:b0 + 4, :].rearrange("p a b -> p (a b)"), ALU.mult, [PT, maskT], [PT])
                        if kb == 0:
                            pO = pO_ring.next()
                        E = 4 * (s + 1)
                        for j in range(4):
                            kt = 4 * kb + j
                            MM(pO[:, 0:129], PT[:, j * 128:(j + 1) * 128], V[:, kt, :], kt == 0, kt == E - 1, [PT, V], [pO])
                        if deferred is not None:
                            deferred()
                            deferred = None
                        if kb == s:
                            def deferred(pO=pO, s=s, hh=hh):
                                rsum = rs_ring.next()
                                P.op("dve", lambda e, o_=rsum[:], i_=pO[:, 128:129]: e.reciprocal(out=o_, in_=i_), [pO], [rsum])
                                col = hh * 128
                                ACTV(O_all[:, s, col:col + 128], pO[:, 0:128], AF.Copy, [pO, rsum], [O_all], scale=rsum[:])
                    if deferred is not None:
                        deferred()
                        deferred = None
                P.dma("sp", O_scr.rearrange("(s p) d -> p s d", p=128), O_all[:], reads=[O_all])
                P.barrier()
                dump("B3", "O", O_scr, [NQ * 128, D], BF16)
                if stop == "B3":
                    P.barrier()
                checkpoint("B3")

        stK.close()
        with ExitStack() as stR:
            x1 = alloc(stR, "x1", [128, NQ, D], F32)
            with ExitStack() as st:
                wo = alloc(st, "wo", [128, 16, D], BF16)
                load_w(wo, w_out, 16)
                xring = Ring(st, "xc", 2, [128, D], F32)
                Or = Ring(st, "Oc", 2, [128, D], BF16)
                OT_r = Ring(st, "OT", 2, [128, 16, 128], BF16)
                ptr_ring = Ring(st, "ptrC", 2, [128, 8, 128], BF16, psum=True)
                pm_ring = Ring(st, "pmC", 4, [128, 512], F32, psum=True)
                for s in range(NQ):
                    qs = slice(s * 128, (s + 1) * 128)
                    x_t = xring.next()
                    P.dma("sp", x_t[:], xq[qs, :], writes=[x_t])
                    O_t = Or.next()
                    P.dma("sp", O_t[:], O_scr[qs, :], writes=[O_t])
                    OT = OT_r.next()
                    for g in range(2):
                        pt = ptr_ring.next()
                        for j in range(8):
                            c = g * 8 + j
                            TRN(pt[:, j, :], O_t[:, c * 128:(c + 1) * 128], [O_t], [pt])
                        CP("act" if g == 0 else "dve", OT[:, g * 8:(g + 1) * 8, :], pt[:], [pt], [OT])
                    for g in range(4):
                        pm = pm_ring.next()
                        for c in range(16):
                            MM(pm[:], OT[:, c, :], wo[:, c, g * 512:(g + 1) * 512], c == 0, c == 15, [OT, wo], [pm])
                        TTO("dve", x1[:, s, g * 512:(g + 1) * 512], pm[:], x_t[:, g * 512:(g + 1) * 512], ALU.add,
                            [pm, x_t], [x1])
                P.barrier()
                dump("C", "x1C", x1[:], [128, NQ, D], F32)
                if stop == "C":
                    P.barrier()
                checkpoint("C")

            with ExitStack() as st:
                wq_c = alloc(st, "wqc", [128, 16, 512], BF16)
                wk_c = alloc(st, "wkc", [128, 16, 512], BF16)
                wv_c = alloc(st, "wvc", [128, 16, 512], BF16)
                wo_c = alloc(st, "woc", [128, 4, D], BF16)
                gc = load_g(st, "g_cross", g_cross, 16)
                gme = load_g(st, "g_mem", g_mem, 16)
                load_w(wk_c, wkc, 16, gme)
                load_w(wv_c, wvc, 16, gme)
                load_w(wq_c, wqc, 16, gc)
                load_w(wo_c, woc, 4)
                memT = alloc(st, "memT", [128, 16, 256], BF16)
                KcT = alloc(st, "KcT", [128, 4, 256], BF16)
                Vc = alloc(st, "Vc", [128, 2, 4, 129], BF16)
                MEMSET("pool", Vc[:, :, :, 128:129], 1.0, [Vc])
                xring = Ring(st, "xm", 2, [128, D], F32)
                junk = alloc(st, "junkD", [128, D], BF16)
                statr = Ring(st, "statD", 4, [128, 4], F32)
                xbr = Ring(st, "xbD", 1, [128, D], BF16)
                hTr = Ring(st, "hTD", 2, [128, 16, 128], BF16)
                ptr_ring = Ring(st, "ptrD", 2, [128, 8, 128], BF16, psum=True)
                pm_ring = Ring(st, "pmD", 2, [128, 512], F32, psum=True)
                pS_ring = Ring(st, "pSD", 2, [128, 512], F32, psum=True)
                pO_ring = Ring(st, "pOD", 2, [128, 512], F32, psum=True)
                QcT_r = Ring(st, "QcT", 2, [128, 4, 128], BF16)
                PT_ring = Ring(st, "PTD", 4, [128, 512], BF16)
                rs_ring = Ring(st, "rsD", 2, [128, 4], F32)
                Oc_r = Ring(st, "Oc_tok", 2, [128, 512], BF16)
                OcT_r = Ring(st, "OcT", 2, [128, 4, 128], BF16)
                for mt in range(2):
                    x_t = xring.next()
                    P.dma("sp", x_t[:], mem[mt * 128:(mt + 1) * 128, :], writes=[x_t])
                    norm_transpose(x_t[:], x_t, lambda g, mt=mt: memT[:, g * 8:(g + 1) * 8, mt * 128:(mt + 1) * 128], memT,
                                   junk, statr.next(), xbr.next(), ptr_ring)
                for h in range(4):
                    pm = pm_ring.next()
                    for c in range(16):
                        MM(pm[:, 0:256], wk_c[:, c, h * 128:(h + 1) * 128], memT[:, c, :], c == 0, c == 15, [wk_c, memT], [pm])
                    CP("act", KcT[:, h, :], pm[:, 0:256], [pm], [KcT])
                for mt in range(2):
                    pm = pm_ring.next()
                    for c in range(16):
                        MM(pm[:], memT[:, c, mt * 128:(mt + 1) * 128], wv_c[:, c, :], c == 0, c == 15, [memT, wv_c], [pm])
                    CP("dve", Vc[:, mt, :, 0:128], pm[:].rearrange("p (h d) -> p h d", h=4), [pm], [Vc])
                cscale = 128.0 ** -0.5
                dctx = {}
                x1d = [Buf("x1d%d" % s_) for s_ in range(NQ)]

                def d1(s):
                        hT = hTr.next()
                        norm_transpose(x1[:, s, :], x1d[s], lambda g, hT=hT: hT[:, g * 8:(g + 1) * 8, :], hT, junk, statr.next(),
                                       xbr.next(), ptr_ring)
                        pm = pm_ring.next()
                        for h in range(4):
                            for c in range(16):
                                MM(pm[:, h * 128:(h + 1) * 128], wq_c[:, c, h * 128:(h + 1) * 128], hT[:, c, :], c == 0, c == 15,
                                   [wq_c, hT], [pm])
                        QcT = QcT_r.next()
                        CP("act", QcT[:], pm[:].rearrange("p (h d) -> p h d", h=4), [pm], [QcT])
                        dctx[s] = QcT

                def d2(s):
                        QcT = dctx.pop(s)
                        PTs = []
                        for hp in range(2):
                            pS = pS_ring.next()
                            for hl in range(2):
                                h = hp * 2 + hl
                                for mt in range(2):
                                    cs = (hl * 2 + mt) * 128
                                    MM(pS[:, cs:cs + 128], KcT[:, h, mt * 128:(mt + 1) * 128], QcT[:, h, :], True, True, [KcT, QcT], [pS])
                            PT = PT_ring.next()
                            ACTV(PT[:], pS[:], AF.Exp, [pS], [PT], scale=cscale)
                            PTs.append(PT)
                        rsum = rs_ring.next()
                        Oc = Oc_r.next()
                        for hp in range(2):
                            pO = pO_ring.next()
                            PT = PTs[hp]
                            for hl in range(2):
                                h = hp * 2 + hl
                                for mt in range(2):
                                    cs = (hl * 2 + mt) * 128
                                    MM(pO[:, hl * 256:hl * 256 + 129], PT[:, cs:cs + 128], Vc[:, mt, h, :], mt == 0, mt == 1, [PT, Vc], [pO])
                            for hl in range(2):
                                h = hp * 2 + hl
                                P.op("dve", lambda e, o_=rsum[:, h:h + 1], i_=pO[:, hl * 256 + 128:hl * 256 + 129]: e.reciprocal(out=o_, in_=i_), [pO], [rsum])
                                ACTV(Oc[:, h * 128:(h + 1) * 128], pO[:, hl * 256:hl * 256 + 128], AF.Copy, [pO, rsum], [Oc], scale=rsum[:, h:h + 1])
                        OcT = OcT_r.next()
                        pt = ptr_ring.next()
                        for c in range(4):
                            TRN(pt[:, c, :], Oc[:, c * 128:(c + 1) * 128], [Oc], [pt])
                        CP("dve", OcT[:], pt[:, 0:4, :], [pt], [OcT])
                        for g in range(4):
                            pm = pm_ring.next()
                            for c in range(4):
                                MM(pm[:], OcT[:, c, :], wo_c[:, c, g * 512:(g + 1) * 512], c == 0, c == 3, [OcT, wo_c], [pm])
                            TTO("dve", x1[:, s, g * 512:(g + 1) * 512], pm[:], x1[:, s, g * 512:(g + 1) * 512], ALU.add,
                                [pm, x1d[s]], [x1d[s]])

                d1(0)
                for s in range(NQ):
                    if s + 1 < NQ:
                        d1(s + 1)
                    d2(s)
                P.barrier()
                dump("D", "x1D", x1[:], [128, NQ, D], F32)
                if stop == "D":
                    P.barrier()
                checkpoint("D")

            with ExitStack() as st:
                hmT = alloc(st, "hmT", [128, 16, NQ * 128], BF16)
                with ExitStack() as st2:
                    junk = alloc(st2, "junkE", [128, D], BF16)
                    statr = Ring(st2, "statE", 4, [128, 4], F32)
                    xbr = Ring(st2, "xbE", 2, [128, D], BF16)
                    ptr_ring = Ring(st2, "ptrE", 2, [128, 8, 128], BF16, psum=True)
                    for s in range(NQ):
                        norm_transpose(x1[:, s, :], x1, lambda g, s=s: hmT[:, g * 8:(g + 1) * 8, s * 128:(s + 1) * 128], hmT,
                                       junk, statr.next(), xbr.next(), ptr_ring)
                    P.barrier()
                with ExitStack() as st2:
                    gml = load_g(st2, "g_mlp", g_mlp, 16)
                    wu_ring = Ring(st2, "wu", 2, [128, 16, 512], BF16)
                    wd_ring = Ring(st2, "wd", 2, [128, 4, D], BF16)
                    aT_ring = Ring(st2, "aT", 2, [128, 4, NQ * 128], BF16)
                    rt_ring = Ring(st2, "rt", 2, [128, 512], F32)
                    et_ring = Ring(st2, "et", 2, [128, 512], F32)
                    x1s = [[Buf("x1_%d_%d" % (s_, g_)) for g_ in range(4)] for s_ in range(NQ)]
                    pu_ring = Ring(st2, "pu", 3, [128, 512], F32, psum=True)
                    pd_ring = Ring(st2, "pd", 4, [128, 512], F32, psum=True)
                    NCH = DFF // 512

                    def load_chunk(ch):
                        wu = wu_ring.next()
                        wd = wd_ring.next()
                        P.dma("pool", wu[:], w_up[:, ch * 512:(ch + 1) * 512].rearrange("(c p) n -> p c n", p=128), writes=[wu])
                        P.dma("pool", wd[:], w_down[ch * 512:(ch + 1) * 512, :].rearrange("(f p) n -> p f n", p=128), writes=[wd])
                        for c in range(16):
                            TS("dve", wu[:, c, :], wu[:, c, :], gml[:, c:c + 1], None, ALU.mult, None, [wu, gml], [wu])
                        return wu, wd

                    nxt = load_chunk(0)
                    for ch in range(NCH):
                        wu, wd = nxt
                        if ch + 1 < NCH:
                            nxt = load_chunk(ch + 1)
                        aT = aT_ring.next()
                        for fb in range(4):
                            for half in range(2):
                                pu = pu_ring.next()
                                ts_ = slice(half * 512, (half + 1) * 512)
                                for c in range(16):
                                    MM(pu[:], wu[:, c, fb * 128:(fb + 1) * 128], hmT[:, c, ts_], c == 0, c == 15, [wu, hmT], [pu])
                                rt = rt_ring.next()
                                ACTV(rt[:], pu[:], AF.Relu, [pu], [rt])
                                TTO("pool", aT[:, fb, ts_], rt[:], rt[:], ALU.mult, [rt], [aT])
                        for s in range(NQ):
                            for g in range(4):
                                pd = pd_ring.next()
                                for fb in range(4):
                                    MM(pd[:], aT[:, fb, s * 128:(s + 1) * 128], wd[:, fb, g * 512:(g + 1) * 512], fb == 0, fb == 3,
                                       [aT, wd], [pd])
                                xs_ = x1[:, s, g * 512:(g + 1) * 512]
                                if g % 2 == 0:
                                    TTO("dve", xs_, pd[:], xs_, ALU.add, [pd, x1s[s][g]], [x1s[s][g]])
                                else:
                                    et = et_ring.next()
                                    CP("act", et[:], pd[:], [pd], [et])
                                    TTO("pool", xs_, et[:], xs_, ALU.add, [et, x1s[s][g]], [x1s[s][g]])
                    P.barrier()
            with ExitStack() as st:
                gf = alloc(st, "g_fin", [128, D], F32)
                P.dma("sp", gf[:], g_fin, writes=[gf])
                junk = alloc(st, "junkF", [128, D], BF16)
                statr = Ring(st, "statF", 4, [128, 4], F32)
                y_ring = Ring(st, "y", 2, [128, D], F32)
                outs = []
                for s in range(NQ):
                    stat = statr.next()
                    rs = rstd_of(x1[:, s, :], D, [x1], junk, stat)
                    y = y_ring.next()
                    STT("dve", y[:], x1[:, s, :], rs, gf[:], ALU.mult, ALU.mult, [x1, stat, gf], [y])
                    outs.append(P.dma("sp", out[s * 128:(s + 1) * 128, :], y[:], reads=[y]))
        final = [o_ for o_ in outs if o_ is not None]
      except _Stop:
        final = []
      P.emit(nc, final_waits=list(final) + dump_ops)
    return nc


def _qtiles(r):
    return [r, 7 - r, 8 + r, 15 - r, 16 + r, 23 - r, 24 + r, 31 - r]


_NC_CACHE = {}
_PREP_ONLY = [False]


def kernel(x, mem, positions, g_mix, w_in, g_cq, g_ckv, w_qb, w_kvb, w_out, g_cross, g_mem, w_q_cross, w_k_cross,
           w_v_cross, w_o_cross, g_mlp, w_up, w_down, g_final):
    f32 = np.float32
    x = np.asarray(x, f32)
    mem = np.asarray(mem, f32)
    positions = np.asarray(positions, np.int32)
    w_in0 = np.asarray(w_in, f32)[0]
    o = np.cumsum([0, 512, 256, 64, 1024, 1024, 1024, 1024, 64, 16])
    c_q, c_kv, k_rope, q_d, k_d, v_d, q_idx, k_idx, w_idx = [w_in0[:, o[i]:o[i + 1]] for i in range(9)]
    wk_side = np.ascontiguousarray(np.concatenate([c_kv, k_rope, k_idx, k_d, v_d], axis=1))
    wq_side = np.ascontiguousarray(np.concatenate([c_q, q_idx, w_idx, q_d], axis=1))
    wqb0 = np.asarray(w_qb, f32)[0].reshape(512, 8, 192)
    wqb_r = np.ascontiguousarray(np.concatenate([wqb0[:, :, :128].reshape(512, 1024), wqb0[:, :, 128:].reshape(512, 512)], axis=1))

    def gl(g, k):
        return np.ascontiguousarray(np.asarray(g, f32).reshape(k, 128).T)

    common = {
        "invf": np.ascontiguousarray(np.broadcast_to((THETA ** (-np.arange(32, dtype=np.float64) / 32)).astype(f32), (128, 32))),
        "pow2": np.ascontiguousarray(np.broadcast_to((2.0 ** -(np.arange(NBIS) + 1.0)).astype(f32), (128, NBIS))),
        "ident": np.eye(128, dtype=f32),
        "wk_side": wk_side, "wq_side": wq_side,
        "g_mix": gl(np.asarray(g_mix)[0], 16),
        "w_qb": wqb_r, "g_cq": gl(np.asarray(g_cq)[0], 4),
        "w_kvb": np.ascontiguousarray(np.asarray(w_kvb, f32)[0]), "g_ckv": gl(np.asarray(g_ckv)[0], 2),
        "w_out": np.ascontiguousarray(np.asarray(w_out, f32)[0]),
        "g_cross": gl(np.asarray(g_cross)[0], 16), "g_mem": gl(np.asarray(g_mem)[0], 16),
        "wqc": np.ascontiguousarray(np.asarray(w_q_cross, f32)[0]),
        "wkc": np.ascontiguousarray(np.asarray(w_k_cross, f32)[0]),
        "wvc": np.ascontiguousarray(np.asarray(w_v_cross, f32)[0]),
        "woc": np.ascontiguousarray(np.asarray(w_o_cross, f32)[0]),
        "g_mlp": gl(np.asarray(g_mlp)[0], 16),
        "w_up": np.ascontiguousarray(np.asarray(w_up, f32)[0]),
        "w_down": np.ascontiguousarray(np.asarray(w_down, f32)[0]),
        "g_fin": np.ascontiguousarray(np.broadcast_to(np.asarray(g_final, f32).reshape(1, D), (128, D))),
    }
    in_maps = []
    for c in range(8):
        b, r = c // 4, c % 4
        qt = _qtiles(r)
        xq = np.ascontiguousarray(np.concatenate([x[b, t * 128:(t + 1) * 128] for t in qt], axis=0))
        posk = np.ascontiguousarray(positions[b].reshape(NT, 128).T)
        posq = np.ascontiguousarray(np.stack([positions[b, t * 128:(t + 1) * 128] for t in qt], axis=1))
        zmT = np.zeros((128, NQ, 512), f32)
        zbm = np.zeros((128, NQ, 512), f32)
        for s, t in enumerate(qt):
            qpos = t * 128 + np.arange(128)
            for j in range(4):
                kpos = (4 * s + j) * 128 + np.arange(128)
                allowed = (kpos[:, None] // 64) <= (qpos[None, :] // 64)
                zmT[:, s, j * 128:(j + 1) * 128] = allowed
                zbm[:, s, j * 128:(j + 1) * 128] = np.where(allowed.T, 0.0, -BIG)
        m = dict(common)
        m.update({"x_all": np.ascontiguousarray(x[b]), "xq": xq, "posk": posk, "posq": posq,
                  "mem": np.ascontiguousarray(mem[b]), "zmT": zmT, "zb": zbm})
        in_maps.append(m)
    if _PREP_ONLY[0]:
        return in_maps
    if "nc" not in _NC_CACHE:
        _NC_CACHE["nc"] = build_program()
    res = run_bass_kernel_spmd(_NC_CACHE["nc"], in_maps, core_ids=list(range(8)))
    outp = np.zeros((2, S, D), f32)
    for c in range(8):
        b, r = c // 4, c % 4
        o_c = np.asarray(res.results[c]["out"])
        for s, t in enumerate(_qtiles(r)):
            outp[b, t * 128:(t + 1) * 128] = o_c[s * 128:(s + 1) * 128]
    return outp
```
